# Optimizing a Trainium2 kernel written in Bass

```python
import math
import jax, jax.numpy as jnp
from jax import lax
import numpy as np

D_MODEL = 1024
BATCH = 32
SEQ = 2048
DEPTH = 1

HEAD_DIM = 64
DIFF_HEADS = 4
DIFF_QK = 2 * HEAD_DIM
DIFF_V = 2 * HEAD_DIM
DIFF_WIDTH = DIFF_HEADS * DIFF_V
SWA_Q_HEADS = 8
SWA_KV_HEADS = 2
SWA_GROUP = SWA_Q_HEADS // SWA_KV_HEADS
SWA_WIDTH = SWA_Q_HEADS * HEAD_DIM
WINDOW = 128
BLOCK = 128
MIX_WIDTH = DIFF_WIDTH + SWA_WIDTH
N_ATT_HEADS = SWA_Q_HEADS + DIFF_HEADS
COL_DQ = DIFF_HEADS * DIFF_QK
COL_DK = DIFF_HEADS * DIFF_QK
COL_DV = DIFF_HEADS * DIFF_V
COL_SQ = SWA_Q_HEADS * HEAD_DIM
COL_SK = SWA_KV_HEADS * HEAD_DIM
COL_SV = SWA_KV_HEADS * HEAD_DIM
IN_WIDTH = COL_DQ + COL_DK + COL_DV + COL_SQ + COL_SK + COL_SV
SPLITS = (COL_DQ, COL_DQ + COL_DK, COL_DQ + COL_DK + COL_DV, COL_DQ + COL_DK + COL_DV + COL_SQ, COL_DQ + COL_DK + COL_DV + COL_SQ + COL_SK)
PEER_HEADS = 8
N_KEYS = 128
N_EXPERTS = N_KEYS * N_KEYS
PEER_QUERY = 256
PEER_HALF = PEER_QUERY // 2
PEER_TOPK = 16
PEER_CHUNK = 128
LN_EPS = 1e-5
DEEPNORM_ALPHA = (2 * DEPTH) ** 0.25
DEEPNORM_BETA = (8 * DEPTH) ** -0.25
NEG_INF = -1e30

kernel_name = "hybrid_diffattn_swa_peer_deepnorm"


def layer_norm(x, g, b):
    xf = x.astype(jnp.float32)
    mu = jnp.mean(xf, axis=-1, keepdims=True)
    var = jnp.mean(jnp.square(xf - mu), axis=-1, keepdims=True)
    return ((xf - mu) * lax.rsqrt(var + LN_EPS)).astype(x.dtype) * g + b


def rms_norm(x, g):
    xf = x.astype(jnp.float32)
    return (xf * lax.rsqrt(jnp.mean(xf * xf, axis=-1, keepdims=True) + LN_EPS)).astype(x.dtype) * g


def alibi_slopes():
    i = jnp.arange(1, N_ATT_HEADS + 1, dtype=jnp.float32)
    return jnp.exp2(-8.0 * i / N_ATT_HEADS)


def diff_attention(q, k, v, lq1, lk1, lq2, lk2, subln_g, slopes, lambda_init):
    B_, S_ = q.shape[0], q.shape[1]
    lam = (jnp.exp(jnp.sum((lq1 * lk1).astype(jnp.float32)))
           - jnp.exp(jnp.sum((lq2 * lk2).astype(jnp.float32))) + lambda_init)
    scale = HEAD_DIM ** -0.5
    outs = []
    for blk in range(S_ // BLOCK):
        start, end = blk * BLOCK, (blk + 1) * BLOCK
        qb = q[:, start:end]
        kp = k[:, :end]
        vp = v[:, :end]
        s = jnp.einsum('bqhmd,bkhmd->bhmqk', qb, kp).astype(jnp.float32) * scale
        dist = (jnp.arange(start, end)[:, None] - jnp.arange(end)[None, :]).astype(jnp.float32)
        bias = -slopes[:, None, None, None] * dist[None, None]
        s = jnp.where(dist >= 0, s + bias, NEG_INF)
        p = jax.nn.softmax(s, axis=-1)
        a = p[:, :, 0] - lam * p[:, :, 1]
        outs.append(jnp.einsum('bhqk,bkhe->bqhe', a.astype(v.dtype), vp))
    o = jnp.concatenate(outs, axis=1)
    o = rms_norm(o, subln_g) * (1.0 - lambda_init)
    return o.reshape(B_, S_, DIFF_WIDTH)


def swa_attention(q, k, v, sinks, slopes):
    B_, S_ = q.shape[0], q.shape[1]
    nb = S_ // BLOCK
    qb = q.reshape(B_, nb, BLOCK, SWA_KV_HEADS, SWA_GROUP, HEAD_DIM)

    def banded(z):
        zb = z.reshape(B_, nb, BLOCK, SWA_KV_HEADS, HEAD_DIM)
        prev = jnp.pad(zb, ((0, 0), (1, 0), (0, 0), (0, 0), (0, 0)))[:, :-1]
        return jnp.concatenate([prev, zb], axis=2)

    kb, vb = banded(k), banded(v)
    s = jnp.einsum('bnqhgd,bnkhd->bnhgqk', qb, kb).astype(jnp.float32) * (HEAD_DIM ** -0.5)
    i = jnp.arange(BLOCK)[:, None]
    j = jnp.arange(2 * BLOCK)[None, :]
    dist = i - j + BLOCK
    key_pos = jnp.arange(nb)[:, None, None] * BLOCK - BLOCK + j[None]
    valid = (dist >= 0) & (dist < WINDOW) & (key_pos >= 0)
    sl = slopes.reshape(SWA_KV_HEADS, SWA_GROUP)[:, :, None, None]
    s = s - sl * dist.astype(jnp.float32)
    s = jnp.where(valid[:, None, None], s, NEG_INF)
    sink = sinks.astype(jnp.float32).reshape(SWA_KV_HEADS, SWA_GROUP)[:, :, None, None]
    m = jnp.maximum(jnp.max(s, axis=-1, keepdims=True), sink)
    p = jnp.exp(s - m)
    p = p / (jnp.sum(p, axis=-1, keepdims=True) + jnp.exp(sink - m))
    o = jnp.einsum('bnhgqk,bnkhd->bnqhgd', p.astype(v.dtype), vb)
    return o.reshape(B_, S_, SWA_WIDTH)


def peer(h, w_pq, sub_keys, u_tab, v_tab):
    B_, S_, D = h.shape
    hc = h.reshape(-1, PEER_CHUNK, D)

    def chunk(hx):
        q = (hx @ w_pq).reshape(PEER_CHUNK, PEER_HEADS, 2, PEER_HALF)
        sc = jnp.einsum('thpd,hpnd->thpn', q, sub_keys).astype(jnp.float32)
        v_half, i_half = lax.top_k(sc, PEER_TOPK)
        cand = v_half[:, :, 0, :, None] + v_half[:, :, 1, None, :]
        cand_idx = i_half[:, :, 0, :, None] * N_KEYS + i_half[:, :, 1, None, :]
        cand = cand.reshape(PEER_CHUNK, PEER_HEADS, PEER_TOPK * PEER_TOPK)
        cand_idx = cand_idx.reshape(PEER_CHUNK, PEER_HEADS, PEER_TOPK * PEER_TOPK)
        top_s, top_pos = lax.top_k(cand, PEER_TOPK)
        idx = jnp.take_along_axis(cand_idx, top_pos, axis=-1)
        g = jax.nn.softmax(top_s, axis=-1)
        a = jax.nn.gelu(jnp.einsum('thkd,td->thk', u_tab[idx], hx), approximate=False)
        return jnp.einsum('thk,thkd->td', (g * a).astype(hx.dtype), v_tab[idx])

    return lax.map(chunk, hc).reshape(B_, S_, D)


def setup_inputs(seed: int = 0) -> dict:
    key = jax.random.key(seed)
    ks = jax.random.split(key, 20)
    f32 = jnp.float32

    def nrm(k, shape, s):
        return jax.random.normal(k, shape, f32) * s

    col = jnp.arange(IN_WIDTH)
    is_v = ((col >= SPLITS[1]) & (col < SPLITS[2])) | (col >= SPLITS[4])
    col_scale = jnp.where(is_v, DEEPNORM_BETA, 1.0).astype(f32)
    L, D = DEPTH, D_MODEL
    return {
        "x": nrm(ks[0], (BATCH, SEQ, D), 1.0),
        "c": nrm(ks[1], (BATCH, D), 1.0),
        "w_ada": nrm(ks[2], (L, D, 6 * D), D ** -0.5),
        "b_ada": nrm(ks[3], (L, 6 * D), 0.01),
        "w_in": nrm(ks[4], (L, D, IN_WIDTH), D ** -0.5) * col_scale,
        "lambda_q1": nrm(ks[5], (L, HEAD_DIM), 0.1),
        "lambda_k1": nrm(ks[6], (L, HEAD_DIM), 0.1),
        "lambda_q2": nrm(ks[7], (L, HEAD_DIM), 0.1),
        "lambda_k2": nrm(ks[8], (L, HEAD_DIM), 0.1),
        "subln_g": 1.0 + nrm(ks[9], (L, DIFF_V), 0.02),
        "sinks": nrm(ks[10], (L, SWA_Q_HEADS), 0.5),
        "w_out": nrm(ks[11], (L, MIX_WIDTH, D), MIX_WIDTH ** -0.5 * DEEPNORM_BETA),
        "ln1_g": 1.0 + nrm(ks[12], (L, D), 0.02),
        "ln1_b": nrm(ks[13], (L, D), 0.02),
        "w_pq": nrm(ks[14], (L, D, PEER_HEADS * PEER_QUERY), D ** -0.5),
        "sub_keys": nrm(ks[15], (L, PEER_HEADS, 2, N_KEYS, PEER_HALF), PEER_HALF ** -0.5),
        "u_tab": nrm(ks[16], (L, N_EXPERTS, D), D ** -0.5 * DEEPNORM_BETA),
        "v_tab": nrm(ks[17], (L, N_EXPERTS, D), DEEPNORM_BETA),
        "ln2_g": 1.0 + nrm(ks[18], (L, D), 0.02),
        "ln2_b": nrm(ks[19], (L, D), 0.02),
    }


def reference(x, c, w_ada, b_ada, w_in, lambda_q1, lambda_k1, lambda_q2, lambda_k2, subln_g, sinks, w_out, ln1_g, ln1_b, w_pq, sub_keys, u_tab, v_tab, ln2_g, ln2_b):
    B_, S_, _ = x.shape
    slopes = alibi_slopes()
    swa_slopes = slopes[:SWA_Q_HEADS]
    diff_slopes = slopes[SWA_Q_HEADS:]
    for l in range(DEPTH):
        lambda_init = 0.8 - 0.6 * math.exp(-0.3 * l)
        mod = jax.nn.silu(c) @ w_ada[l] + b_ada[l]
        sh1, sc1, g1, sh2, sc2, g2 = jnp.split(mod[:, None, :], 6, axis=-1)
        h = x * (1.0 + sc1) + sh1
        proj = h @ w_in[l]
        dq, dk, dv, sq, sk, sv = jnp.split(proj, SPLITS, axis=-1)
        diff_out = diff_attention(
            dq.reshape(B_, S_, DIFF_HEADS, 2, HEAD_DIM),
            dk.reshape(B_, S_, DIFF_HEADS, 2, HEAD_DIM),
            dv.reshape(B_, S_, DIFF_HEADS, DIFF_V),
            lambda_q1[l], lambda_k1[l], lambda_q2[l], lambda_k2[l], subln_g[l],
            diff_slopes, lambda_init)
        swa_out = swa_attention(
            sq.reshape(B_, S_, SWA_KV_HEADS, SWA_GROUP, HEAD_DIM),
            sk.reshape(B_, S_, SWA_KV_HEADS, HEAD_DIM),
            sv.reshape(B_, S_, SWA_KV_HEADS, HEAD_DIM),
            sinks[l], swa_slopes)
        mixed = jnp.concatenate([diff_out, swa_out], axis=-1) @ w_out[l]
        x = layer_norm(DEEPNORM_ALPHA * x + g1 * mixed, ln1_g[l], ln1_b[l])
        h = x * (1.0 + sc2) + sh2
        ffn = peer(h, w_pq[l], sub_keys[l], u_tab[l], v_tab[l])
        x = layer_norm(DEEPNORM_ALPHA * x + g2 * ffn, ln2_g[l], ln2_b[l])
    return x
```

```python
import math
from contextlib import ExitStack

import numpy as np
import ml_dtypes

import concourse.bass as bass
import concourse.mybir as mybir
from concourse.bass_utils import run_bass_kernel_spmd

F32 = mybir.dt.float32
BF16 = mybir.dt.bfloat16
I32 = mybir.dt.int32
U32 = mybir.dt.uint32
AF = mybir.ActivationFunctionType
ALU = mybir.AluOpType
AX = mybir.AxisListType

NCORES = 8
D = 1024
KC = D // 128
INW = 2304
NEXP = 16384
EPS = 1e-5
ALPHA = 2.0 ** 0.25
LAMBDA_INIT = 0.8 - 0.6 * math.exp(0.0)
NSLOT = 12


class Buf:
    __slots__ = ("name", "writer", "readers", "dsem", "dcnt")

    def __init__(self, name):
        self.name = name
        self.writer = None
        self.readers = []
        self.dsem = None
        self.dcnt = 0


class Emitter:
    ROT = 30000

    def __init__(self, nc, stack):
        self.nc = nc
        self.stack = stack
        self.eng = {"pe": nc.tensor, "act": nc.scalar, "dve": nc.vector,
                    "pool": nc.gpsimd, "sp": nc.sync}
        self.sem = {}
        self.cnt = {}
        self.own = {e: set() for e in self.eng}
        self.seen = {e: {} for e in self.eng}
        self.nsem = 0
        self.tags = []
        self.ninstr = {e: 0 for e in self.eng}
        for e in self.eng:
            self._newsem(e)

    def _alloc_sem(self, name):
        self.nsem += 1
        return self.stack.enter_context(self.nc.semaphore(name))

    def _newsem(self, e):
        self.sem[e] = self._alloc_sem(f"s_{e}_{self.nsem}")
        self.own[e].add(id(self.sem[e]))
        self.cnt[e] = 0

    def _wait(self, e, ev):
        sem, val = ev
        key = id(sem)
        if e == "pe" and key in self.own["pe"]:
            return
        if self.seen[e].get(key, 0) >= val:
            return
        self.seen[e][key] = val
        self.eng[e].wait_ge(sem, val)

    def _deps(self, e, reads, writes):
        for b in reads:
            if b.writer is not None:
                self._wait(e, b.writer)
        for b in writes:
            if b.writer is not None:
                self._wait(e, b.writer)
            for r in b.readers:
                self._wait(e, r)

    def _commit(self, ev, reads, writes):
        for b in reads:
            b.readers.append(ev)
            if len(b.readers) > 48:
                last = {}
                for s, v in b.readers:
                    k = id(s)
                    if k not in last or last[k][1] < v:
                        last[k] = (s, v)
                b.readers = list(last.values())
        for b in writes:
            b.writer = ev
            b.readers = []

    def op(self, e, fn, reads=(), writes=()):
        self._deps(e, reads, writes)
        if self.cnt[e] >= self.ROT:
            self._newsem(e)
        ins = fn(self.eng[e])
        self.cnt[e] += 1
        self.ninstr[e] += 1
        ins.then_inc(self.sem[e], 1)
        ev = (self.sem[e], self.cnt[e])
        self._commit(ev, reads, writes)
        return ev

    def dma(self, e, fn, tag, reads=(), writes=()):
        self._deps(e, reads, writes)
        if tag.dsem is None:
            tag.dsem = self._alloc_sem(f"d_{tag.name}")
            self.tags.append(tag)
        ins = fn(self.eng[e])
        tag.dcnt += 16
        self.ninstr[e] += 1
        ins.then_inc(tag.dsem, 16)
        ev = (tag.dsem, tag.dcnt)
        self._commit(ev, reads, writes)
        return ev

    def barrier(self):
        evs = [(self.sem[e2], self.cnt[e2]) for e2 in self.eng if self.cnt[e2] > 0]
        evs += [(t.dsem, t.dcnt) for t in self.tags if t.dcnt > 0]
        for e in self.eng:
            for sem, val in evs:
                key = id(sem)
                if self.seen[e].get(key, 0) >= val:
                    continue
                self.seen[e][key] = val
                self.eng[e].wait_ge(sem, val)


def _slopes():
    i = np.arange(1, 13, dtype=np.float32)
    return np.exp2(-8.0 * i / 12.0).astype(np.float32)


def build(NB, S):
    NT = S // 128
    NTOK = NB * S
    nc = bass.Bass("TRN2", target_bir_lowering=False)

    def din(name, shape, dt=F32):
        return nc.dram_tensor(name, list(shape), dt, kind="ExternalInput").ap()

    x_tok = din("x_tok", [NTOK, D])
    xT_d = din("xT", [NB, 128, KC, S])
    cT_d = din("cT", [128, KC, NB])
    wada_d = din("w_ada", [128, KC, 6 * D])
    bcol_d = din("b_ada_col", [128, 48])
    brow_d = din("b_ada_row", [1, 6 * D])
    win_d = din("w_in", [128, KC, INW])
    lam_d = din("lam_in", [128, 256])
    subg_d = din("subln_g", [128, 128])
    sinks_d = din("sinks", [128, 8])
    wout_d = din("w_out", [128, KC, D])
    lnr_d = din("ln_rows", [128, 4, D])
    wpq_d = din("w_pq", [128, KC, 2048])
    skT_d = din("skT", [128, 16, 128])
    utab_d = din("u_tab", [NEXP, D])
    vtab_d = din("v_tab", [NEXP, D])
    identb_d = din("identb", [128, 128], BF16)
    identf_d = din("identf", [128, 128])
    maskneg_d = din("maskneg", [128, 128], BF16)
    wtab_d = din("wtab", [128, NT, 4])
    swab_d = din("swab", [128, 8, 256])
    out_d = nc.dram_tensor("out", [NTOK, D], F32, kind="ExternalOutput").ap()
    x1s_d = nc.dram_tensor("x1s", [NTOK, D], F32, kind="Internal").ap()
    h2s_d = nc.dram_tensor("h2s", [NTOK, D], F32, kind="Internal").ap()
    rows_d = nc.dram_tensor("rows_s", [NB, 4, 128, D], F32, kind="Internal").ap()

    with ExitStack() as top:
        def SB(st, name, shape, dt=F32):
            return st.enter_context(nc.sbuf_tensor("s_" + name, list(shape), dt)), Buf(name)

        banks = []
        for i in range(8):
            t = top.enter_context(nc.psum_tensor(f"pb{i}", [128, 512], F32))
            banks.append((t, Buf(f"pb{i}")))
        colmod, b_colmod = SB(top, "colmod", [128, 16, NB])
        cst, b_cst = SB(top, "cst", [128, 8])
        top.enter_context(nc.Block())
        em = Emitter(nc, top)
        ctag = Buf("ctag")

        def load_consts(items):
            ev = None
            for (s_ap, d_ap, b) in items:
                ev = em.dma("sp", lambda e, s_ap=s_ap, d_ap=d_ap: e.dma_start(out=s_ap, in_=d_ap), ctag, writes=[b])
            for (_, _, b) in items:
                b.writer = ev

        em.op("dve", lambda e: e.memset(cst[:], 0.0), writes=[b_cst])
        em.op("dve", lambda e: e.memset(cst[:, 1:2], EPS), writes=[b_cst])

        with ExitStack() as p0:
            cT, b_cT = SB(p0, "cT", [128, KC, NB])
            siluT, b_siluT = SB(p0, "siluT", [128, KC, NB])
            bcol, b_bcol = SB(p0, "bcol", [128, 48])
            lam, b_lam = SB(p0, "lam", [128, 256])
            lj, b_lj = SB(p0, "lj", [128, 64])
            ls, b_ls = SB(p0, "ls", [128, 4])
            ones, b_ones = SB(p0, "ones", [128, 128])
            sbc, b_sbc = SB(p0, "sbc", [128, NB, KC, 128])
            wst = [SB(p0, f"wst{i}", [128, KC, 512]) for i in range(2)]
            brs = [SB(p0, f"brs{i}", [1, 512]) for i in range(2)]
            rst = [SB(p0, f"rst{i}", [128, 512]) for i in range(2)]
            load_consts([(cT[:], cT_d, b_cT), (bcol[:], bcol_d, b_bcol), (lam[:], lam_d, b_lam)])
            em.op("dve", lambda e: e.memset(ones[:], 1.0), writes=[b_ones])
            em.op("act", lambda e: e.activation(out=siluT[:], in_=cT[:], func=AF.Silu), reads=[b_cT], writes=[b_siluT])
            for t in range(2):
                em.op("dve", lambda e, t=t: e.scalar_tensor_tensor(
                    out=lj[:], in0=lam[:, t * 128:t * 128 + 64], scalar=1.0, in1=lam[:, t * 128 + 64:t * 128 + 128],
                    op0=ALU.mult, op1=ALU.mult, accum_out=ls[:, t:t + 1]), reads=[b_lam], writes=[b_lj, b_ls])
            em.op("act", lambda e: e.activation(out=ls[:, 2:4], in_=ls[:, 0:2], func=AF.Exp), reads=[b_ls], writes=[b_ls])
            em.op("dve", lambda e: e.scalar_tensor_tensor(out=cst[:, 0:1], in0=ls[:, 3:4], scalar=-LAMBDA_INIT, in1=ls[:, 2:3],
                                                           op0=ALU.add, op1=ALU.subtract), reads=[b_ls], writes=[b_cst])
            for b in range(NB):
                for kc in range(KC):
                    em.op("dve", lambda e, b=b, kc=kc: e.tensor_scalar(
                        out=sbc[:, b, kc, :], in0=ones[:], scalar1=siluT[:, kc, b:b + 1], scalar2=None, op0=ALU.mult),
                        reads=[b_ones, b_siluT], writes=[b_sbc])
            rr = 0
            for piece in range(12):
                (w, b_w) = wst[piece % 2]
                em.dma("sp", lambda e, w=w, piece=piece: e.dma_start(out=w[:], in_=wada_d[:, :, piece * 512:(piece + 1) * 512]),
                       b_w, writes=[b_w])
                if piece < 4:
                    for jb4 in range(4):
                        jb = piece * 4 + jb4
                        pt, b_pt = banks[rr % 8]; rr += 1
                        for kc in range(KC):
                            em.op("pe", lambda e, pt=pt, w=w, kc=kc, jb4=jb4: e.matmul(
                                pt[:, 0:NB], lhsT=w[:, kc, jb4 * 128:(jb4 + 1) * 128], rhs=siluT[:, kc, :],
                                start=(kc == 0), stop=(kc == KC - 1)), reads=[b_w, b_siluT], writes=[b_pt])
                        em.op("dve", lambda e, pt=pt, jb=jb: e.tensor_scalar(
                            out=colmod[:, jb, :], in0=pt[:, 0:NB], scalar1=bcol[:, jb:jb + 1],
                            scalar2=(1.0 if jb >= 8 else 0.0), op0=ALU.add, op1=ALU.add),
                            reads=[b_pt, b_bcol], writes=[b_colmod])
                else:
                    prm = (piece - 4) // 2
                    half = (piece - 4) % 2
                    (br, b_br) = brs[piece % 2]
                    em.dma("sp", lambda e, br=br, piece=piece: e.dma_start(out=br[:], in_=brow_d[:, piece * 512:(piece + 1) * 512]),
                           b_br, writes=[b_br])
                    for b in range(NB):
                        pt, b_pt = banks[rr % 8]; rr += 1
                        for kc in range(KC):
                            em.op("pe", lambda e, pt=pt, w=w, kc=kc, b=b: e.matmul(
                                pt[:, :], lhsT=sbc[:, b, kc, :], rhs=w[:, kc, :], start=(kc == 0), stop=False),
                                reads=[b_w, b_sbc], writes=[b_pt])
                        em.op("pe", lambda e, pt=pt, br=br: e.matmul(pt[:, :], lhsT=ones[0:1, :], rhs=br[0:1, :], start=False, stop=True),
                              reads=[b_br, b_ones], writes=[b_pt])
                        (r, b_r) = rst[(piece * NB + b) % 2]
                        em.op("act", lambda e, r=r, pt=pt, prm=prm: e.activation(
                            out=r[:], in_=pt[:, :], func=AF.Identity, bias=(cst[:, 2:3] if prm != 2 else ones[:, 0:1]), scale=1.0),
                            reads=[b_pt, b_cst, b_ones], writes=[b_r])
                        em.dma("sp", lambda e, r=r, b=b, prm=prm, half=half: e.dma_start(
                            out=rows_d[b, prm, :, half * 512:(half + 1) * 512], in_=r[:]), b_r, reads=[b_r])
            em.barrier()

        with ExitStack() as pa:
            win, b_win = SB(pa, "win", [128, KC, INW], BF16)
            wout, b_wout = SB(pa, "wout", [128, KC, D], BF16)
            identb, b_identb = SB(pa, "identb", [128, 128], BF16)
            maskneg, b_maskneg = SB(pa, "maskneg", [128, 128], BF16)
            wtab, b_wtab = SB(pa, "wtab", [128, NT, 4])
            swab, b_swab = SB(pa, "swab", [128, 8, 256])
            ln1, b_ln1 = SB(pa, "ln1", [128, 2, D])
            subg, b_subg = SB(pa, "subg", [128, 128])
            sinks, b_sinks = SB(pa, "sinks", [128, 8])
            rows, b_rows = SB(pa, "rows", [128, 3, D])
            dkT, _ = SB(pa, "dkT", [128, 4, S], BF16)
            dV, _ = SB(pa, "dV", [128, NT, 4, 130], BF16)
            skT, _ = SB(pa, "skTa", [64, 2, S], BF16)
            sv, _ = SB(pa, "sv", [128, NT, 128], BF16)
            b_kv = [Buf(f"kv{i}") for i in range(NT)]
            xTt = [SB(pa, f"xTt{i}", [128, KC, 128]) for i in range(2)]
            xt = [SB(pa, f"xt{i}", [128, D]) for i in range(2)]
            hT, b_hT = SB(pa, "hT", [128, KC, 128], BF16)
            dqT, b_dqT = SB(pa, "dqT", [128, 4, 128], BF16)
            sqT, b_sqT = SB(pa, "sqT", [64, 8, 128], BF16)
            PT = [SB(pa, f"PT{i}", [128, 512], BF16) for i in range(3)]
            od = [SB(pa, f"od{i}", [128, 128]) for i in range(2)]
            oj, b_oj = SB(pa, "oj", [128, 128], BF16)
            on, b_on = SB(pa, "on", [128, D], BF16)
            oT, b_oT = SB(pa, "oT", [128, KC, 128], BF16)
            ssb = [SB(pa, f"ssb{i}", [128, 256]) for i in range(2)]
            Psw = [SB(pa, f"Psw{i}", [128, 256], BF16) for i in range(2)]
            PTs = [SB(pa, f"PTs{i}", [128, 256], BF16) for i in range(2)]
            sm = [SB(pa, f"sm{i}", [128, 8]) for i in range(4)]
            tA, b_tA = SB(pa, "tA", [128, D])
            tB, b_tB = SB(pa, "tB", [128, D])
            x1o = [SB(pa, f"x1o{i}", [128, D]) for i in range(2)]
            h2o = [SB(pa, f"h2o{i}", [128, D]) for i in range(2)]
            st6, b_st6 = SB(pa, "st6", [128, 2, 6])
            wstg = [SB(pa, f"wstg{i}", [128, KC, 256]) for i in range(2)]

            load_consts([(identb[:], identb_d, b_identb), (maskneg[:], maskneg_d, b_maskneg), (wtab[:], wtab_d, b_wtab),
                         (swab[:], swab_d, b_swab), (ln1[:], lnr_d[:, 0:2, :], b_ln1), (subg[:], subg_d, b_subg),
                         (sinks[:], sinks_d, b_sinks)])
            em.op("dve", lambda e: e.tensor_scalar(out=subg[:], in0=subg[:], scalar1=(1.0 - LAMBDA_INIT), scalar2=None, op0=ALU.mult),
                  reads=[b_subg], writes=[b_subg])
            for pc in range(INW // 256 + D // 256):
                (wg, b_wg) = wstg[pc % 2]
                if pc < INW // 256:
                    src = win_d[:, :, pc * 256:(pc + 1) * 256]; dst = win[:, :, pc * 256:(pc + 1) * 256]; bd = b_win
                else:
                    q = pc - INW // 256
                    src = wout_d[:, :, q * 256:(q + 1) * 256]; dst = wout[:, :, q * 256:(q + 1) * 256]; bd = b_wout
                em.dma("sp", lambda e, wg=wg, src=src: e.dma_start(out=wg[:], in_=src), b_wg, writes=[b_wg])
                em.op("dve" if pc % 2 == 0 else "pool", lambda e, wg=wg, dst=dst: e.tensor_copy(out=dst, in_=wg[:]),
                      reads=[b_wg], writes=[bd])

            ACC = [banks[0], banks[1]]
            MIX = [banks[2], banks[3]]
            gen = [banks[4], banks[5], banks[6], banks[7]]
            gi = [0]

            def gbank():
                r = gen[gi[0] % 4]; gi[0] += 1
                return r

            def load_tile(g):
                b, i = divmod(g, NT)
                (xa, b_xa) = xTt[g % 2]
                (xb, b_xb) = xt[g % 2]
                em.dma("sp", lambda e: e.dma_start(out=xa[:], in_=xT_d[b, :, :, i * 128:(i + 1) * 128]), b_xa, writes=[b_xa])
                em.dma("sp", lambda e: e.dma_start(out=xb[:], in_=x_tok[g * 128:(g + 1) * 128, :]), b_xb, writes=[b_xb])

            cnt3 = [0, 0, 0]
            load_tile(0)
            for g in range(NB * NT):
                b, i = divmod(g, NT)
                if i == 0:
                    em.dma("sp", lambda e: e.dma_start(out=rows[:], in_=rows_d[b, 0:3, :, :].rearrange("r p d -> p r d")),
                           b_rows, writes=[b_rows])
                if g + 1 < NB * NT:
                    load_tile(g + 1)
                (xa, b_xa) = xTt[g % 2]
                (xb, b_xb) = xt[g % 2]
                bkv = b_kv[i]
                for kc in range(KC):
                    em.op("act", lambda e, kc=kc: e.activation(out=hT[:, kc, :], in_=xa[:, kc, :], func=AF.Identity,
                                                                bias=colmod[:, kc, b:b + 1], scale=colmod[:, 8 + kc, b:b + 1]),
                          reads=[b_xa, b_colmod], writes=[b_hT])
                pq, b_pq = gbank()
                for h in range(4):
                    for kc in range(KC):
                        em.op("pe", lambda e, h=h, kc=kc: e.matmul(pq[:, h * 128:(h + 1) * 128], lhsT=win[:, kc, h * 128:(h + 1) * 128],
                                                                    rhs=hT[:, kc, :], start=(kc == 0), stop=(kc == KC - 1)),
                              reads=[b_win, b_hT], writes=[b_pq])
                em.op("dve", lambda e: e.tensor_copy(out=dqT[:].rearrange("p h t -> p (h t)"), in_=pq[:, :]), reads=[b_pq], writes=[b_dqT])
                pk, b_pk = gbank()
                for h in range(4):
                    for kc in range(KC):
                        em.op("pe", lambda e, h=h, kc=kc: e.matmul(pk[:, h * 128:(h + 1) * 128], lhsT=win[:, kc, 512 + h * 128:512 + (h + 1) * 128],
                                                                    rhs=hT[:, kc, :], start=(kc == 0), stop=(kc == KC - 1)),
                              reads=[b_win, b_hT], writes=[b_pk])
                em.op("act", lambda e: e.activation(out=dkT[:, :, i * 128:(i + 1) * 128], in_=pk[:, :].rearrange("p (h t) -> p h t", h=4), func=AF.Copy),
                      reads=[b_pk], writes=[bkv])
                for half in range(2):
                    psq, b_psq = gbank()
                    for hh in range(4):
                        hq = half * 4 + hh
                        for kc in range(KC):
                            em.op("pe", lambda e, hq=hq, hh=hh, kc=kc: e.matmul(
                                psq[0:64, hh * 128:(hh + 1) * 128], lhsT=win[:, kc, 1536 + hq * 64:1536 + (hq + 1) * 64],
                                rhs=hT[:, kc, :], start=(kc == 0), stop=(kc == KC - 1)), reads=[b_win, b_hT], writes=[b_psq])
                    em.op("dve", lambda e, half=half: e.tensor_copy(out=sqT[:, half * 4:(half + 1) * 4, :].rearrange("p h t -> p (h t)"), in_=psq[0:64, :]),
                          reads=[b_psq], writes=[b_sqT])
                psk, b_psk = gbank()
                for gk in range(2):
                    for kc in range(KC):
                        em.op("pe", lambda e, gk=gk, kc=kc: e.matmul(
                            psk[0:64, gk * 128:(gk + 1) * 128], lhsT=win[:, kc, 2048 + gk * 64:2048 + (gk + 1) * 64],
                            rhs=hT[:, kc, :], start=(kc == 0), stop=(kc == KC - 1)), reads=[b_win, b_hT], writes=[b_psk])
                for kc in range(KC):
                    em.op("pe", lambda e, kc=kc: e.matmul(psk[:, 256:384], lhsT=hT[:, kc, :], rhs=win[:, kc, 2176:2304],
                                                           start=(kc == 0), stop=(kc == KC - 1), skip_group_check=True),
                          reads=[b_win, b_hT], writes=[b_psk])
                em.op("act", lambda e: e.activation(out=skT[:, :, i * 128:(i + 1) * 128], in_=psk[0:64, 0:256].rearrange("p (h t) -> p h t", h=2), func=AF.Copy),
                      reads=[b_psk], writes=[bkv])
                em.op("act", lambda e: e.activation(out=sv[:, i, :], in_=psk[:, 256:384], func=AF.Copy), reads=[b_psk], writes=[bkv])
                pv, b_pv = gbank()
                for kc in range(KC):
                    em.op("pe", lambda e, kc=kc: e.matmul(pv[:, :], lhsT=hT[:, kc, :], rhs=win[:, kc, 1024:1536],
                                                           start=(kc == 0), stop=(kc == KC - 1)), reads=[b_win, b_hT], writes=[b_pv])
                for h in range(4):
                    em.op("act" if h % 2 == 0 else "dve",
                          (lambda e, h=h: e.activation(out=dV[:, i, h, 0:128], in_=pv[:, h * 128:(h + 1) * 128], func=AF.Identity, bias=cst[:, 2:3], scale=wtab[:, i, h:h + 1]))
                          if h % 2 == 0 else
                          (lambda e, h=h: e.tensor_scalar(out=dV[:, i, h, 0:128], in0=pv[:, h * 128:(h + 1) * 128], scalar1=wtab[:, i, h:h + 1], scalar2=None, op0=ALU.mult)),
                          reads=[b_pv, b_wtab], writes=[bkv])
                em.op("dve", lambda e: e.tensor_copy(out=dV[:, i, :, 128], in_=wtab[:, i, :]), reads=[b_wtab], writes=[bkv])

                for h in range(4):
                    for m in range(2):
                        acc, b_acc = ACC[m]
                        for g0 in range(0, i + 1, 4):
                            kbs = list(range(g0, min(g0 + 4, i + 1)))
                            sp_, b_sp = gbank()
                            for s_, kb in enumerate(kbs):
                                em.op("pe", lambda e, s_=s_, kb=kb: e.matmul(
                                    sp_[:, s_ * 128:(s_ + 1) * 128], lhsT=dkT[64 * m:64 * m + 64, h, kb * 128:(kb + 1) * 128],
                                    rhs=dqT[64 * m:64 * m + 64, h, :], start=True, stop=(kb != i)),
                                    reads=[b_kv[kb], b_dqT], writes=[b_sp])
                                if kb == i:
                                    em.op("pe", lambda e, s_=s_: e.matmul(sp_[:, s_ * 128:(s_ + 1) * 128], lhsT=identb[:, :], rhs=maskneg[:, :],
                                                                          start=False, stop=True),
                                          reads=[b_identb, b_maskneg], writes=[b_sp])
                            n = len(kbs) * 128
                            (pt_, b_pt_) = PT[cnt3[0] % 3]; cnt3[0] += 1
                            em.op("act", lambda e, n=n, pt_=pt_: e.activation(out=pt_[:, 0:n], in_=sp_[:, 0:n], func=AF.Exp, scale=0.125),
                                  reads=[b_sp], writes=[b_pt_])
                            for s_, kb in enumerate(kbs):
                                em.op("pe", lambda e, s_=s_, kb=kb, pt_=pt_: e.matmul(
                                    acc[:, 0:129], lhsT=pt_[:, s_ * 128:(s_ + 1) * 128], rhs=dV[:, kb, h, 0:129],
                                    start=(kb == 0), stop=(kb == i)), reads=[b_pt_, b_kv[kb]], writes=[b_acc])
                    (s4, b_s4) = sm[cnt3[1] % 4]; cnt3[1] += 1
                    (o1, b_o1) = od[0]
                    (o2, b_o2) = od[1]
                    a0, b_a0 = ACC[0]
                    a1, b_a1 = ACC[1]
                    em.op("dve", lambda e: e.reciprocal(out=s4[:, 0:1], in_=a0[:, 128:129]), reads=[b_a0], writes=[b_s4])
                    em.op("dve", lambda e: e.reciprocal(out=s4[:, 1:2], in_=a1[:, 128:129]), reads=[b_a1], writes=[b_s4])
                    em.op("dve", lambda e: e.tensor_tensor(out=s4[:, 2:3], in0=s4[:, 1:2], in1=cst[:, 0:1], op=ALU.mult), reads=[b_s4, b_cst], writes=[b_s4])
                    em.op("act", lambda e: e.activation(out=o1[:], in_=a0[:, 0:128], func=AF.Identity, bias=cst[:, 2:3], scale=s4[:, 0:1]), reads=[b_a0, b_s4], writes=[b_o1])
                    em.op("dve", lambda e: e.scalar_tensor_tensor(out=o2[:], in0=a1[:, 0:128], scalar=s4[:, 2:3], in1=o1[:], op0=ALU.mult, op1=ALU.add),
                          reads=[b_a1, b_s4, b_o1], writes=[b_o2])
                    em.op("dve", lambda e: e.scalar_tensor_tensor(out=oj[:], in0=o2[:], scalar=1.0, in1=o2[:], op0=ALU.mult, op1=ALU.mult, accum_out=s4[:, 3:4]),
                          reads=[b_o2], writes=[b_oj, b_s4])
                    em.op("act", lambda e: e.activation(out=s4[:, 4:5], in_=s4[:, 3:4], func=AF.Ln, bias=cst[:, 1:2], scale=1.0 / 128.0), reads=[b_s4, b_cst], writes=[b_s4])
                    em.op("act", lambda e: e.activation(out=s4[:, 5:6], in_=s4[:, 4:5], func=AF.Exp, scale=-0.5), reads=[b_s4], writes=[b_s4])
                    em.op("dve", lambda e, h=h: e.scalar_tensor_tensor(out=on[:, h * 128:(h + 1) * 128], in0=o2[:], scalar=s4[:, 5:6], in1=subg[:], op0=ALU.mult, op1=ALU.mult),
                          reads=[b_o2, b_s4, b_subg], writes=[b_on])

                for hq in range(8):
                    gk = hq // 4
                    nk = 256 if i > 0 else 128
                    k0 = (i - 1) * 128 if i > 0 else 0
                    kvdeps = [b_kv[i]] + ([b_kv[i - 1]] if i > 0 else [])
                    sp_, b_sp = gbank()
                    em.op("pe", lambda e, hq=hq, gk=gk, nk=nk, k0=k0: e.matmul(sp_[:, 0:nk], lhsT=sqT[:, hq, :], rhs=skT[:, gk, k0:k0 + nk], start=True, stop=True),
                          reads=[b_sqT] + kvdeps, writes=[b_sp])
                    (sb_, b_sb) = ssb[hq % 2]
                    (s4, b_s4) = sm[cnt3[1] % 4]; cnt3[1] += 1
                    em.op("dve", lambda e, hq=hq, nk=nk: e.scalar_tensor_tensor(out=sb_[:, 0:nk], in0=sp_[:, 0:nk], scalar=0.125, in1=swab[:, hq, 256 - nk:256],
                                                                              op0=ALU.mult, op1=ALU.add), reads=[b_sp, b_swab], writes=[b_sb])
                    em.op("dve", lambda e, nk=nk: e.tensor_reduce(out=s4[:, 0:1], in_=sb_[:, 0:nk], axis=AX.X, op=ALU.max), reads=[b_sb], writes=[b_s4])
                    em.op("dve", lambda e, hq=hq: e.tensor_scalar(out=s4[:, 1:2], in0=s4[:, 0:1], scalar1=sinks[:, hq:hq + 1], scalar2=-1.0, op0=ALU.max, op1=ALU.mult),
                          reads=[b_s4, b_sinks], writes=[b_s4])
                    (pw, b_pw) = Psw[hq % 2]
                    em.op("act", lambda e, nk=nk: e.activation(out=pw[:, 0:nk], in_=sb_[:, 0:nk], func=AF.Exp, bias=s4[:, 1:2], scale=1.0, accum_out=s4[:, 2:3]),
                          reads=[b_sb, b_s4], writes=[b_pw, b_s4])
                    em.op("act", lambda e, hq=hq: e.activation(out=s4[:, 3:4], in_=s4[:, 1:2], func=AF.Exp, bias=sinks[:, hq:hq + 1], scale=1.0),
                          reads=[b_s4, b_sinks], writes=[b_s4])
                    em.op("dve", lambda e: e.tensor_tensor(out=s4[:, 4:5], in0=s4[:, 2:3], in1=s4[:, 3:4], op=ALU.add), reads=[b_s4], writes=[b_s4])
                    em.op("dve", lambda e: e.reciprocal(out=s4[:, 5:6], in_=s4[:, 4:5]), reads=[b_s4], writes=[b_s4])
                    tp, b_tp = gbank()
                    tpv = tp[:, 0:128].bitcast(BF16)
                    for bl in range(nk // 128):
                        em.op("pe", lambda e, bl=bl: e.transpose(out=tpv[:, bl * 128:(bl + 1) * 128], in_=pw[:, bl * 128:(bl + 1) * 128], identity=identb[:]),
                              reads=[b_pw, b_identb], writes=[b_tp])
                    (pts, b_pts) = PTs[hq % 2]
                    em.op("act", lambda e, nk=nk: e.activation(out=pts[:, 0:nk], in_=tpv[:, 0:nk], func=AF.Copy), reads=[b_tp], writes=[b_pts])
                    for bl in range(nk // 128):
                        kt = (i - 1 + bl) if i > 0 else i
                        em.op("pe", lambda e, bl=bl, kt=kt, gk=gk, nk=nk: e.matmul(tp[:, 256:320], lhsT=pts[:, bl * 128:(bl + 1) * 128], rhs=sv[:, kt, gk * 64:(gk + 1) * 64],
                                                                                   start=(bl == 0), stop=(bl == nk // 128 - 1), skip_group_check=True),
                              reads=[b_pts] + kvdeps, writes=[b_tp])
                    em.op("act", lambda e, hq=hq: e.activation(out=on[:, 512 + hq * 64:512 + (hq + 1) * 64], in_=tp[:, 256:320], func=AF.Identity, bias=cst[:, 2:3], scale=s4[:, 5:6]),
                          reads=[b_tp, b_s4], writes=[b_on])

                tp, b_tp = gbank()
                tpv = tp[:, :].bitcast(BF16)
                for c in range(KC):
                    em.op("pe", lambda e, c=c: e.transpose(out=tpv[:, c * 128:(c + 1) * 128], in_=on[:, c * 128:(c + 1) * 128], identity=identb[:]),
                          reads=[b_on, b_identb], writes=[b_tp])
                em.op("dve", lambda e: e.tensor_copy(out=oT[:].rearrange("p c t -> p (c t)"), in_=tpv[:, :]), reads=[b_tp], writes=[b_oT])
                for half in range(2):
                    mx, b_mx = MIX[half]
                    for c in range(KC):
                        em.op("pe", lambda e, c=c, half=half, mx=mx: e.matmul(mx[:, :], lhsT=oT[:, c, :], rhs=wout[:, c, half * 512:(half + 1) * 512],
                                                                             start=(c == 0), stop=(c == KC - 1)), reads=[b_oT, b_wout], writes=[b_mx])
                for half in range(2):
                    mx, b_mx = MIX[half]
                    em.op("dve", lambda e, half=half, mx=mx: e.tensor_tensor(out=tA[:, half * 512:(half + 1) * 512], in0=mx[:, :], in1=rows[:, 0, half * 512:(half + 1) * 512], op=ALU.mult),
                          reads=[b_mx, b_rows], writes=[b_tA])
                em.op("dve", lambda e: e.scalar_tensor_tensor(out=tB[:], in0=xb[:], scalar=ALPHA, in1=tA[:], op0=ALU.mult, op1=ALU.add),
                      reads=[b_xb, b_tA], writes=[b_tB])
                (s4, b_s4) = sm[cnt3[1] % 4]; cnt3[1] += 1
                for c in range(2):
                    em.op("dve", lambda e, c=c: e.bn_stats(out=st6[:, c, :], in_=tB[:, c * 512:(c + 1) * 512]), reads=[b_tB], writes=[b_st6])
                em.op("dve", lambda e: e.bn_aggr(out=s4[:, 0:2], in_=st6[:].rearrange("p a b -> p (a b)")), reads=[b_st6], writes=[b_s4])
                em.op("act", lambda e: e.activation(out=s4[:, 2:3], in_=s4[:, 1:2], func=AF.Ln, bias=cst[:, 1:2], scale=1.0), reads=[b_s4, b_cst], writes=[b_s4])
                em.op("act", lambda e: e.activation(out=s4[:, 3:4], in_=s4[:, 2:3], func=AF.Exp, scale=-0.5), reads=[b_s4], writes=[b_s4])
                em.op("dve", lambda e: e.tensor_scalar(out=tA[:], in0=tB[:], scalar1=s4[:, 0:1], scalar2=s4[:, 3:4], op0=ALU.subtract, op1=ALU.mult),
                      reads=[b_tB, b_s4], writes=[b_tA])
                (x1, b_x1) = x1o[g % 2]
                (h2, b_h2) = h2o[g % 2]
                em.op("pool", lambda e: e.tensor_tensor(out=tB[:], in0=tA[:], in1=ln1[:, 0, :], op=ALU.mult), reads=[b_tA, b_ln1], writes=[b_tB])
                em.op("pool", lambda e: e.tensor_tensor(out=x1[:], in0=tB[:], in1=ln1[:, 1, :], op=ALU.add), reads=[b_tB, b_ln1], writes=[b_x1])
                em.dma("sp", lambda e: e.dma_start(out=x1s_d[g * 128:(g + 1) * 128, :], in_=x1[:]), b_x1, reads=[b_x1])
                em.op("pool", lambda e: e.tensor_tensor(out=tA[:], in0=x1[:], in1=rows[:, 2, :], op=ALU.mult), reads=[b_x1, b_rows], writes=[b_tA])
                em.op("pool", lambda e: e.tensor_tensor(out=h2[:], in0=tA[:], in1=rows[:, 1, :], op=ALU.add), reads=[b_tA, b_rows], writes=[b_h2])
                em.dma("sp", lambda e: e.dma_start(out=h2s_d[g * 128:(g + 1) * 128, :], in_=h2[:]), b_h2, reads=[b_h2])
            em.barrier()

        with ExitStack() as pb:
            wpq, b_wpq = SB(pb, "wpq", [128, KC, 2048])
            skTp, b_skTp = SB(pb, "skTp", [128, 16, 128])
            identf, b_identf = SB(pb, "identf", [128, 128])
            ln2, b_ln2 = SB(pb, "ln2", [128, 2, D])
            g2r, b_g2r = SB(pb, "g2r", [128, D])
            GB = [SB(pb, f"GB{i}", [128, D]) for i in range(NSLOT)]
            x1t = [SB(pb, f"x1t{i}", [128, D]) for i in range(2)]
            h2t = [SB(pb, f"h2t{i}", [128, D]) for i in range(2)]
            h2T, b_h2T = SB(pb, "h2T", [128, KC, 128])
            qT, b_qT = SB(pb, "qT", [128, 16, 128])
            vals, b_vals = SB(pb, "vals", [128, 16, 16])
            idxs, b_idxs = SB(pb, "idxs", [128, 16, 16], U32)
            idxf, b_idxf = SB(pb, "idxf", [128, 16, 16])
            scw = [SB(pb, f"scw{i}", [128, 128]) for i in range(2)]
            cand = [SB(pb, f"cand{i}", [128, 256]) for i in range(2)]
            cidx = [SB(pb, f"cidx{i}", [128, 256]) for i in range(2)]
            cw = [SB(pb, f"cw{i}", [128, 256]) for i in range(2)]
            tops, b_tops = SB(pb, "tops", [128, 8, 16])
            gate = [SB(pb, f"gate{i}", [128, 8, 16]) for i in range(2)]
            eidf, b_eidf = SB(pb, "eidf", [128, 128])
            eid = [SB(pb, f"eid{i}", [128, 128], I32) for i in range(2)]
            eidT = [SB(pb, f"eidT{i}", [128, 128], I32) for i in range(2)]
            apre, b_apre = SB(pb, "apre", [128, 128])
            ga, b_ga = SB(pb, "ga", [128, 128])
            gaT, b_gaT = SB(pb, "gaT", [128, 128])
            junk, _ = SB(pb, "junk", [128, D], BF16)
            junk2, _ = SB(pb, "junk2", [128, 256], BF16)
            ffnT, b_ffnT = SB(pb, "ffnT", [128, KC, 128])
            wA, b_wA = SB(pb, "wA", [128, D])
            wB, b_wB = SB(pb, "wB", [128, D])
            outt = [SB(pb, f"outt{i}", [128, D]) for i in range(2)]
            st6b, b_st6b = SB(pb, "st6b", [128, 2, 6])
            smb = [SB(pb, f"smb{i}", [128, 16]) for i in range(2)]

            load_consts([(wpq[:, 0:4, :], wpq_d[:, 0:4, :], b_wpq), (wpq[:, 4:8, :], wpq_d[:, 4:8, :], b_wpq),
                         (skTp[:], skT_d, b_skTp), (identf[:], identf_d, b_identf), (ln2[:], lnr_d[:, 2:4, :], b_ln2)])
            VT = [banks[0], banks[1]]
            bi = [0]

            def nbank():
                r = banks[2 + bi[0] % 6]; bi[0] += 1
                return r

            def load_tile_b(g):
                (a, b_a) = x1t[g % 2]
                (h, b_h) = h2t[g % 2]
                em.dma("sp", lambda e: e.dma_start(out=h[:], in_=h2s_d[g * 128:(g + 1) * 128, :]), b_h, writes=[b_h])
                em.dma("sp", lambda e: e.dma_start(out=a[:], in_=x1s_d[g * 128:(g + 1) * 128, :]), b_a, writes=[b_a])

            def score(g):
                (hh, b_hh) = h2t[g % 2]
                for half in range(2):
                    tp, b_tp = nbank()
                    for c4 in range(4):
                        c = half * 4 + c4
                        em.op("pe", lambda e, c=c, c4=c4, tp=tp: e.transpose(out=tp[:, c4 * 128:(c4 + 1) * 128], in_=hh[:, c * 128:(c + 1) * 128], identity=identf[:]),
                              reads=[b_hh, b_identf], writes=[b_tp])
                    em.op("act", lambda e, tp=tp, half=half: e.activation(out=h2T[:, half * 4:(half + 1) * 4, :].rearrange("p c t -> p (c t)"), in_=tp[:, :], func=AF.Copy),
                          reads=[b_tp], writes=[b_h2T])
                for q4 in range(4):
                    pq, b_pq = nbank()
                    for c4 in range(4):
                        c16 = q4 * 4 + c4
                        for kc in range(KC):
                            em.op("pe", lambda e, c16=c16, c4=c4, kc=kc, pq=pq: e.matmul(pq[:, c4 * 128:(c4 + 1) * 128], lhsT=wpq[:, kc, c16 * 128:(c16 + 1) * 128],
                                                                                       rhs=h2T[:, kc, :], start=(kc == 0), stop=(kc == KC - 1)),
                                  reads=[b_wpq, b_h2T], writes=[b_pq])
                    em.op("act", lambda e, pq=pq, q4=q4: e.activation(out=qT[:, q4 * 4:(q4 + 1) * 4, :].rearrange("p c t -> p (c t)"), in_=pq[:, :], func=AF.Copy),
                          reads=[b_pq], writes=[b_qT])
                scb = []
                for q4 in range(4):
                    ps_, b_ps = nbank()
                    scb.append((ps_, b_ps))
                    for c4 in range(4):
                        c16 = q4 * 4 + c4
                        em.op("pe", lambda e, c16=c16, c4=c4, ps_=ps_: e.matmul(ps_[:, c4 * 128:(c4 + 1) * 128], lhsT=qT[:, c16, :], rhs=skTp[:, c16, :], start=True, stop=True),
                              reads=[b_qT, b_skTp], writes=[b_ps])
                return scb

            def topk(g, scb):
                for c16 in range(16):
                    ps_, b_ps = scb[c16 // 4]
                    src = ps_[:, (c16 % 4) * 128:(c16 % 4 + 1) * 128]
                    (sw, b_sw) = scw[c16 % 2]
                    em.op("dve", lambda e, c16=c16, src=src: e.max(out=vals[:, c16, 0:8], in_=src), reads=[b_ps], writes=[b_vals])
                    em.op("dve", lambda e, c16=c16, src=src: e.max_index(out=idxs[:, c16, 0:8], in_max=vals[:, c16, 0:8], in_values=src), reads=[b_ps, b_vals], writes=[b_idxs])
                    em.op("dve", lambda e, c16=c16, src=src, sw=sw: e.match_replace(out=sw[:], in_to_replace=vals[:, c16, 0:8], in_values=src, imm_value=-1e30),
                          reads=[b_ps, b_vals], writes=[b_sw])
                    em.op("dve", lambda e, c16=c16, sw=sw: e.max(out=vals[:, c16, 8:16], in_=sw[:]), reads=[b_sw], writes=[b_vals])
                    em.op("dve", lambda e, c16=c16, sw=sw: e.max_index(out=idxs[:, c16, 8:16], in_max=vals[:, c16, 8:16], in_values=sw[:]), reads=[b_sw, b_vals], writes=[b_idxs])
                em.op("dve", lambda e: e.tensor_copy(out=idxf[:], in_=idxs[:]), reads=[b_idxs], writes=[b_idxf])
                i4 = idxf[:].rearrange("p (h two) k -> p h two k", two=2)
                em.op("dve", lambda e: e.tensor_scalar(out=i4[:, :, 0, :], in0=i4[:, :, 0, :], scalar1=128.0, scalar2=None, op0=ALU.mult), reads=[b_idxf], writes=[b_idxf])
                for h in range(8):
                    (cd, b_cd) = cand[h % 2]
                    (ci, b_ci) = cidx[h % 2]
                    (cw_, b_cw) = cw[h % 2]
                    em.op("dve", lambda e, h=h, cd=cd: e.tensor_tensor(out=cd[:].rearrange("p (i j) -> p i j", i=16),
                                                                     in0=vals[:, 2 * h, :].unsqueeze(2).to_broadcast([128, 16, 16]),
                                                                     in1=vals[:, 2 * h + 1, :].unsqueeze(1).to_broadcast([128, 16, 16]), op=ALU.add),
                          reads=[b_vals], writes=[b_cd])
                    em.op("dve", lambda e, h=h, ci=ci: e.tensor_tensor(out=ci[:].rearrange("p (i j) -> p i j", i=16),
                                                                     in0=idxf[:, 2 * h, :].unsqueeze(2).to_broadcast([128, 16, 16]),
                                                                     in1=idxf[:, 2 * h + 1, :].unsqueeze(1).to_broadcast([128, 16, 16]), op=ALU.add),
                          reads=[b_idxf], writes=[b_ci])
                    em.op("dve", lambda e, h=h, cd=cd: e.max(out=tops[:, h, 0:8], in_=cd[:]), reads=[b_cd], writes=[b_tops])
                    em.op("dve", lambda e, h=h, cd=cd, cw_=cw_: e.match_replace(out=cw_[:], in_to_replace=tops[:, h, 0:8], in_values=cd[:], imm_value=-1e30),
                          reads=[b_cd, b_tops], writes=[b_cw])
                    em.op("dve", lambda e, h=h, cw_=cw_: e.max(out=tops[:, h, 8:16], in_=cw_[:]), reads=[b_cw], writes=[b_tops])
                    for k in range(16):
                        last = (k == 15)
                        em.op("dve", lambda e, h=h, k=k, cd=cd, ci=ci: e.scalar_tensor_tensor(
                            out=junk2[:], in0=cd[:], scalar=tops[:, h, k:k + 1], in1=ci[:], op0=ALU.is_equal, op1=ALU.mult,
                            accum_out=eidf[:, h * 16 + k:h * 16 + k + 1]),
                            reads=[b_cd, b_ci, b_tops], writes=([b_eidf] if (last and h == 7) else []))
                (ei, b_ei) = eid[g % 2]
                (eiT, b_eiT) = eidT[g % 2]
                em.op("dve", lambda e: e.tensor_scalar(out=eidf[:], in0=eidf[:], scalar1=float(NEXP - 1), scalar2=0.0, op0=ALU.min, op1=ALU.max),
                      reads=[b_eidf], writes=[b_eidf])
                em.op("dve", lambda e: e.tensor_copy(out=ei[:], in_=eidf[:]), reads=[b_eidf], writes=[b_ei])
                (gt, b_gt) = gate[g % 2]
                em.op("dve", lambda e: e.tensor_tensor(out=gt[:], in0=tops[:], in1=tops[:, :, 0:1].to_broadcast([128, 8, 16]), op=ALU.subtract),
                      reads=[b_tops], writes=[b_gt])
                em.op("act", lambda e: e.activation(out=gt[:], in_=gt[:], func=AF.Exp), reads=[b_gt], writes=[b_gt])
                (s8, b_s8) = smb[g % 2]
                em.op("dve", lambda e: e.tensor_reduce(out=s8[:, 0:8], in_=gt[:], axis=AX.X, op=ALU.add), reads=[b_gt], writes=[b_s8])
                em.op("dve", lambda e: e.reciprocal(out=s8[:, 8:16], in_=s8[:, 0:8]), reads=[b_s8], writes=[b_s8])
                em.op("dve", lambda e: e.tensor_tensor(out=gt[:], in0=gt[:], in1=s8[:, 8:16].unsqueeze(2).to_broadcast([128, 8, 16]), op=ALU.mult),
                      reads=[b_gt, b_s8], writes=[b_gt])
                tp, b_tp = nbank()
                em.op("pe", lambda e: e.transpose(out=tp[:, 0:128], in_=eidf[:], identity=identf[:]), reads=[b_eidf, b_identf], writes=[b_tp])
                em.op("act", lambda e: e.activation(out=eiT[:], in_=tp[:, 0:128], func=AF.Copy), reads=[b_tp], writes=[b_eiT])

            slot = [0]
            NG = NB * NT
            load_tile_b(0)
            scb_cur = score(0)
            topk(0, scb_cur)
            for g in range(NG):
                b, i = divmod(g, NT)
                if i == 0:
                    em.dma("sp", lambda e: e.dma_start(out=g2r[:], in_=rows_d[b, 3, :, :]), b_g2r, writes=[b_g2r])
                if g + 1 < NG:
                    load_tile_b(g + 1)
                    scb_next = score(g + 1)
                (xa, b_xa) = x1t[g % 2]
                (hh, b_hh) = h2t[g % 2]
                (ei, b_ei) = eid[g % 2]
                (eiT, b_eiT) = eidT[g % 2]
                (gt, b_gt) = gate[g % 2]
                (s8, b_s8) = smb[g % 2]
                for j in range(128):
                    (gb, b_gb) = GB[slot[0] % NSLOT]; slot[0] += 1
                    em.dma("pool", lambda e, j=j, gb=gb: e.indirect_dma_start(out=gb[:, :], out_offset=None, in_=utab_d,
                                                                           in_offset=bass.IndirectOffsetOnAxis(ap=ei[:, j:j + 1], axis=0)),
                           b_gb, reads=[b_ei], writes=[b_gb])
                    em.op("dve", lambda e, j=j, gb=gb: e.scalar_tensor_tensor(out=junk[:], in0=gb[:], scalar=1.0, in1=hh[:], op0=ALU.mult, op1=ALU.mult,
                                                                            accum_out=apre[:, j:j + 1]),
                          reads=[b_gb, b_hh], writes=([b_apre] if j == 127 else []))
                em.op("act", lambda e: e.activation(out=ga[:], in_=apre[:], func=AF.Gelu), reads=[b_apre], writes=[b_ga])
                em.op("dve", lambda e: e.tensor_tensor(out=ga[:], in0=ga[:], in1=gt[:].rearrange("p h k -> p (h k)"), op=ALU.mult),
                      reads=[b_ga, b_gt], writes=[b_ga])
                tp, b_tp = nbank()
                em.op("pe", lambda e: e.transpose(out=tp[:, 0:128], in_=ga[:], identity=identf[:]), reads=[b_ga, b_identf], writes=[b_tp])
                em.op("act", lambda e: e.activation(out=gaT[:], in_=tp[:, 0:128], func=AF.Copy), reads=[b_tp], writes=[b_gaT])
                for t in range(128):
                    (gb, b_gb) = GB[slot[0] % NSLOT]; slot[0] += 1
                    em.dma("pool", lambda e, t=t, gb=gb: e.indirect_dma_start(out=gb[:, :], out_offset=None, in_=vtab_d,
                                                                           in_offset=bass.IndirectOffsetOnAxis(ap=eiT[:, t:t + 1], axis=0)),
                           b_gb, reads=[b_eiT], writes=[b_gb])
                    for c in range(KC):
                        vt, b_vt = VT[c // 4]
                        col = (c % 4) * 128 + t
                        em.op("pe", lambda e, c=c, t=t, gb=gb, vt=vt, col=col: e.matmul(vt[:, col:col + 1], lhsT=gb[:, c * 128:(c + 1) * 128], rhs=gaT[:, t:t + 1],
                                                                                      start=True, stop=True, skip_group_check=True),
                              reads=[b_gb, b_gaT], writes=[b_vt])
                if g + 1 < NG:
                    topk(g + 1, scb_next)
                for half in range(2):
                    vt, b_vt = VT[half]
                    em.op("act", lambda e, half=half, vt=vt: e.activation(out=ffnT[:, half * 4:(half + 1) * 4, :].rearrange("p c t -> p (c t)"), in_=vt[:, :], func=AF.Copy),
                          reads=[b_vt], writes=[b_ffnT])
                fts = []
                for half in range(2):
                    ft, b_ft = nbank()
                    fts.append((ft, b_ft))
                    for c4 in range(4):
                        c = half * 4 + c4
                        em.op("pe", lambda e, c=c, c4=c4, ft=ft: e.transpose(out=ft[:, c4 * 128:(c4 + 1) * 128], in_=ffnT[:, c, :], identity=identf[:]),
                              reads=[b_ffnT, b_identf], writes=[b_ft])
                for half in range(2):
                    ft, b_ft = fts[half]
                    em.op("dve", lambda e, half=half, ft=ft: e.tensor_tensor(out=wB[:, half * 512:(half + 1) * 512], in0=ft[:, :], in1=g2r[:, half * 512:(half + 1) * 512], op=ALU.mult),
                          reads=[b_ft, b_g2r], writes=[b_wB])
                em.op("dve", lambda e: e.scalar_tensor_tensor(out=wA[:], in0=xa[:], scalar=ALPHA, in1=wB[:], op0=ALU.mult, op1=ALU.add),
                      reads=[b_xa, b_wB], writes=[b_wA])
                for c in range(2):
                    em.op("dve", lambda e, c=c: e.bn_stats(out=st6b[:, c, :], in_=wA[:, c * 512:(c + 1) * 512]), reads=[b_wA], writes=[b_st6b])
                em.op("dve", lambda e: e.bn_aggr(out=s8[:, 0:2], in_=st6b[:].rearrange("p a b -> p (a b)")), reads=[b_st6b], writes=[b_s8])
                em.op("act", lambda e: e.activation(out=s8[:, 2:3], in_=s8[:, 1:2], func=AF.Ln, bias=cst[:, 1:2], scale=1.0), reads=[b_s8, b_cst], writes=[b_s8])
                em.op("act", lambda e: e.activation(out=s8[:, 3:4], in_=s8[:, 2:3], func=AF.Exp, scale=-0.5), reads=[b_s8], writes=[b_s8])
                em.op("dve", lambda e: e.tensor_scalar(out=wB[:], in0=wA[:], scalar1=s8[:, 0:1], scalar2=s8[:, 3:4], op0=ALU.subtract, op1=ALU.mult),
                      reads=[b_wA, b_s8], writes=[b_wB])
                (ot, b_ot) = outt[g % 2]
                em.op("dve", lambda e: e.tensor_tensor(out=wA[:], in0=wB[:], in1=ln2[:, 0, :], op=ALU.mult), reads=[b_wB, b_ln2], writes=[b_wA])
                em.op("dve", lambda e: e.tensor_tensor(out=ot[:], in0=wA[:], in1=ln2[:, 1, :], op=ALU.add), reads=[b_wA, b_ln2], writes=[b_ot])
                em.dma("sp", lambda e: e.dma_start(out=out_d[g * 128:(g + 1) * 128, :], in_=ot[:]), b_ot, reads=[b_ot])
                if g + 1 < NG:
                    scb_cur = scb_next
            em.barrier()
        build.info = dict(ninstr=dict(em.ninstr), nsem=em.nsem)
    return nc


def _host_layout(inputs, NB, S):
    f32 = np.float32
    x = np.ascontiguousarray(inputs["x"], dtype=f32)
    B = x.shape[0]
    NT = S // 128
    c = np.asarray(inputs["c"], f32)

    def kcl(w):
        return np.ascontiguousarray(w.reshape(KC, 128, -1).transpose(1, 0, 2))

    def rep(v, n=128):
        return np.ascontiguousarray(np.broadcast_to(np.asarray(v, f32).reshape(1, -1), (n, np.asarray(v).size)))

    shared = {
        "w_ada": kcl(np.asarray(inputs["w_ada"][0], f32)),
        "b_ada_col": np.ascontiguousarray(np.asarray(inputs["b_ada"][0], f32).reshape(48, 128).T),
        "b_ada_row": np.ascontiguousarray(np.asarray(inputs["b_ada"][0], f32).reshape(1, -1)),
        "w_in": kcl(np.asarray(inputs["w_in"][0], f32)),
        "lam_in": np.ascontiguousarray(np.concatenate([rep(inputs["lambda_q1"][0]), rep(inputs["lambda_k1"][0]),
                                                       rep(inputs["lambda_q2"][0]), rep(inputs["lambda_k2"][0])], axis=1)),
        "subln_g": rep(inputs["subln_g"][0]),
        "sinks": rep(inputs["sinks"][0]),
        "w_out": kcl(np.asarray(inputs["w_out"][0], f32)),
        "ln_rows": np.ascontiguousarray(np.stack([rep(inputs["ln1_g"][0]), rep(inputs["ln1_b"][0]),
                                                  rep(inputs["ln2_g"][0]), rep(inputs["ln2_b"][0])], axis=1)),
        "w_pq": kcl(np.asarray(inputs["w_pq"][0], f32)),
        "skT": np.ascontiguousarray(np.asarray(inputs["sub_keys"][0], f32).reshape(16, 128, 128).transpose(2, 0, 1)),
        "u_tab": np.ascontiguousarray(np.asarray(inputs["u_tab"][0], f32)),
        "v_tab": np.ascontiguousarray(np.asarray(inputs["v_tab"][0], f32)),
    }
    sl = _slopes()
    kk = np.arange(128)
    shared["identb"] = np.eye(128, dtype=f32).astype(ml_dtypes.bfloat16)
    shared["identf"] = np.eye(128, dtype=f32)
    shared["maskneg"] = np.where(kk[:, None] > kk[None, :], -30000.0, 0.0).astype(f32).astype(ml_dtypes.bfloat16)
    kpos = (np.arange(NT)[None, :] * 128 + kk[:, None]).astype(np.float64)
    shared["wtab"] = np.exp(sl[8:12][None, None, :].astype(np.float64) * (kpos[:, :, None] - (S - 1))).astype(f32)
    qi = np.arange(128)[:, None]
    kj = np.arange(256)[None, :]
    dist = qi - kj + 128
    valid = (dist >= 0) & (dist < 128)
    swab = np.where(valid[:, None, :], -sl[:8][None, :, None] * dist[:, None, :].astype(f32), f32(-1e30)).astype(f32)
    shared["swab"] = np.ascontiguousarray(swab)

    in_maps = []
    for core in range(NCORES):
        xs = x[core * NB:(core + 1) * NB]
        m = dict(shared)
        m["x_tok"] = np.ascontiguousarray(xs.reshape(NB * S, D))
        m["xT"] = np.ascontiguousarray(xs.reshape(NB, S, KC, 128).transpose(0, 3, 2, 1))
        m["cT"] = np.ascontiguousarray(c[core * NB:(core + 1) * NB].reshape(NB, KC, 128).transpose(2, 1, 0))
        in_maps.append(m)
    return in_maps


_CACHE = {}


def kernel(**inputs):
    x = np.asarray(inputs["x"])
    B, S, _ = x.shape
    NB = B // NCORES
    key = (NB, S)
    if key not in _CACHE:
        _CACHE[key] = build(NB, S)
    nc = _CACHE[key]
    in_maps = _host_layout(inputs, NB, S)
    res = run_bass_kernel_spmd(nc, in_maps, core_ids=list(range(NCORES)))
    outs = [np.asarray(res.results[cidx]["out"], np.float32).reshape(NB, S, D) for cidx in range(NCORES)]
    return np.concatenate(outs, axis=0)
```

```python
import math
from contextlib import ExitStack

import numpy as np
import ml_dtypes

import concourse.bass as bass
import concourse.mybir as mybir
from concourse.bass_utils import run_bass_kernel_spmd

F32 = mybir.dt.float32
BF16 = mybir.dt.bfloat16
I32 = mybir.dt.int32
U32 = mybir.dt.uint32
AF = mybir.ActivationFunctionType
ALU = mybir.AluOpType
AX = mybir.AxisListType

NCORES = 8
D = 1024
KC = D // 128
INW = 2304
NEXP = 16384
EPS = 1e-5
ALPHA = 2.0 ** 0.25
LAMBDA_INIT = 0.8 - 0.6 * math.exp(0.0)
NSLOT = 24


class Buf:
    __slots__ = ("name", "writer", "readers", "dsem", "dcnt")

    def __init__(self, name):
        self.name = name
        self.writer = None
        self.readers = []
        self.dsem = None
        self.dcnt = 0


class Emitter:
    ROT = 30000

    def __init__(self, nc, stack):
        self.nc = nc
        self.stack = stack
        self.eng = {"pe": nc.tensor, "act": nc.scalar, "dve": nc.vector,
                    "pool": nc.gpsimd, "sp": nc.sync}
        self.sem = {}
        self.cnt = {}
        self.own = {e: set() for e in self.eng}
        self.seen = {e: {} for e in self.eng}
        self.nsem = 0
        self.tags = []
        self.ninstr = {e: 0 for e in self.eng}
        for e in self.eng:
            self._newsem(e)

    def _alloc_sem(self, name):
        self.nsem += 1
        return self.stack.enter_context(self.nc.semaphore(name))

    def _newsem(self, e):
        self.sem[e] = self._alloc_sem(f"s_{e}_{self.nsem}")
        self.own[e].add(id(self.sem[e]))
        self.cnt[e] = 0

    def _wait(self, e, ev):
        sem, val = ev
        key = id(sem)
        if e == "pe" and key in self.own["pe"]:
            return
        if self.seen[e].get(key, 0) >= val:
            return
        self.seen[e][key] = val
        self.eng[e].wait_ge(sem, val)

    def _deps(self, e, reads, writes):
        for b in reads:
            if b.writer is not None:
                self._wait(e, b.writer)
        for b in writes:
            if b.writer is not None:
                self._wait(e, b.writer)
            for r in b.readers:
                self._wait(e, r)

    def _commit(self, ev, reads, writes):
        for b in reads:
            b.readers.append(ev)
            if len(b.readers) > 48:
                last = {}
                for s, v in b.readers:
                    k = id(s)
                    if k not in last or last[k][1] < v:
                        last[k] = (s, v)
                b.readers = list(last.values())
        for b in writes:
            b.writer = ev
            b.readers = []

    def op(self, e, fn, reads=(), writes=()):
        self._deps(e, reads, writes)
        if self.cnt[e] >= self.ROT:
            self._newsem(e)
        ins = fn(self.eng[e])
        self.cnt[e] += 1
        self.ninstr[e] += 1
        ins.then_inc(self.sem[e], 1)
        ev = (self.sem[e], self.cnt[e])
        self._commit(ev, reads, writes)
        return ev

    def dma(self, e, fn, tag, reads=(), writes=()):
        self._deps(e, reads, writes)
        if tag.dsem is None:
            tag.dsem = self._alloc_sem(f"d_{tag.name}")
            self.tags.append(tag)
        ins = fn(self.eng[e])
        tag.dcnt += 16
        self.ninstr[e] += 1
        ins.then_inc(tag.dsem, 16)
        ev = (tag.dsem, tag.dcnt)
        self._commit(ev, reads, writes)
        return ev

    def barrier(self):
        evs = [(self.sem[e2], self.cnt[e2]) for e2 in self.eng if self.cnt[e2] > 0]
        evs += [(t.dsem, t.dcnt) for t in self.tags if t.dcnt > 0]
        for e in self.eng:
            for sem, val in evs:
                key = id(sem)
                if self.seen[e].get(key, 0) >= val:
                    continue
                self.seen[e][key] = val
                self.eng[e].wait_ge(sem, val)


def _slopes():
    i = np.arange(1, 13, dtype=np.float32)
    return np.exp2(-8.0 * i / 12.0).astype(np.float32)


def build(NB, S):
    NT = S // 128
    NTOK = NB * S
    nc = bass.Bass("TRN2", target_bir_lowering=False)

    def din(name, shape, dt=F32):
        return nc.dram_tensor(name, list(shape), dt, kind="ExternalInput").ap()

    x_tok = din("x_tok", [NTOK, D])
    xT_d = din("xT", [NB, 128, KC, S])
    cT_d = din("cT", [128, KC, NB])
    wada_d = din("w_ada", [128, KC, 6 * D])
    bcol_d = din("b_ada_col", [128, 48])
    brow_d = din("b_ada_row", [1, 6 * D])
    win_d = din("w_in", [128, KC, INW])
    lam_d = din("lam_in", [128, 256])
    subg_d = din("subln_g", [128, 128])
    sinks_d = din("sinks", [128, 8])
    wout_d = din("w_out", [128, KC, D])
    lnr_d = din("ln_rows", [128, 4, D])
    wpq_d = din("w_pq", [128, KC, 2048])
    skT_d = din("skT", [128, 16, 128])
    utab_d = din("u_tab", [NEXP, D])
    vtab_d = din("v_tab", [NEXP, D])
    identb_d = din("identb", [128, 128], BF16)
    identf_d = din("identf", [128, 128])
    maskneg_d = din("maskneg", [128, 128], BF16)
    wtab_d = din("wtab", [128, NT, 4])
    swab_d = din("swab", [128, 8, 256])
    out_d = nc.dram_tensor("out", [NTOK, D], F32, kind="ExternalOutput").ap()
    x1s_d = nc.dram_tensor("x1s", [NTOK, D], F32, kind="Internal").ap()
    h2s_d = nc.dram_tensor("h2s", [NTOK, D], F32, kind="Internal").ap()
    rows_d = nc.dram_tensor("rows_s", [NB, 4, 128, D], F32, kind="Internal").ap()
    ubf_d = nc.dram_tensor("u_bf", [NEXP, D], BF16, kind="Internal").ap()
    vbf_d = nc.dram_tensor("v_bf", [NEXP, D], BF16, kind="Internal").ap()

    with ExitStack() as top:
        def SB(st, name, shape, dt=F32):
            return st.enter_context(nc.sbuf_tensor("s_" + name, list(shape), dt)), Buf(name)

        banks = []
        for i in range(8):
            t = top.enter_context(nc.psum_tensor(f"pb{i}", [128, 512], F32))
            banks.append((t, Buf(f"pb{i}")))
        colmod, b_colmod = SB(top, "colmod", [128, 16, NB])
        cst, b_cst = SB(top, "cst", [128, 8])
        top.enter_context(nc.Block())
        em = Emitter(nc, top)
        ctag = Buf("ctag")

        def load_consts(items):
            ev = None
            for (s_ap, d_ap, b) in items:
                ev = em.dma("sp", lambda e, s_ap=s_ap, d_ap=d_ap: e.dma_start(out=s_ap, in_=d_ap), ctag, writes=[b])
            for (_, _, b) in items:
                b.writer = ev

        em.op("dve", lambda e: e.memset(cst[:], 0.0), writes=[b_cst])
        em.op("dve", lambda e: e.memset(cst[:, 1:2], EPS), writes=[b_cst])

        with ExitStack() as p0:
            cT, b_cT = SB(p0, "cT", [128, KC, NB])
            siluT, b_siluT = SB(p0, "siluT", [128, KC, NB])
            bcol, b_bcol = SB(p0, "bcol", [128, 48])
            lam, b_lam = SB(p0, "lam", [128, 256])
            lj, b_lj = SB(p0, "lj", [128, 64])
            ls, b_ls = SB(p0, "ls", [128, 4])
            ones, b_ones = SB(p0, "ones", [128, 128])
            sbc, b_sbc = SB(p0, "sbc", [128, NB, KC, 128])
            wst = [SB(p0, f"wst{i}", [128, KC, 512]) for i in range(2)]
            brs = [SB(p0, f"brs{i}", [1, 512]) for i in range(2)]
            rst = [SB(p0, f"rst{i}", [128, 512]) for i in range(2)]
            load_consts([(cT[:], cT_d, b_cT), (bcol[:], bcol_d, b_bcol), (lam[:], lam_d, b_lam)])
            em.op("dve", lambda e: e.memset(ones[:], 1.0), writes=[b_ones])
            em.op("act", lambda e: e.activation(out=siluT[:], in_=cT[:], func=AF.Silu), reads=[b_cT], writes=[b_siluT])
            for t in range(2):
                em.op("dve", lambda e, t=t: e.scalar_tensor_tensor(
                    out=lj[:], in0=lam[:, t * 128:t * 128 + 64], scalar=1.0, in1=lam[:, t * 128 + 64:t * 128 + 128],
                    op0=ALU.mult, op1=ALU.mult, accum_out=ls[:, t:t + 1]), reads=[b_lam], writes=[b_lj, b_ls])
            em.op("act", lambda e: e.activation(out=ls[:, 2:4], in_=ls[:, 0:2], func=AF.Exp), reads=[b_ls], writes=[b_ls])
            em.op("dve", lambda e: e.scalar_tensor_tensor(out=cst[:, 0:1], in0=ls[:, 3:4], scalar=-LAMBDA_INIT, in1=ls[:, 2:3],
                                                           op0=ALU.add, op1=ALU.subtract), reads=[b_ls], writes=[b_cst])
            for b in range(NB):
                for kc in range(KC):
                    em.op("dve", lambda e, b=b, kc=kc: e.tensor_scalar(
                        out=sbc[:, b, kc, :], in0=ones[:], scalar1=siluT[:, kc, b:b + 1], scalar2=None, op0=ALU.mult),
                        reads=[b_ones, b_siluT], writes=[b_sbc])
            rr = 0
            for piece in range(12):
                (w, b_w) = wst[piece % 2]
                em.dma("sp", lambda e, w=w, piece=piece: e.dma_start(out=w[:], in_=wada_d[:, :, piece * 512:(piece + 1) * 512]),
                       b_w, writes=[b_w])
                if piece < 4:
                    for jb4 in range(4):
                        jb = piece * 4 + jb4
                        pt, b_pt = banks[rr % 8]; rr += 1
                        for kc in range(KC):
                            em.op("pe", lambda e, pt=pt, w=w, kc=kc, jb4=jb4: e.matmul(
                                pt[:, 0:NB], lhsT=w[:, kc, jb4 * 128:(jb4 + 1) * 128], rhs=siluT[:, kc, :],
                                start=(kc == 0), stop=(kc == KC - 1)), reads=[b_w, b_siluT], writes=[b_pt])
                        em.op("dve", lambda e, pt=pt, jb=jb: e.tensor_scalar(
                            out=colmod[:, jb, :], in0=pt[:, 0:NB], scalar1=bcol[:, jb:jb + 1],
                            scalar2=(1.0 if jb >= 8 else 0.0), op0=ALU.add, op1=ALU.add),
                            reads=[b_pt, b_bcol], writes=[b_colmod])
                else:
                    prm = (piece - 4) // 2
                    half = (piece - 4) % 2
                    (br, b_br) = brs[piece % 2]
                    em.dma("sp", lambda e, br=br, piece=piece: e.dma_start(out=br[:], in_=brow_d[:, piece * 512:(piece + 1) * 512]),
                           b_br, writes=[b_br])
                    for b in range(NB):
                        pt, b_pt = banks[rr % 8]; rr += 1
                        for kc in range(KC):
                            em.op("pe", lambda e, pt=pt, w=w, kc=kc, b=b: e.matmul(
                                pt[:, :], lhsT=sbc[:, b, kc, :], rhs=w[:, kc, :], start=(kc == 0), stop=False),
                                reads=[b_w, b_sbc], writes=[b_pt])
                        em.op("pe", lambda e, pt=pt, br=br: e.matmul(pt[:, :], lhsT=ones[0:1, :], rhs=br[0:1, :], start=False, stop=True),
                              reads=[b_br, b_ones], writes=[b_pt])
                        (r, b_r) = rst[(piece * NB + b) % 2]
                        em.op("act", lambda e, r=r, pt=pt, prm=prm: e.activation(
                            out=r[:], in_=pt[:, :], func=AF.Identity, bias=(cst[:, 2:3] if prm != 2 else ones[:, 0:1]), scale=1.0),
                            reads=[b_pt, b_cst, b_ones], writes=[b_r])
                        em.dma("sp", lambda e, r=r, b=b, prm=prm, half=half: e.dma_start(
                            out=rows_d[b, prm, :, half * 512:(half + 1) * 512], in_=r[:]), b_r, reads=[b_r])
            em.barrier()

        with ExitStack() as pc:
            cf = [SB(pc, f"cf{i}", [128, 4, D]) for i in range(2)]
            cb = [SB(pc, f"cb{i}", [128, 4, D], BF16) for i in range(2)]
            RPP = NEXP // 128
            n = 0
            for (src_d, dst_d) in ((utab_d, ubf_d), (vtab_d, vbf_d)):
                sv_ = src_d.rearrange("(p i) d -> p i d", p=128)
                dv_ = dst_d.rearrange("(p i) d -> p i d", p=128)
                for i0 in range(0, RPP, 4):
                    (f_, b_f) = cf[n % 2]
                    (h_, b_h) = cb[n % 2]
                    em.dma("sp", lambda e, f_=f_, sv_=sv_, i0=i0: e.dma_start(out=f_[:], in_=sv_[:, i0:i0 + 4, :]), b_f, writes=[b_f])
                    if n % 2 == 0:
                        em.op("act", lambda e, f_=f_, h_=h_: e.activation(out=h_[:], in_=f_[:], func=AF.Copy), reads=[b_f], writes=[b_h])
                    else:
                        em.op("dve", lambda e, f_=f_, h_=h_: e.tensor_copy(out=h_[:], in_=f_[:]), reads=[b_f], writes=[b_h])
                    em.dma("sp", lambda e, h_=h_, dv_=dv_, i0=i0: e.dma_start(out=dv_[:, i0:i0 + 4, :], in_=h_[:]), b_h, reads=[b_h])
                    n += 1
            em.barrier()

        with ExitStack() as pa:
            win, b_win = SB(pa, "win", [128, KC, INW], BF16)
            wout, b_wout = SB(pa, "wout", [128, KC, D], BF16)
            identb, b_identb = SB(pa, "identb", [128, 128], BF16)
            maskneg, b_maskneg = SB(pa, "maskneg", [128, 128], BF16)
            wtab, b_wtab = SB(pa, "wtab", [128, NT, 4])
            swab, b_swab = SB(pa, "swab", [128, 8, 256])
            ln1, b_ln1 = SB(pa, "ln1", [128, 2, D])
            subg, b_subg = SB(pa, "subg", [128, 128])
            sinks, b_sinks = SB(pa, "sinks", [128, 8])
            rows, b_rows = SB(pa, "rows", [128, 3, D])
            dkT, _ = SB(pa, "dkT", [128, 4, S], BF16)
            dV, _ = SB(pa, "dV", [128, NT, 4, 130], BF16)
            skT, _ = SB(pa, "skTa", [64, 2, S], BF16)
            sv, _ = SB(pa, "sv", [128, NT, 128], BF16)
            b_kv = [Buf(f"kv{i}") for i in range(NT)]
            xTt = [SB(pa, f"xTt{i}", [128, KC, 128]) for i in range(2)]
            xt = [SB(pa, f"xt{i}", [128, D]) for i in range(2)]
            hT, b_hT = SB(pa, "hT", [128, KC, 128], BF16)
            dqT, b_dqT = SB(pa, "dqT", [128, 4, 128], BF16)
            sqT, b_sqT = SB(pa, "sqT", [64, 8, 128], BF16)
            PT = [SB(pa, f"PT{i}", [128, 512], BF16) for i in range(3)]
            od = [SB(pa, f"od{i}", [128, 128]) for i in range(2)]
            oj, b_oj = SB(pa, "oj", [128, 128], BF16)
            on, b_on = SB(pa, "on", [128, D], BF16)
            oT, b_oT = SB(pa, "oT", [128, KC, 128], BF16)
            ssb = [SB(pa, f"ssb{i}", [128, 256]) for i in range(2)]
            Psw = [SB(pa, f"Psw{i}", [128, 256], BF16) for i in range(2)]
            PTs = [SB(pa, f"PTs{i}", [128, 256], BF16) for i in range(2)]
            sm = [SB(pa, f"sm{i}", [128, 8]) for i in range(4)]
            tA, b_tA = SB(pa, "tA", [128, D])
            tB, b_tB = SB(pa, "tB", [128, D])
            x1o = [SB(pa, f"x1o{i}", [128, D]) for i in range(2)]
            h2o = [SB(pa, f"h2o{i}", [128, D]) for i in range(2)]
            st6, b_st6 = SB(pa, "st6", [128, 2, 6])
            wstg = [SB(pa, f"wstg{i}", [128, KC, 256]) for i in range(2)]

            load_consts([(identb[:], identb_d, b_identb), (maskneg[:], maskneg_d, b_maskneg), (wtab[:], wtab_d, b_wtab),
                         (swab[:], swab_d, b_swab), (ln1[:], lnr_d[:, 0:2, :], b_ln1), (subg[:], subg_d, b_subg),
                         (sinks[:], sinks_d, b_sinks)])
            em.op("dve", lambda e: e.tensor_scalar(out=subg[:], in0=subg[:], scalar1=(1.0 - LAMBDA_INIT), scalar2=None, op0=ALU.mult),
                  reads=[b_subg], writes=[b_subg])
            for pc in range(INW // 256 + D // 256):
                (wg, b_wg) = wstg[pc % 2]
                if pc < INW // 256:
                    src = win_d[:, :, pc * 256:(pc + 1) * 256]; dst = win[:, :, pc * 256:(pc + 1) * 256]; bd = b_win
                else:
                    q = pc - INW // 256
                    src = wout_d[:, :, q * 256:(q + 1) * 256]; dst = wout[:, :, q * 256:(q + 1) * 256]; bd = b_wout
                em.dma("sp", lambda e, wg=wg, src=src: e.dma_start(out=wg[:], in_=src), b_wg, writes=[b_wg])
                em.op("dve" if pc % 2 == 0 else "pool", lambda e, wg=wg, dst=dst: e.tensor_copy(out=dst, in_=wg[:]),
                      reads=[b_wg], writes=[bd])

            ACC = [banks[0], banks[1]]
            MIX = [banks[2], banks[3]]
            gen = [banks[4], banks[5], banks[6], banks[7]]
            gi = [0]

            def gbank():
                r = gen[gi[0] % 4]; gi[0] += 1
                return r

            def load_tile(g):
                b, i = divmod(g, NT)
                (xa, b_xa) = xTt[g % 2]
                (xb, b_xb) = xt[g % 2]
                em.dma("sp", lambda e: e.dma_start(out=xa[:], in_=xT_d[b, :, :, i * 128:(i + 1) * 128]), b_xa, writes=[b_xa])
                em.dma("sp", lambda e: e.dma_start(out=xb[:], in_=x_tok[g * 128:(g + 1) * 128, :]), b_xb, writes=[b_xb])

            cnt3 = [0, 0, 0]
            load_tile(0)
            for g in range(NB * NT):
                b, i = divmod(g, NT)
                if i == 0:
                    em.dma("sp", lambda e: e.dma_start(out=rows[:], in_=rows_d[b, 0:3, :, :].rearrange("r p d -> p r d")),
                           b_rows, writes=[b_rows])
                if g + 1 < NB * NT:
                    load_tile(g + 1)
                (xa, b_xa) = xTt[g % 2]
                (xb, b_xb) = xt[g % 2]
                bkv = b_kv[i]
                for kc in range(KC):
                    em.op("act", lambda e, kc=kc: e.activation(out=hT[:, kc, :], in_=xa[:, kc, :], func=AF.Identity,
                                                                bias=colmod[:, kc, b:b + 1], scale=colmod[:, 8 + kc, b:b + 1]),
                          reads=[b_xa, b_colmod], writes=[b_hT])
                pq, b_pq = gbank()
                for h in range(4):
                    for kc in range(KC):
                        em.op("pe", lambda e, h=h, kc=kc: e.matmul(pq[:, h * 128:(h + 1) * 128], lhsT=win[:, kc, h * 128:(h + 1) * 128],
                                                                    rhs=hT[:, kc, :], start=(kc == 0), stop=(kc == KC - 1)),
                              reads=[b_win, b_hT], writes=[b_pq])
                em.op("dve", lambda e: e.tensor_copy(out=dqT[:].rearrange("p h t -> p (h t)"), in_=pq[:, :]), reads=[b_pq], writes=[b_dqT])
                pk, b_pk = gbank()
                for h in range(4):
                    for kc in range(KC):
                        em.op("pe", lambda e, h=h, kc=kc: e.matmul(pk[:, h * 128:(h + 1) * 128], lhsT=win[:, kc, 512 + h * 128:512 + (h + 1) * 128],
                                                                    rhs=hT[:, kc, :], start=(kc == 0), stop=(kc == KC - 1)),
                              reads=[b_win, b_hT], writes=[b_pk])
                em.op("act", lambda e: e.activation(out=dkT[:, :, i * 128:(i + 1) * 128], in_=pk[:, :].rearrange("p (h t) -> p h t", h=4), func=AF.Copy),
                      reads=[b_pk], writes=[bkv])
                for half in range(2):
                    psq, b_psq = gbank()
                    for hh in range(4):
                        hq = half * 4 + hh
                        for kc in range(KC):
                            em.op("pe", lambda e, hq=hq, hh=hh, kc=kc: e.matmul(
                                psq[0:64, hh * 128:(hh + 1) * 128], lhsT=win[:, kc, 1536 + hq * 64:1536 + (hq + 1) * 64],
                                rhs=hT[:, kc, :], start=(kc == 0), stop=(kc == KC - 1)), reads=[b_win, b_hT], writes=[b_psq])
                    em.op("dve", lambda e, half=half: e.tensor_copy(out=sqT[:, half * 4:(half + 1) * 4, :].rearrange("p h t -> p (h t)"), in_=psq[0:64, :]),
                          reads=[b_psq], writes=[b_sqT])
                psk, b_psk = gbank()
                for gk in range(2):
                    for kc in range(KC):
                        em.op("pe", lambda e, gk=gk, kc=kc: e.matmul(
                            psk[0:64, gk * 128:(gk + 1) * 128], lhsT=win[:, kc, 2048 + gk * 64:2048 + (gk + 1) * 64],
                            rhs=hT[:, kc, :], start=(kc == 0), stop=(kc == KC - 1)), reads=[b_win, b_hT], writes=[b_psk])
                for kc in range(KC):
                    em.op("pe", lambda e, kc=kc: e.matmul(psk[:, 256:384], lhsT=hT[:, kc, :], rhs=win[:, kc, 2176:2304],
                                                           start=(kc == 0), stop=(kc == KC - 1), skip_group_check=True),
                          reads=[b_win, b_hT], writes=[b_psk])
                em.op("act", lambda e: e.activation(out=skT[:, :, i * 128:(i + 1) * 128], in_=psk[0:64, 0:256].rearrange("p (h t) -> p h t", h=2), func=AF.Copy),
                      reads=[b_psk], writes=[bkv])
                em.op("act", lambda e: e.activation(out=sv[:, i, :], in_=psk[:, 256:384], func=AF.Copy), reads=[b_psk], writes=[bkv])
                pv, b_pv = gbank()
                for kc in range(KC):
                    em.op("pe", lambda e, kc=kc: e.matmul(pv[:, :], lhsT=hT[:, kc, :], rhs=win[:, kc, 1024:1536],
                                                           start=(kc == 0), stop=(kc == KC - 1)), reads=[b_win, b_hT], writes=[b_pv])
                for h in range(4):
                    em.op("act" if h % 2 == 0 else "dve",
                          (lambda e, h=h: e.activation(out=dV[:, i, h, 0:128], in_=pv[:, h * 128:(h + 1) * 128], func=AF.Identity, bias=cst[:, 2:3], scale=wtab[:, i, h:h + 1]))
                          if h % 2 == 0 else
                          (lambda e, h=h: e.tensor_scalar(out=dV[:, i, h, 0:128], in0=pv[:, h * 128:(h + 1) * 128], scalar1=wtab[:, i, h:h + 1], scalar2=None, op0=ALU.mult)),
                          reads=[b_pv, b_wtab], writes=[bkv])
                em.op("dve", lambda e: e.tensor_copy(out=dV[:, i, :, 128], in_=wtab[:, i, :]), reads=[b_wtab], writes=[bkv])

                for h in range(4):
                    for m in range(2):
                        acc, b_acc = ACC[m]
                        for g0 in range(0, i + 1, 4):
                            kbs = list(range(g0, min(g0 + 4, i + 1)))
                            sp_, b_sp = gbank()
                            for s_, kb in enumerate(kbs):
                                em.op("pe", lambda e, s_=s_, kb=kb: e.matmul(
                                    sp_[:, s_ * 128:(s_ + 1) * 128], lhsT=dkT[64 * m:64 * m + 64, h, kb * 128:(kb + 1) * 128],
                                    rhs=dqT[64 * m:64 * m + 64, h, :], start=True, stop=(kb != i)),
                                    reads=[b_kv[kb], b_dqT], writes=[b_sp])
                                if kb == i:
                                    em.op("pe", lambda e, s_=s_: e.matmul(sp_[:, s_ * 128:(s_ + 1) * 128], lhsT=identb[:, :], rhs=maskneg[:, :],
                                                                          start=False, stop=True),
                                          reads=[b_identb, b_maskneg], writes=[b_sp])
                            n = len(kbs) * 128
                            (pt_, b_pt_) = PT[cnt3[0] % 3]; cnt3[0] += 1
                            em.op("act", lambda e, n=n, pt_=pt_: e.activation(out=pt_[:, 0:n], in_=sp_[:, 0:n], func=AF.Exp, scale=0.125),
                                  reads=[b_sp], writes=[b_pt_])
                            for s_, kb in enumerate(kbs):
                                em.op("pe", lambda e, s_=s_, kb=kb, pt_=pt_: e.matmul(
                                    acc[:, 0:129], lhsT=pt_[:, s_ * 128:(s_ + 1) * 128], rhs=dV[:, kb, h, 0:129],
                                    start=(kb == 0), stop=(kb == i)), reads=[b_pt_, b_kv[kb]], writes=[b_acc])
                    (s4, b_s4) = sm[cnt3[1] % 4]; cnt3[1] += 1
                    (o1, b_o1) = od[0]
                    (o2, b_o2) = od[1]
                    a0, b_a0 = ACC[0]
                    a1, b_a1 = ACC[1]
                    em.op("dve", lambda e: e.reciprocal(out=s4[:, 0:1], in_=a0[:, 128:129]), reads=[b_a0], writes=[b_s4])
                    em.op("dve", lambda e: e.reciprocal(out=s4[:, 1:2], in_=a1[:, 128:129]), reads=[b_a1], writes=[b_s4])
                    em.op("dve", lambda e: e.tensor_tensor(out=s4[:, 2:3], in0=s4[:, 1:2], in1=cst[:, 0:1], op=ALU.mult), reads=[b_s4, b_cst], writes=[b_s4])
                    em.op("act", lambda e: e.activation(out=o1[:], in_=a0[:, 0:128], func=AF.Identity, bias=cst[:, 2:3], scale=s4[:, 0:1]), reads=[b_a0, b_s4], writes=[b_o1])
                    em.op("dve", lambda e: e.scalar_tensor_tensor(out=o2[:], in0=a1[:, 0:128], scalar=s4[:, 2:3], in1=o1[:], op0=ALU.mult, op1=ALU.add),
                          reads=[b_a1, b_s4, b_o1], writes=[b_o2])
                    em.op("dve", lambda e: e.scalar_tensor_tensor(out=oj[:], in0=o2[:], scalar=1.0, in1=o2[:], op0=ALU.mult, op1=ALU.mult, accum_out=s4[:, 3:4]),
                          reads=[b_o2], writes=[b_oj, b_s4])
                    em.op("act", lambda e: e.activation(out=s4[:, 4:5], in_=s4[:, 3:4], func=AF.Ln, bias=cst[:, 1:2], scale=1.0 / 128.0), reads=[b_s4, b_cst], writes=[b_s4])
                    em.op("act", lambda e: e.activation(out=s4[:, 5:6], in_=s4[:, 4:5], func=AF.Exp, scale=-0.5), reads=[b_s4], writes=[b_s4])
                    em.op("dve", lambda e, h=h: e.scalar_tensor_tensor(out=on[:, h * 128:(h + 1) * 128], in0=o2[:], scalar=s4[:, 5:6], in1=subg[:], op0=ALU.mult, op1=ALU.mult),
                          reads=[b_o2, b_s4, b_subg], writes=[b_on])

                for hq in range(8):
                    gk = hq // 4
                    nk = 256 if i > 0 else 128
                    k0 = (i - 1) * 128 if i > 0 else 0
                    kvdeps = [b_kv[i]] + ([b_kv[i - 1]] if i > 0 else [])
                    sp_, b_sp = gbank()
                    em.op("pe", lambda e, hq=hq, gk=gk, nk=nk, k0=k0: e.matmul(sp_[:, 0:nk], lhsT=sqT[:, hq, :], rhs=skT[:, gk, k0:k0 + nk], start=True, stop=True),
                          reads=[b_sqT] + kvdeps, writes=[b_sp])
                    (sb_, b_sb) = ssb[hq % 2]
                    (s4, b_s4) = sm[cnt3[1] % 4]; cnt3[1] += 1
                    em.op("dve", lambda e, hq=hq, nk=nk: e.scalar_tensor_tensor(out=sb_[:, 0:nk], in0=sp_[:, 0:nk], scalar=0.125, in1=swab[:, hq, 256 - nk:256],
                                                                              op0=ALU.mult, op1=ALU.add), reads=[b_sp, b_swab], writes=[b_sb])
                    em.op("dve", lambda e, nk=nk: e.tensor_reduce(out=s4[:, 0:1], in_=sb_[:, 0:nk], axis=AX.X, op=ALU.max), reads=[b_sb], writes=[b_s4])
                    em.op("dve", lambda e, hq=hq: e.tensor_scalar(out=s4[:, 1:2], in0=s4[:, 0:1], scalar1=sinks[:, hq:hq + 1], scalar2=-1.0, op0=ALU.max, op1=ALU.mult),
                          reads=[b_s4, b_sinks], writes=[b_s4])
                    (pw, b_pw) = Psw[hq % 2]
                    em.op("act", lambda e, nk=nk: e.activation(out=pw[:, 0:nk], in_=sb_[:, 0:nk], func=AF.Exp, bias=s4[:, 1:2], scale=1.0, accum_out=s4[:, 2:3]),
                          reads=[b_sb, b_s4], writes=[b_pw, b_s4])
                    em.op("act", lambda e, hq=hq: e.activation(out=s4[:, 3:4], in_=s4[:, 1:2], func=AF.Exp, bias=sinks[:, hq:hq + 1], scale=1.0),
                          reads=[b_s4, b_sinks], writes=[b_s4])
                    em.op("dve", lambda e: e.tensor_tensor(out=s4[:, 4:5], in0=s4[:, 2:3], in1=s4[:, 3:4], op=ALU.add), reads=[b_s4], writes=[b_s4])
                    em.op("dve", lambda e: e.reciprocal(out=s4[:, 5:6], in_=s4[:, 4:5]), reads=[b_s4], writes=[b_s4])
                    tp, b_tp = gbank()
                    tpv = tp[:, 0:128].bitcast(BF16)
                    for bl in range(nk // 128):
                        em.op("pe", lambda e, bl=bl: e.transpose(out=tpv[:, bl * 128:(bl + 1) * 128], in_=pw[:, bl * 128:(bl + 1) * 128], identity=identb[:]),
                              reads=[b_pw, b_identb], writes=[b_tp])
                    (pts, b_pts) = PTs[hq % 2]
                    em.op("act", lambda e, nk=nk: e.activation(out=pts[:, 0:nk], in_=tpv[:, 0:nk], func=AF.Copy), reads=[b_tp], writes=[b_pts])
                    for bl in range(nk // 128):
                        kt = (i - 1 + bl) if i > 0 else i
                        em.op("pe", lambda e, bl=bl, kt=kt, gk=gk, nk=nk: e.matmul(tp[:, 256:320], lhsT=pts[:, bl * 128:(bl + 1) * 128], rhs=sv[:, kt, gk * 64:(gk + 1) * 64],
                                                                                   start=(bl == 0), stop=(bl == nk // 128 - 1), skip_group_check=True),
                              reads=[b_pts] + kvdeps, writes=[b_tp])
                    em.op("act", lambda e, hq=hq: e.activation(out=on[:, 512 + hq * 64:512 + (hq + 1) * 64], in_=tp[:, 256:320], func=AF.Identity, bias=cst[:, 2:3], scale=s4[:, 5:6]),
                          reads=[b_tp, b_s4], writes=[b_on])

                tp, b_tp = gbank()
                tpv = tp[:, :].bitcast(BF16)
                for c in range(KC):
                    em.op("pe", lambda e, c=c: e.transpose(out=tpv[:, c * 128:(c + 1) * 128], in_=on[:, c * 128:(c + 1) * 128], identity=identb[:]),
                          reads=[b_on, b_identb], writes=[b_tp])
                em.op("dve", lambda e: e.tensor_copy(out=oT[:].rearrange("p c t -> p (c t)"), in_=tpv[:, :]), reads=[b_tp], writes=[b_oT])
                for half in range(2):
                    mx, b_mx = MIX[half]
                    for c in range(KC):
                        em.op("pe", lambda e, c=c, half=half, mx=mx: e.matmul(mx[:, :], lhsT=oT[:, c, :], rhs=wout[:, c, half * 512:(half + 1) * 512],
                                                                             start=(c == 0), stop=(c == KC - 1)), reads=[b_oT, b_wout], writes=[b_mx])
                for half in range(2):
                    mx, b_mx = MIX[half]
                    em.op("dve", lambda e, half=half, mx=mx: e.tensor_tensor(out=tA[:, half * 512:(half + 1) * 512], in0=mx[:, :], in1=rows[:, 0, half * 512:(half + 1) * 512], op=ALU.mult),
                          reads=[b_mx, b_rows], writes=[b_tA])
                em.op("dve", lambda e: e.scalar_tensor_tensor(out=tB[:], in0=xb[:], scalar=ALPHA, in1=tA[:], op0=ALU.mult, op1=ALU.add),
                      reads=[b_xb, b_tA], writes=[b_tB])
                (s4, b_s4) = sm[cnt3[1] % 4]; cnt3[1] += 1
                for c in range(2):
                    em.op("dve", lambda e, c=c: e.bn_stats(out=st6[:, c, :], in_=tB[:, c * 512:(c + 1) * 512]), reads=[b_tB], writes=[b_st6])
                em.op("dve", lambda e: e.bn_aggr(out=s4[:, 0:2], in_=st6[:].rearrange("p a b -> p (a b)")), reads=[b_st6], writes=[b_s4])
                em.op("act", lambda e: e.activation(out=s4[:, 2:3], in_=s4[:, 1:2], func=AF.Ln, bias=cst[:, 1:2], scale=1.0), reads=[b_s4, b_cst], writes=[b_s4])
                em.op("act", lambda e: e.activation(out=s4[:, 3:4], in_=s4[:, 2:3], func=AF.Exp, scale=-0.5), reads=[b_s4], writes=[b_s4])
                em.op("dve", lambda e: e.tensor_scalar(out=tA[:], in0=tB[:], scalar1=s4[:, 0:1], scalar2=s4[:, 3:4], op0=ALU.subtract, op1=ALU.mult),
                      reads=[b_tB, b_s4], writes=[b_tA])
                (x1, b_x1) = x1o[g % 2]
                (h2, b_h2) = h2o[g % 2]
                em.op("pool", lambda e: e.tensor_tensor(out=tB[:], in0=tA[:], in1=ln1[:, 0, :], op=ALU.mult), reads=[b_tA, b_ln1], writes=[b_tB])
                em.op("pool", lambda e: e.tensor_tensor(out=x1[:], in0=tB[:], in1=ln1[:, 1, :], op=ALU.add), reads=[b_tB, b_ln1], writes=[b_x1])
                em.dma("sp", lambda e: e.dma_start(out=x1s_d[g * 128:(g + 1) * 128, :], in_=x1[:]), b_x1, reads=[b_x1])
                em.op("pool", lambda e: e.tensor_tensor(out=tA[:], in0=x1[:], in1=rows[:, 2, :], op=ALU.mult), reads=[b_x1, b_rows], writes=[b_tA])
                em.op("pool", lambda e: e.tensor_tensor(out=h2[:], in0=tA[:], in1=rows[:, 1, :], op=ALU.add), reads=[b_tA, b_rows], writes=[b_h2])
                em.dma("sp", lambda e: e.dma_start(out=h2s_d[g * 128:(g + 1) * 128, :], in_=h2[:]), b_h2, reads=[b_h2])
            em.barrier()

        with ExitStack() as pb:
            wpq, b_wpq = SB(pb, "wpq", [128, KC, 2048])
            skTp, b_skTp = SB(pb, "skTp", [128, 16, 128])
            identf, b_identf = SB(pb, "identf", [128, 128])
            ln2, b_ln2 = SB(pb, "ln2", [128, 2, D])
            g2r, b_g2r = SB(pb, "g2r", [128, D])
            GB = [SB(pb, f"GB{i}", [128, D], BF16) for i in range(NSLOT)]
            x1t = [SB(pb, f"x1t{i}", [128, D]) for i in range(2)]
            h2t = [SB(pb, f"h2t{i}", [128, D]) for i in range(2)]
            h2T, b_h2T = SB(pb, "h2T", [128, KC, 128])
            qT, b_qT = SB(pb, "qT", [128, 16, 128])
            vals, b_vals = SB(pb, "vals", [128, 16, 16])
            idxs, b_idxs = SB(pb, "idxs", [128, 16, 16], U32)
            idxf, b_idxf = SB(pb, "idxf", [128, 16, 16])
            scw = [SB(pb, f"scw{i}", [128, 128]) for i in range(2)]
            cand = [SB(pb, f"cand{i}", [128, 256]) for i in range(2)]
            cidx = [SB(pb, f"cidx{i}", [128, 256]) for i in range(2)]
            cw = [SB(pb, f"cw{i}", [128, 256]) for i in range(2)]
            tops, b_tops = SB(pb, "tops", [128, 8, 16])
            gate = [SB(pb, f"gate{i}", [128, 8, 16]) for i in range(2)]
            eidf, b_eidf = SB(pb, "eidf", [128, 128])
            eid = [SB(pb, f"eid{i}", [128, 128], I32) for i in range(2)]
            eidT = [SB(pb, f"eidT{i}", [128, 128], I32) for i in range(2)]
            apre, b_apre = SB(pb, "apre", [128, 128])
            ga, b_ga = SB(pb, "ga", [128, 128])
            gaT, b_gaT = SB(pb, "gaT", [128, 128], BF16)
            junk, _ = SB(pb, "junk", [128, D], BF16)
            junk2, _ = SB(pb, "junk2", [128, 256], BF16)
            ffnT, b_ffnT = SB(pb, "ffnT", [128, KC, 128])
            wA, b_wA = SB(pb, "wA", [128, D])
            wB, b_wB = SB(pb, "wB", [128, D])
            outt = [SB(pb, f"outt{i}", [128, D]) for i in range(2)]
            st6b, b_st6b = SB(pb, "st6b", [128, 2, 6])
            smb = [SB(pb, f"smb{i}", [128, 16]) for i in range(2)]

            load_consts([(wpq[:, 0:4, :], wpq_d[:, 0:4, :], b_wpq), (wpq[:, 4:8, :], wpq_d[:, 4:8, :], b_wpq),
                         (skTp[:], skT_d, b_skTp), (identf[:], identf_d, b_identf), (ln2[:], lnr_d[:, 2:4, :], b_ln2)])
            VT = [banks[0], banks[1]]
            bi = [0]

            def nbank():
                r = banks[2 + bi[0] % 6]; bi[0] += 1
                return r

            def load_tile_b(g):
                (a, b_a) = x1t[g % 2]
                (h, b_h) = h2t[g % 2]
                em.dma("sp", lambda e: e.dma_start(out=h[:], in_=h2s_d[g * 128:(g + 1) * 128, :]), b_h, writes=[b_h])
                em.dma("sp", lambda e: e.dma_start(out=a[:], in_=x1s_d[g * 128:(g + 1) * 128, :]), b_a, writes=[b_a])

            def score(g):
                (hh, b_hh) = h2t[g % 2]
                for half in range(2):
                    tp, b_tp = nbank()
                    for c4 in range(4):
                        c = half * 4 + c4
                        em.op("pe", lambda e, c=c, c4=c4, tp=tp: e.transpose(out=tp[:, c4 * 128:(c4 + 1) * 128], in_=hh[:, c * 128:(c + 1) * 128], identity=identf[:]),
                              reads=[b_hh, b_identf], writes=[b_tp])
                    em.op("act", lambda e, tp=tp, half=half: e.activation(out=h2T[:, half * 4:(half + 1) * 4, :].rearrange("p c t -> p (c t)"), in_=tp[:, :], func=AF.Copy),
                          reads=[b_tp], writes=[b_h2T])
                for q4 in range(4):
                    pq, b_pq = nbank()
                    for c4 in range(4):
                        c16 = q4 * 4 + c4
                        for kc in range(KC):
                            em.op("pe", lambda e, c16=c16, c4=c4, kc=kc, pq=pq: e.matmul(pq[:, c4 * 128:(c4 + 1) * 128], lhsT=wpq[:, kc, c16 * 128:(c16 + 1) * 128],
                                                                                       rhs=h2T[:, kc, :], start=(kc == 0), stop=(kc == KC - 1)),
                                  reads=[b_wpq, b_h2T], writes=[b_pq])
                    em.op("act", lambda e, pq=pq, q4=q4: e.activation(out=qT[:, q4 * 4:(q4 + 1) * 4, :].rearrange("p c t -> p (c t)"), in_=pq[:, :], func=AF.Copy),
                          reads=[b_pq], writes=[b_qT])
                scb = []
                for q4 in range(4):
                    ps_, b_ps = nbank()
                    scb.append((ps_, b_ps))
                    for c4 in range(4):
                        c16 = q4 * 4 + c4
                        em.op("pe", lambda e, c16=c16, c4=c4, ps_=ps_: e.matmul(ps_[:, c4 * 128:(c4 + 1) * 128], lhsT=qT[:, c16, :], rhs=skTp[:, c16, :], start=True, stop=True),
                              reads=[b_qT, b_skTp], writes=[b_ps])
                return scb

            def topk(g, scb):
                for c16 in range(16):
                    ps_, b_ps = scb[c16 // 4]
                    src = ps_[:, (c16 % 4) * 128:(c16 % 4 + 1) * 128]
                    (sw, b_sw) = scw[c16 % 2]
                    em.op("dve", lambda e, c16=c16, src=src: e.max(out=vals[:, c16, 0:8], in_=src), reads=[b_ps], writes=[b_vals])
                    em.op("dve", lambda e, c16=c16, src=src: e.max_index(out=idxs[:, c16, 0:8], in_max=vals[:, c16, 0:8], in_values=src), reads=[b_ps, b_vals], writes=[b_idxs])
                    em.op("dve", lambda e, c16=c16, src=src, sw=sw: e.match_replace(out=sw[:], in_to_replace=vals[:, c16, 0:8], in_values=src, imm_value=-1e30),
                          reads=[b_ps, b_vals], writes=[b_sw])
                    em.op("dve", lambda e, c16=c16, sw=sw: e.max(out=vals[:, c16, 8:16], in_=sw[:]), reads=[b_sw], writes=[b_vals])
                    em.op("dve", lambda e, c16=c16, sw=sw: e.max_index(out=idxs[:, c16, 8:16], in_max=vals[:, c16, 8:16], in_values=sw[:]), reads=[b_sw, b_vals], writes=[b_idxs])
                em.op("dve", lambda e: e.tensor_copy(out=idxf[:], in_=idxs[:]), reads=[b_idxs], writes=[b_idxf])
                i4 = idxf[:].rearrange("p (h two) k -> p h two k", two=2)
                em.op("dve", lambda e: e.tensor_scalar(out=i4[:, :, 0, :], in0=i4[:, :, 0, :], scalar1=128.0, scalar2=None, op0=ALU.mult), reads=[b_idxf], writes=[b_idxf])
                for h in range(8):
                    (cd, b_cd) = cand[h % 2]
                    (ci, b_ci) = cidx[h % 2]
                    (cw_, b_cw) = cw[h % 2]
                    em.op("dve", lambda e, h=h, cd=cd: e.tensor_tensor(out=cd[:].rearrange("p (i j) -> p i j", i=16),
                                                                     in0=vals[:, 2 * h, :].unsqueeze(2).to_broadcast([128, 16, 16]),
                                                                     in1=vals[:, 2 * h + 1, :].unsqueeze(1).to_broadcast([128, 16, 16]), op=ALU.add),
                          reads=[b_vals], writes=[b_cd])
                    em.op("dve", lambda e, h=h, ci=ci: e.tensor_tensor(out=ci[:].rearrange("p (i j) -> p i j", i=16),
                                                                     in0=idxf[:, 2 * h, :].unsqueeze(2).to_broadcast([128, 16, 16]),
                                                                     in1=idxf[:, 2 * h + 1, :].unsqueeze(1).to_broadcast([128, 16, 16]), op=ALU.add),
                          reads=[b_idxf], writes=[b_ci])
                    em.op("dve", lambda e, h=h, cd=cd: e.max(out=tops[:, h, 0:8], in_=cd[:]), reads=[b_cd], writes=[b_tops])
                    em.op("dve", lambda e, h=h, cd=cd, cw_=cw_: e.match_replace(out=cw_[:], in_to_replace=tops[:, h, 0:8], in_values=cd[:], imm_value=-1e30),
                          reads=[b_cd, b_tops], writes=[b_cw])
                    em.op("dve", lambda e, h=h, cw_=cw_: e.max(out=tops[:, h, 8:16], in_=cw_[:]), reads=[b_cw], writes=[b_tops])
                    for k in range(16):
                        last = (k == 15)
                        em.op("dve", lambda e, h=h, k=k, cd=cd, ci=ci: e.scalar_tensor_tensor(
                            out=junk2[:], in0=cd[:], scalar=tops[:, h, k:k + 1], in1=ci[:], op0=ALU.is_equal, op1=ALU.mult,
                            accum_out=eidf[:, h * 16 + k:h * 16 + k + 1]),
                            reads=[b_cd, b_ci, b_tops], writes=([b_eidf] if (last and h == 7) else []))
                (ei, b_ei) = eid[g % 2]
                (eiT, b_eiT) = eidT[g % 2]
                em.op("dve", lambda e: e.tensor_scalar(out=eidf[:], in0=eidf[:], scalar1=float(NEXP - 1), scalar2=0.0, op0=ALU.min, op1=ALU.max),
                      reads=[b_eidf], writes=[b_eidf])
                em.op("dve", lambda e: e.tensor_copy(out=ei[:], in_=eidf[:]), reads=[b_eidf], writes=[b_ei])
                (gt, b_gt) = gate[g % 2]
                em.op("dve", lambda e: e.tensor_tensor(out=gt[:], in0=tops[:], in1=tops[:, :, 0:1].to_broadcast([128, 8, 16]), op=ALU.subtract),
                      reads=[b_tops], writes=[b_gt])
                em.op("act", lambda e: e.activation(out=gt[:], in_=gt[:], func=AF.Exp), reads=[b_gt], writes=[b_gt])
                (s8, b_s8) = smb[g % 2]
                em.op("dve", lambda e: e.tensor_reduce(out=s8[:, 0:8], in_=gt[:], axis=AX.X, op=ALU.add), reads=[b_gt], writes=[b_s8])
                em.op("dve", lambda e: e.reciprocal(out=s8[:, 8:16], in_=s8[:, 0:8]), reads=[b_s8], writes=[b_s8])
                em.op("dve", lambda e: e.tensor_tensor(out=gt[:], in0=gt[:], in1=s8[:, 8:16].unsqueeze(2).to_broadcast([128, 8, 16]), op=ALU.mult),
                      reads=[b_gt, b_s8], writes=[b_gt])
                tp, b_tp = nbank()
                em.op("pe", lambda e: e.transpose(out=tp[:, 0:128], in_=eidf[:], identity=identf[:]), reads=[b_eidf, b_identf], writes=[b_tp])
                em.op("act", lambda e: e.activation(out=eiT[:], in_=tp[:, 0:128], func=AF.Copy), reads=[b_tp], writes=[b_eiT])

            slot = [0]
            NG = NB * NT
            load_tile_b(0)
            scb_cur = score(0)
            topk(0, scb_cur)
            for g in range(NG):
                b, i = divmod(g, NT)
                if i == 0:
                    em.dma("sp", lambda e: e.dma_start(out=g2r[:], in_=rows_d[b, 3, :, :]), b_g2r, writes=[b_g2r])
                if g + 1 < NG:
                    load_tile_b(g + 1)
                    scb_next = score(g + 1)
                (xa, b_xa) = x1t[g % 2]
                (hh, b_hh) = h2t[g % 2]
                (ei, b_ei) = eid[g % 2]
                (eiT, b_eiT) = eidT[g % 2]
                (gt, b_gt) = gate[g % 2]
                (s8, b_s8) = smb[g % 2]
                for j in range(128):
                    (gb, b_gb) = GB[slot[0] % NSLOT]; slot[0] += 1
                    em.dma("pool", lambda e, j=j, gb=gb: e.indirect_dma_start(out=gb[:, :], out_offset=None, in_=ubf_d,
                                                                           in_offset=bass.IndirectOffsetOnAxis(ap=ei[:, j:j + 1], axis=0)),
                           b_gb, reads=[b_ei], writes=[b_gb])
                    em.op("dve", lambda e, j=j, gb=gb: e.scalar_tensor_tensor(out=junk[:], in0=gb[:], scalar=1.0, in1=hh[:], op0=ALU.mult, op1=ALU.mult,
                                                                            accum_out=apre[:, j:j + 1]),
                          reads=[b_gb, b_hh], writes=([b_apre] if j == 127 else []))
                em.op("act", lambda e: e.activation(out=ga[:], in_=apre[:], func=AF.Gelu), reads=[b_apre], writes=[b_ga])
                em.op("dve", lambda e: e.tensor_tensor(out=ga[:], in0=ga[:], in1=gt[:].rearrange("p h k -> p (h k)"), op=ALU.mult),
                      reads=[b_ga, b_gt], writes=[b_ga])
                tp, b_tp = nbank()
                em.op("pe", lambda e: e.transpose(out=tp[:, 0:128], in_=ga[:], identity=identf[:]), reads=[b_ga, b_identf], writes=[b_tp])
                em.op("act", lambda e: e.activation(out=gaT[:], in_=tp[:, 0:128], func=AF.Copy), reads=[b_tp], writes=[b_gaT])
                for t in range(128):
                    (gb, b_gb) = GB[slot[0] % NSLOT]; slot[0] += 1
                    em.dma("pool", lambda e, t=t, gb=gb: e.indirect_dma_start(out=gb[:, :], out_offset=None, in_=vbf_d,
                                                                           in_offset=bass.IndirectOffsetOnAxis(ap=eiT[:, t:t + 1], axis=0)),
                           b_gb, reads=[b_eiT], writes=[b_gb])
                    for c in range(KC):
                        vt, b_vt = VT[c // 4]
                        col = (c % 4) * 128 + t
                        em.op("pe", lambda e, c=c, t=t, gb=gb, vt=vt, col=col: e.matmul(vt[:, col:col + 1], lhsT=gb[:, c * 128:(c + 1) * 128], rhs=gaT[:, t:t + 1],
                                                                                      start=True, stop=True, skip_group_check=True),
                              reads=[b_gb, b_gaT], writes=[b_vt])
                if g + 1 < NG:
                    topk(g + 1, scb_next)
                for half in range(2):
                    vt, b_vt = VT[half]
                    em.op("act", lambda e, half=half, vt=vt: e.activation(out=ffnT[:, half * 4:(half + 1) * 4, :].rearrange("p c t -> p (c t)"), in_=vt[:, :], func=AF.Copy),
                          reads=[b_vt], writes=[b_ffnT])
                fts = []
                for half in range(2):
                    ft, b_ft = nbank()
                    fts.append((ft, b_ft))
                    for c4 in range(4):
                        c = half * 4 + c4
                        em.op("pe", lambda e, c=c, c4=c4, ft=ft: e.transpose(out=ft[:, c4 * 128:(c4 + 1) * 128], in_=ffnT[:, c, :], identity=identf[:]),
                              reads=[b_ffnT, b_identf], writes=[b_ft])
                for half in range(2):
                    ft, b_ft = fts[half]
                    em.op("dve", lambda e, half=half, ft=ft: e.tensor_tensor(out=wB[:, half * 512:(half + 1) * 512], in0=ft[:, :], in1=g2r[:, half * 512:(half + 1) * 512], op=ALU.mult),
                          reads=[b_ft, b_g2r], writes=[b_wB])
                em.op("dve", lambda e: e.scalar_tensor_tensor(out=wA[:], in0=xa[:], scalar=ALPHA, in1=wB[:], op0=ALU.mult, op1=ALU.add),
                      reads=[b_xa, b_wB], writes=[b_wA])
                for c in range(2):
                    em.op("dve", lambda e, c=c: e.bn_stats(out=st6b[:, c, :], in_=wA[:, c * 512:(c + 1) * 512]), reads=[b_wA], writes=[b_st6b])
                em.op("dve", lambda e: e.bn_aggr(out=s8[:, 0:2], in_=st6b[:].rearrange("p a b -> p (a b)")), reads=[b_st6b], writes=[b_s8])
                em.op("act", lambda e: e.activation(out=s8[:, 2:3], in_=s8[:, 1:2], func=AF.Ln, bias=cst[:, 1:2], scale=1.0), reads=[b_s8, b_cst], writes=[b_s8])
                em.op("act", lambda e: e.activation(out=s8[:, 3:4], in_=s8[:, 2:3], func=AF.Exp, scale=-0.5), reads=[b_s8], writes=[b_s8])
                em.op("dve", lambda e: e.tensor_scalar(out=wB[:], in0=wA[:], scalar1=s8[:, 0:1], scalar2=s8[:, 3:4], op0=ALU.subtract, op1=ALU.mult),
                      reads=[b_wA, b_s8], writes=[b_wB])
                (ot, b_ot) = outt[g % 2]
                em.op("dve", lambda e: e.tensor_tensor(out=wA[:], in0=wB[:], in1=ln2[:, 0, :], op=ALU.mult), reads=[b_wB, b_ln2], writes=[b_wA])
                em.op("dve", lambda e: e.tensor_tensor(out=ot[:], in0=wA[:], in1=ln2[:, 1, :], op=ALU.add), reads=[b_wA, b_ln2], writes=[b_ot])
                em.dma("sp", lambda e: e.dma_start(out=out_d[g * 128:(g + 1) * 128, :], in_=ot[:]), b_ot, reads=[b_ot])
                if g + 1 < NG:
                    scb_cur = scb_next
            em.barrier()
        build.info = dict(ninstr=dict(em.ninstr), nsem=em.nsem)
    return nc


def _host_layout(inputs, NB, S):
    f32 = np.float32
    x = np.ascontiguousarray(inputs["x"], dtype=f32)
    B = x.shape[0]
    NT = S // 128
    c = np.asarray(inputs["c"], f32)

    def kcl(w):
        return np.ascontiguousarray(w.reshape(KC, 128, -1).transpose(1, 0, 2))

    def rep(v, n=128):
        return np.ascontiguousarray(np.broadcast_to(np.asarray(v, f32).reshape(1, -1), (n, np.asarray(v).size)))

    shared = {
        "w_ada": kcl(np.asarray(inputs["w_ada"][0], f32)),
        "b_ada_col": np.ascontiguousarray(np.asarray(inputs["b_ada"][0], f32).reshape(48, 128).T),
        "b_ada_row": np.ascontiguousarray(np.asarray(inputs["b_ada"][0], f32).reshape(1, -1)),
        "w_in": kcl(np.asarray(inputs["w_in"][0], f32)),
        "lam_in": np.ascontiguousarray(np.concatenate([rep(inputs["lambda_q1"][0]), rep(inputs["lambda_k1"][0]),
                                                       rep(inputs["lambda_q2"][0]), rep(inputs["lambda_k2"][0])], axis=1)),
        "subln_g": rep(inputs["subln_g"][0]),
        "sinks": rep(inputs["sinks"][0]),
        "w_out": kcl(np.asarray(inputs["w_out"][0], f32)),
        "ln_rows": np.ascontiguousarray(np.stack([rep(inputs["ln1_g"][0]), rep(inputs["ln1_b"][0]),
                                                  rep(inputs["ln2_g"][0]), rep(inputs["ln2_b"][0])], axis=1)),
        "w_pq": kcl(np.asarray(inputs["w_pq"][0], f32)),
        "skT": np.ascontiguousarray(np.asarray(inputs["sub_keys"][0], f32).reshape(16, 128, 128).transpose(2, 0, 1)),
        "u_tab": np.ascontiguousarray(np.asarray(inputs["u_tab"][0], f32)),
        "v_tab": np.ascontiguousarray(np.asarray(inputs["v_tab"][0], f32)),
    }
    sl = _slopes()
    kk = np.arange(128)
    shared["identb"] = np.eye(128, dtype=f32).astype(ml_dtypes.bfloat16)
    shared["identf"] = np.eye(128, dtype=f32)
    shared["maskneg"] = np.where(kk[:, None] > kk[None, :], -30000.0, 0.0).astype(f32).astype(ml_dtypes.bfloat16)
    kpos = (np.arange(NT)[None, :] * 128 + kk[:, None]).astype(np.float64)
    shared["wtab"] = np.exp(sl[8:12][None, None, :].astype(np.float64) * (kpos[:, :, None] - (S - 1))).astype(f32)
    qi = np.arange(128)[:, None]
    kj = np.arange(256)[None, :]
    dist = qi - kj + 128
    valid = (dist >= 0) & (dist < 128)
    swab = np.where(valid[:, None, :], -sl[:8][None, :, None] * dist[:, None, :].astype(f32), f32(-1e30)).astype(f32)
    shared["swab"] = np.ascontiguousarray(swab)

    in_maps = []
    for core in range(NCORES):
        xs = x[core * NB:(core + 1) * NB]
        m = dict(shared)
        m["x_tok"] = np.ascontiguousarray(xs.reshape(NB * S, D))
        m["xT"] = np.ascontiguousarray(xs.reshape(NB, S, KC, 128).transpose(0, 3, 2, 1))
        m["cT"] = np.ascontiguousarray(c[core * NB:(core + 1) * NB].reshape(NB, KC, 128).transpose(2, 1, 0))
        in_maps.append(m)
    return in_maps


_CACHE = {}


def kernel(**inputs):
    x = np.asarray(inputs["x"])
    B, S, _ = x.shape
    NB = B // NCORES
    key = (NB, S)
    if key not in _CACHE:
        _CACHE[key] = build(NB, S)
    nc = _CACHE[key]
    in_maps = _host_layout(inputs, NB, S)
    res = run_bass_kernel_spmd(nc, in_maps, core_ids=list(range(NCORES)))
    outs = [np.asarray(res.results[cidx]["out"], np.float32).reshape(NB, S, D) for cidx in range(NCORES)]
    return np.concatenate(outs, axis=0)
```

```python
import math
from contextlib import ExitStack

import numpy as np
import ml_dtypes

import concourse.bass as bass
import concourse.mybir as mybir
from concourse.bass_utils import run_bass_kernel_spmd

F32 = mybir.dt.float32
BF16 = mybir.dt.bfloat16
I32 = mybir.dt.int32
U32 = mybir.dt.uint32
AF = mybir.ActivationFunctionType
ALU = mybir.AluOpType
AX = mybir.AxisListType

NCORES = 8
D = 1024
KC = D // 128
INW = 2304
NEXP = 16384
EPS = 1e-5
ALPHA = 2.0 ** 0.25
LAMBDA_INIT = 0.8 - 0.6 * math.exp(0.0)
NSLOT = 24


class Buf:
    __slots__ = ("name", "writer", "readers", "dsem", "dcnt")

    def __init__(self, name):
        self.name = name
        self.writer = None
        self.readers = []
        self.dsem = None
        self.dcnt = 0


class Emitter:
    ROT = 30000

    def __init__(self, nc, stack):
        self.nc = nc
        self.stack = stack
        self.eng = {"pe": nc.tensor, "act": nc.scalar, "dve": nc.vector,
                    "pool": nc.gpsimd, "sp": nc.sync}
        self.sem = {}
        self.cnt = {}
        self.own = {e: set() for e in self.eng}
        self.seen = {e: {} for e in self.eng}
        self.nsem = 0
        self.tags = []
        self.ninstr = {e: 0 for e in self.eng}
        for e in self.eng:
            self._newsem(e)

    def _alloc_sem(self, name):
        self.nsem += 1
        return self.stack.enter_context(self.nc.semaphore(name))

    def _newsem(self, e):
        self.sem[e] = self._alloc_sem(f"s_{e}_{self.nsem}")
        self.own[e].add(id(self.sem[e]))
        self.cnt[e] = 0

    def _wait(self, e, ev):
        sem, val = ev
        key = id(sem)
        if e == "pe" and key in self.own["pe"]:
            return
        if self.seen[e].get(key, 0) >= val:
            return
        self.seen[e][key] = val
        self.eng[e].wait_ge(sem, val)

    def _deps(self, e, reads, writes):
        for b in reads:
            if b.writer is not None:
                self._wait(e, b.writer)
        for b in writes:
            if b.writer is not None:
                self._wait(e, b.writer)
            for r in b.readers:
                self._wait(e, r)

    def _commit(self, ev, reads, writes):
        for b in reads:
            b.readers.append(ev)
            if len(b.readers) > 48:
                last = {}
                for s, v in b.readers:
                    k = id(s)
                    if k not in last or last[k][1] < v:
                        last[k] = (s, v)
                b.readers = list(last.values())
        for b in writes:
            b.writer = ev
            b.readers = []

    def op(self, e, fn, reads=(), writes=()):
        self._deps(e, reads, writes)
        if self.cnt[e] >= self.ROT:
            self._newsem(e)
        ins = fn(self.eng[e])
        self.cnt[e] += 1
        self.ninstr[e] += 1
        ins.then_inc(self.sem[e], 1)
        ev = (self.sem[e], self.cnt[e])
        self._commit(ev, reads, writes)
        return ev

    def dma(self, e, fn, tag, reads=(), writes=()):
        self._deps(e, reads, writes)
        if tag.dsem is None:
            tag.dsem = self._alloc_sem(f"d_{tag.name}")
            self.tags.append(tag)
        ins = fn(self.eng[e])
        tag.dcnt += 16
        self.ninstr[e] += 1
        ins.then_inc(tag.dsem, 16)
        ev = (tag.dsem, tag.dcnt)
        self._commit(ev, reads, writes)
        return ev

    def barrier(self):
        evs = [(self.sem[e2], self.cnt[e2]) for e2 in self.eng if self.cnt[e2] > 0]
        evs += [(t.dsem, t.dcnt) for t in self.tags if t.dcnt > 0]
        for e in self.eng:
            for sem, val in evs:
                key = id(sem)
                if self.seen[e].get(key, 0) >= val:
                    continue
                self.seen[e][key] = val
                self.eng[e].wait_ge(sem, val)


def _slopes():
    i = np.arange(1, 13, dtype=np.float32)
    return np.exp2(-8.0 * i / 12.0).astype(np.float32)


def build(NB, S):
    NT = S // 128
    NTOK = NB * S
    nc = bass.Bass("TRN2", target_bir_lowering=False)

    def din(name, shape, dt=F32):
        return nc.dram_tensor(name, list(shape), dt, kind="ExternalInput").ap()

    x_tok = din("x_tok", [NTOK, D])
    xT_d = din("xT", [NB, 128, KC, S])
    cT_d = din("cT", [128, KC, NB])
    wada_d = din("w_ada", [128, KC, 6 * D])
    bcol_d = din("b_ada_col", [128, 48])
    brow_d = din("b_ada_row", [1, 6 * D])
    win_d = din("w_in", [128, KC, INW])
    lam_d = din("lam_in", [128, 256])
    subg_d = din("subln_g", [128, 128])
    sinks_d = din("sinks", [128, 8])
    wout_d = din("w_out", [128, KC, D])
    lnr_d = din("ln_rows", [128, 4, D])
    wpq_d = din("w_pq", [128, KC, 2048])
    skT_d = din("skT", [128, 16, 128])
    utab_d = din("u_tab", [NEXP, D])
    vtab_d = din("v_tab", [NEXP, D])
    identb_d = din("identb", [128, 128], BF16)
    identf_d = din("identf", [128, 128])
    maskneg_d = din("maskneg", [128, 128], BF16)
    wtab_d = din("wtab", [128, NT, 4])
    swab_d = din("swab", [128, 8, 256])
    out_d = nc.dram_tensor("out", [NTOK, D], F32, kind="ExternalOutput").ap()
    x1s_d = nc.dram_tensor("x1s", [NTOK, D], F32, kind="Internal").ap()
    h2s_d = nc.dram_tensor("h2s", [NTOK, D], F32, kind="Internal").ap()
    rows_d = nc.dram_tensor("rows_s", [NB, 4, 128, D], F32, kind="Internal").ap()
    ubf_d = nc.dram_tensor("u_bf", [NEXP, D], BF16, kind="Internal").ap()
    vbf_d = nc.dram_tensor("v_bf", [NEXP, D], BF16, kind="Internal").ap()

    with ExitStack() as top:
        def SB(st, name, shape, dt=F32):
            return st.enter_context(nc.sbuf_tensor("s_" + name, list(shape), dt)), Buf(name)

        banks = []
        for i in range(8):
            t = top.enter_context(nc.psum_tensor(f"pb{i}", [128, 512], F32))
            banks.append((t, Buf(f"pb{i}")))
        colmod, b_colmod = SB(top, "colmod", [128, 16, NB])
        cst, b_cst = SB(top, "cst", [128, 8])
        top.enter_context(nc.Block())
        em = Emitter(nc, top)
        ctag = Buf("ctag")

        def load_consts(items):
            ev = None
            for (s_ap, d_ap, b) in items:
                ev = em.dma("sp", lambda e, s_ap=s_ap, d_ap=d_ap: e.dma_start(out=s_ap, in_=d_ap), ctag, writes=[b])
            for (_, _, b) in items:
                b.writer = ev

        em.op("dve", lambda e: e.memset(cst[:], 0.0), writes=[b_cst])
        em.op("dve", lambda e: e.memset(cst[:, 1:2], EPS), writes=[b_cst])

        with ExitStack() as p0:
            cT, b_cT = SB(p0, "cT", [128, KC, NB])
            siluT, b_siluT = SB(p0, "siluT", [128, KC, NB])
            bcol, b_bcol = SB(p0, "bcol", [128, 48])
            lam, b_lam = SB(p0, "lam", [128, 256])
            lj, b_lj = SB(p0, "lj", [128, 64])
            ls, b_ls = SB(p0, "ls", [128, 4])
            ones, b_ones = SB(p0, "ones", [128, 128])
            sbc, b_sbc = SB(p0, "sbc", [128, NB, KC, 128])
            wst = [SB(p0, f"wst{i}", [128, KC, 512]) for i in range(2)]
            brs = [SB(p0, f"brs{i}", [1, 512]) for i in range(2)]
            rst = [SB(p0, f"rst{i}", [128, 512]) for i in range(2)]
            load_consts([(cT[:], cT_d, b_cT), (bcol[:], bcol_d, b_bcol), (lam[:], lam_d, b_lam)])
            em.op("dve", lambda e: e.memset(ones[:], 1.0), writes=[b_ones])
            em.op("act", lambda e: e.activation(out=siluT[:], in_=cT[:], func=AF.Silu), reads=[b_cT], writes=[b_siluT])
            for t in range(2):
                em.op("dve", lambda e, t=t: e.scalar_tensor_tensor(
                    out=lj[:], in0=lam[:, t * 128:t * 128 + 64], scalar=1.0, in1=lam[:, t * 128 + 64:t * 128 + 128],
                    op0=ALU.mult, op1=ALU.mult, accum_out=ls[:, t:t + 1]), reads=[b_lam], writes=[b_lj, b_ls])
            em.op("act", lambda e: e.activation(out=ls[:, 2:4], in_=ls[:, 0:2], func=AF.Exp), reads=[b_ls], writes=[b_ls])
            em.op("dve", lambda e: e.scalar_tensor_tensor(out=cst[:, 0:1], in0=ls[:, 3:4], scalar=-LAMBDA_INIT, in1=ls[:, 2:3],
                                                           op0=ALU.add, op1=ALU.subtract), reads=[b_ls], writes=[b_cst])
            for b in range(NB):
                for kc in range(KC):
                    em.op("dve", lambda e, b=b, kc=kc: e.tensor_scalar(
                        out=sbc[:, b, kc, :], in0=ones[:], scalar1=siluT[:, kc, b:b + 1], scalar2=None, op0=ALU.mult),
                        reads=[b_ones, b_siluT], writes=[b_sbc])
            rr = 0
            for piece in range(12):
                (w, b_w) = wst[piece % 2]
                em.dma("sp", lambda e, w=w, piece=piece: e.dma_start(out=w[:], in_=wada_d[:, :, piece * 512:(piece + 1) * 512]),
                       b_w, writes=[b_w])
                if piece < 4:
                    for jb4 in range(4):
                        jb = piece * 4 + jb4
                        pt, b_pt = banks[rr % 8]; rr += 1
                        for kc in range(KC):
                            em.op("pe", lambda e, pt=pt, w=w, kc=kc, jb4=jb4: e.matmul(
                                pt[:, 0:NB], lhsT=w[:, kc, jb4 * 128:(jb4 + 1) * 128], rhs=siluT[:, kc, :],
                                start=(kc == 0), stop=(kc == KC - 1)), reads=[b_w, b_siluT], writes=[b_pt])
                        em.op("dve", lambda e, pt=pt, jb=jb: e.tensor_scalar(
                            out=colmod[:, jb, :], in0=pt[:, 0:NB], scalar1=bcol[:, jb:jb + 1],
                            scalar2=(1.0 if jb >= 8 else 0.0), op0=ALU.add, op1=ALU.add),
                            reads=[b_pt, b_bcol], writes=[b_colmod])
                else:
                    prm = (piece - 4) // 2
                    half = (piece - 4) % 2
                    (br, b_br) = brs[piece % 2]
                    em.dma("sp", lambda e, br=br, piece=piece: e.dma_start(out=br[:], in_=brow_d[:, piece * 512:(piece + 1) * 512]),
                           b_br, writes=[b_br])
                    for b in range(NB):
                        pt, b_pt = banks[rr % 8]; rr += 1
                        for kc in range(KC):
                            em.op("pe", lambda e, pt=pt, w=w, kc=kc, b=b: e.matmul(
                                pt[:, :], lhsT=sbc[:, b, kc, :], rhs=w[:, kc, :], start=(kc == 0), stop=False),
                                reads=[b_w, b_sbc], writes=[b_pt])
                        em.op("pe", lambda e, pt=pt, br=br: e.matmul(pt[:, :], lhsT=ones[0:1, :], rhs=br[0:1, :], start=False, stop=True),
                              reads=[b_br, b_ones], writes=[b_pt])
                        (r, b_r) = rst[(piece * NB + b) % 2]
                        em.op("act", lambda e, r=r, pt=pt, prm=prm: e.activation(
                            out=r[:], in_=pt[:, :], func=AF.Identity, bias=(cst[:, 2:3] if prm != 2 else ones[:, 0:1]), scale=1.0),
                            reads=[b_pt, b_cst, b_ones], writes=[b_r])
                        em.dma("sp", lambda e, r=r, b=b, prm=prm, half=half: e.dma_start(
                            out=rows_d[b, prm, :, half * 512:(half + 1) * 512], in_=r[:]), b_r, reads=[b_r])
            em.barrier()

        with ExitStack() as pa:
            win, b_win = SB(pa, "win", [128, KC, INW], BF16)
            wout, b_wout = SB(pa, "wout", [128, KC, D], BF16)
            identb, b_identb = SB(pa, "identb", [128, 128], BF16)
            maskneg, b_maskneg = SB(pa, "maskneg", [128, 128], BF16)
            wtab, b_wtab = SB(pa, "wtab", [128, NT, 4])
            swab, b_swab = SB(pa, "swab", [128, 8, 256])
            ln1, b_ln1 = SB(pa, "ln1", [128, 2, D])
            subg, b_subg = SB(pa, "subg", [128, 128])
            sinks, b_sinks = SB(pa, "sinks", [128, 8])
            rows, b_rows = SB(pa, "rows", [128, 3, D])
            dkT, _ = SB(pa, "dkT", [128, 4, S], BF16)
            dV, _ = SB(pa, "dV", [128, NT, 4, 130], BF16)
            skT, _ = SB(pa, "skTa", [64, 2, S], BF16)
            sv, _ = SB(pa, "sv", [128, NT, 128], BF16)
            b_kv = [Buf(f"kv{i}") for i in range(NT)]
            xTt = [SB(pa, f"xTt{i}", [128, KC, 128]) for i in range(2)]
            xt = [SB(pa, f"xt{i}", [128, D]) for i in range(2)]
            hT, b_hT = SB(pa, "hT", [128, KC, 128], BF16)
            dqT, b_dqT = SB(pa, "dqT", [128, 4, 128], BF16)
            sqT, b_sqT = SB(pa, "sqT", [64, 8, 128], BF16)
            PT = [SB(pa, f"PT{i}", [128, 512], BF16) for i in range(3)]
            od = [SB(pa, f"od{i}", [128, 128]) for i in range(2)]
            oj, b_oj = SB(pa, "oj", [128, 128], BF16)
            on, b_on = SB(pa, "on", [128, D], BF16)
            oT, b_oT = SB(pa, "oT", [128, KC, 128], BF16)
            ssb = [SB(pa, f"ssb{i}", [128, 256]) for i in range(2)]
            Psw = [SB(pa, f"Psw{i}", [128, 256], BF16) for i in range(2)]
            PTs = [SB(pa, f"PTs{i}", [128, 256], BF16) for i in range(2)]
            sm = [SB(pa, f"sm{i}", [128, 8]) for i in range(4)]
            tA, b_tA = SB(pa, "tA", [128, D])
            tB, b_tB = SB(pa, "tB", [128, D])
            x1o = [SB(pa, f"x1o{i}", [128, D]) for i in range(2)]
            h2o = [SB(pa, f"h2o{i}", [128, D]) for i in range(2)]
            st6, b_st6 = SB(pa, "st6", [128, 2, 6])
            wstg = [SB(pa, f"wstg{i}", [128, KC, 256]) for i in range(2)]
            cf = [(wstg[i][0][:].rearrange("p k c -> p (k c)").rearrange("p (i d) -> p i d", i=2), wstg[i][1]) for i in range(2)]
            cb = [SB(pa, f"cb{i}", [128, 2, D], BF16) for i in range(2)]
            conv = []
            for (src_d, dst_d) in ((utab_d, ubf_d), (vtab_d, vbf_d)):
                sv_ = src_d.rearrange("(p i) d -> p i d", p=128)
                dv_ = dst_d.rearrange("(p i) d -> p i d", p=128)
                for i0 in range(0, NEXP // 128, 2):
                    conv.append((sv_, dv_, i0))
            conv_n = [0]

            def conv_step():
                if conv_n[0] >= len(conv):
                    return
                (sv_, dv_, i0) = conv[conv_n[0]]
                (f_, b_f) = cf[conv_n[0] % 2]
                (h_, b_h) = cb[conv_n[0] % 2]
                conv_n[0] += 1
                em.dma("sp", lambda e: e.dma_start(out=f_, in_=sv_[:, i0:i0 + 2, :]), b_f, writes=[b_f])
                em.op("pool", lambda e: e.tensor_copy(out=h_[:], in_=f_), reads=[b_f], writes=[b_h])
                em.dma("sp", lambda e: e.dma_start(out=dv_[:, i0:i0 + 2, :], in_=h_[:]), b_h, reads=[b_h])

            load_consts([(identb[:], identb_d, b_identb), (maskneg[:], maskneg_d, b_maskneg), (wtab[:], wtab_d, b_wtab),
                         (swab[:], swab_d, b_swab), (ln1[:], lnr_d[:, 0:2, :], b_ln1), (subg[:], subg_d, b_subg),
                         (sinks[:], sinks_d, b_sinks)])
            em.op("dve", lambda e: e.tensor_scalar(out=subg[:], in0=subg[:], scalar1=(1.0 - LAMBDA_INIT), scalar2=None, op0=ALU.mult),
                  reads=[b_subg], writes=[b_subg])
            for pc in range(INW // 256 + D // 256):
                (wg, b_wg) = wstg[pc % 2]
                if pc < INW // 256:
                    src = win_d[:, :, pc * 256:(pc + 1) * 256]; dst = win[:, :, pc * 256:(pc + 1) * 256]; bd = b_win
                else:
                    q = pc - INW // 256
                    src = wout_d[:, :, q * 256:(q + 1) * 256]; dst = wout[:, :, q * 256:(q + 1) * 256]; bd = b_wout
                em.dma("sp", lambda e, wg=wg, src=src: e.dma_start(out=wg[:], in_=src), b_wg, writes=[b_wg])
                em.op("dve" if pc % 2 == 0 else "pool", lambda e, wg=wg, dst=dst: e.tensor_copy(out=dst, in_=wg[:]),
                      reads=[b_wg], writes=[bd])

            ACC = [banks[0], banks[1]]
            MIX = [banks[2], banks[3]]
            gen = [banks[4], banks[5], banks[6], banks[7]]
            gi = [0]

            def gbank():
                r = gen[gi[0] % 4]; gi[0] += 1
                return r

            def load_tile(g):
                b, i = divmod(g, NT)
                (xa, b_xa) = xTt[g % 2]
                (xb, b_xb) = xt[g % 2]
                em.dma("sp", lambda e: e.dma_start(out=xa[:], in_=xT_d[b, :, :, i * 128:(i + 1) * 128]), b_xa, writes=[b_xa])
                em.dma("sp", lambda e: e.dma_start(out=xb[:], in_=x_tok[g * 128:(g + 1) * 128, :]), b_xb, writes=[b_xb])

            cnt3 = [0, 0, 0]
            load_tile(0)
            for g in range(NB * NT):
                b, i = divmod(g, NT)
                if i == 0:
                    em.dma("sp", lambda e: e.dma_start(out=rows[:], in_=rows_d[b, 0:3, :, :].rearrange("r p d -> p r d")),
                           b_rows, writes=[b_rows])
                if g + 1 < NB * NT:
                    load_tile(g + 1)
                for _ in range(-(-len(conv) // (NB * NT))):
                    conv_step()
                (xa, b_xa) = xTt[g % 2]
                (xb, b_xb) = xt[g % 2]
                bkv = b_kv[i]
                for kc in range(KC):
                    em.op("act", lambda e, kc=kc: e.activation(out=hT[:, kc, :], in_=xa[:, kc, :], func=AF.Identity,
                                                                bias=colmod[:, kc, b:b + 1], scale=colmod[:, 8 + kc, b:b + 1]),
                          reads=[b_xa, b_colmod], writes=[b_hT])
                pq, b_pq = gbank()
                for h in range(4):
                    for kc in range(KC):
                        em.op("pe", lambda e, h=h, kc=kc: e.matmul(pq[:, h * 128:(h + 1) * 128], lhsT=win[:, kc, h * 128:(h + 1) * 128],
                                                                    rhs=hT[:, kc, :], start=(kc == 0), stop=(kc == KC - 1)),
                              reads=[b_win, b_hT], writes=[b_pq])
                em.op("dve", lambda e: e.tensor_copy(out=dqT[:].rearrange("p h t -> p (h t)"), in_=pq[:, :]), reads=[b_pq], writes=[b_dqT])
                pk, b_pk = gbank()
                for h in range(4):
                    for kc in range(KC):
                        em.op("pe", lambda e, h=h, kc=kc: e.matmul(pk[:, h * 128:(h + 1) * 128], lhsT=win[:, kc, 512 + h * 128:512 + (h + 1) * 128],
                                                                    rhs=hT[:, kc, :], start=(kc == 0), stop=(kc == KC - 1)),
                              reads=[b_win, b_hT], writes=[b_pk])
                em.op("act", lambda e: e.activation(out=dkT[:, :, i * 128:(i + 1) * 128], in_=pk[:, :].rearrange("p (h t) -> p h t", h=4), func=AF.Copy),
                      reads=[b_pk], writes=[bkv])
                for half in range(2):
                    psq, b_psq = gbank()
                    for hh in range(4):
                        hq = half * 4 + hh
                        for kc in range(KC):
                            em.op("pe", lambda e, hq=hq, hh=hh, kc=kc: e.matmul(
                                psq[0:64, hh * 128:(hh + 1) * 128], lhsT=win[:, kc, 1536 + hq * 64:1536 + (hq + 1) * 64],
                                rhs=hT[:, kc, :], start=(kc == 0), stop=(kc == KC - 1)), reads=[b_win, b_hT], writes=[b_psq])
                    em.op("dve", lambda e, half=half: e.tensor_copy(out=sqT[:, half * 4:(half + 1) * 4, :].rearrange("p h t -> p (h t)"), in_=psq[0:64, :]),
                          reads=[b_psq], writes=[b_sqT])
                psk, b_psk = gbank()
                for gk in range(2):
                    for kc in range(KC):
                        em.op("pe", lambda e, gk=gk, kc=kc: e.matmul(
                            psk[0:64, gk * 128:(gk + 1) * 128], lhsT=win[:, kc, 2048 + gk * 64:2048 + (gk + 1) * 64],
                            rhs=hT[:, kc, :], start=(kc == 0), stop=(kc == KC - 1)), reads=[b_win, b_hT], writes=[b_psk])
                for kc in range(KC):
                    em.op("pe", lambda e, kc=kc: e.matmul(psk[:, 256:384], lhsT=hT[:, kc, :], rhs=win[:, kc, 2176:2304],
                                                           start=(kc == 0), stop=(kc == KC - 1), skip_group_check=True),
                          reads=[b_win, b_hT], writes=[b_psk])
                em.op("act", lambda e: e.activation(out=skT[:, :, i * 128:(i + 1) * 128], in_=psk[0:64, 0:256].rearrange("p (h t) -> p h t", h=2), func=AF.Copy),
                      reads=[b_psk], writes=[bkv])
                em.op("act", lambda e: e.activation(out=sv[:, i, :], in_=psk[:, 256:384], func=AF.Copy), reads=[b_psk], writes=[bkv])
                pv, b_pv = gbank()
                for kc in range(KC):
                    em.op("pe", lambda e, kc=kc: e.matmul(pv[:, :], lhsT=hT[:, kc, :], rhs=win[:, kc, 1024:1536],
                                                           start=(kc == 0), stop=(kc == KC - 1)), reads=[b_win, b_hT], writes=[b_pv])
                for h in range(4):
                    em.op("act" if h % 2 == 0 else "dve",
                          (lambda e, h=h: e.activation(out=dV[:, i, h, 0:128], in_=pv[:, h * 128:(h + 1) * 128], func=AF.Identity, bias=cst[:, 2:3], scale=wtab[:, i, h:h + 1]))
                          if h % 2 == 0 else
                          (lambda e, h=h: e.tensor_scalar(out=dV[:, i, h, 0:128], in0=pv[:, h * 128:(h + 1) * 128], scalar1=wtab[:, i, h:h + 1], scalar2=None, op0=ALU.mult)),
                          reads=[b_pv, b_wtab], writes=[bkv])
                em.op("dve", lambda e: e.tensor_copy(out=dV[:, i, :, 128], in_=wtab[:, i, :]), reads=[b_wtab], writes=[bkv])

                for h in range(4):
                    for m in range(2):
                        acc, b_acc = ACC[m]
                        for g0 in range(0, i + 1, 4):
                            kbs = list(range(g0, min(g0 + 4, i + 1)))
                            sp_, b_sp = gbank()
                            for s_, kb in enumerate(kbs):
                                em.op("pe", lambda e, s_=s_, kb=kb: e.matmul(
                                    sp_[:, s_ * 128:(s_ + 1) * 128], lhsT=dkT[64 * m:64 * m + 64, h, kb * 128:(kb + 1) * 128],
                                    rhs=dqT[64 * m:64 * m + 64, h, :], start=True, stop=(kb != i)),
                                    reads=[b_kv[kb], b_dqT], writes=[b_sp])
                                if kb == i:
                                    em.op("pe", lambda e, s_=s_: e.matmul(sp_[:, s_ * 128:(s_ + 1) * 128], lhsT=identb[:, :], rhs=maskneg[:, :],
                                                                          start=False, stop=True),
                                          reads=[b_identb, b_maskneg], writes=[b_sp])
                            n = len(kbs) * 128
                            (pt_, b_pt_) = PT[cnt3[0] % 3]; cnt3[0] += 1
                            em.op("act", lambda e, n=n, pt_=pt_: e.activation(out=pt_[:, 0:n], in_=sp_[:, 0:n], func=AF.Exp, scale=0.125),
                                  reads=[b_sp], writes=[b_pt_])
                            for s_, kb in enumerate(kbs):
                                em.op("pe", lambda e, s_=s_, kb=kb, pt_=pt_: e.matmul(
                                    acc[:, 0:129], lhsT=pt_[:, s_ * 128:(s_ + 1) * 128], rhs=dV[:, kb, h, 0:129],
                                    start=(kb == 0), stop=(kb == i)), reads=[b_pt_, b_kv[kb]], writes=[b_acc])
                    (s4, b_s4) = sm[cnt3[1] % 4]; cnt3[1] += 1
                    (o1, b_o1) = od[0]
                    (o2, b_o2) = od[1]
                    a0, b_a0 = ACC[0]
                    a1, b_a1 = ACC[1]
                    em.op("dve", lambda e: e.reciprocal(out=s4[:, 0:1], in_=a0[:, 128:129]), reads=[b_a0], writes=[b_s4])
                    em.op("dve", lambda e: e.reciprocal(out=s4[:, 1:2], in_=a1[:, 128:129]), reads=[b_a1], writes=[b_s4])
                    em.op("dve", lambda e: e.tensor_tensor(out=s4[:, 2:3], in0=s4[:, 1:2], in1=cst[:, 0:1], op=ALU.mult), reads=[b_s4, b_cst], writes=[b_s4])
                    em.op("act", lambda e: e.activation(out=o1[:], in_=a0[:, 0:128], func=AF.Identity, bias=cst[:, 2:3], scale=s4[:, 0:1]), reads=[b_a0, b_s4], writes=[b_o1])
                    em.op("dve", lambda e: e.scalar_tensor_tensor(out=o2[:], in0=a1[:, 0:128], scalar=s4[:, 2:3], in1=o1[:], op0=ALU.mult, op1=ALU.add),
                          reads=[b_a1, b_s4, b_o1], writes=[b_o2])
                    em.op("dve", lambda e: e.scalar_tensor_tensor(out=oj[:], in0=o2[:], scalar=1.0, in1=o2[:], op0=ALU.mult, op1=ALU.mult, accum_out=s4[:, 3:4]),
                          reads=[b_o2], writes=[b_oj, b_s4])
                    em.op("act", lambda e: e.activation(out=s4[:, 4:5], in_=s4[:, 3:4], func=AF.Ln, bias=cst[:, 1:2], scale=1.0 / 128.0), reads=[b_s4, b_cst], writes=[b_s4])
                    em.op("act", lambda e: e.activation(out=s4[:, 5:6], in_=s4[:, 4:5], func=AF.Exp, scale=-0.5), reads=[b_s4], writes=[b_s4])
                    em.op("dve", lambda e, h=h: e.scalar_tensor_tensor(out=on[:, h * 128:(h + 1) * 128], in0=o2[:], scalar=s4[:, 5:6], in1=subg[:], op0=ALU.mult, op1=ALU.mult),
                          reads=[b_o2, b_s4, b_subg], writes=[b_on])

                for hq in range(8):
                    gk = hq // 4
                    nk = 256 if i > 0 else 128
                    k0 = (i - 1) * 128 if i > 0 else 0
                    kvdeps = [b_kv[i]] + ([b_kv[i - 1]] if i > 0 else [])
                    sp_, b_sp = gbank()
                    em.op("pe", lambda e, hq=hq, gk=gk, nk=nk, k0=k0: e.matmul(sp_[:, 0:nk], lhsT=sqT[:, hq, :], rhs=skT[:, gk, k0:k0 + nk], start=True, stop=True),
                          reads=[b_sqT] + kvdeps, writes=[b_sp])
                    (sb_, b_sb) = ssb[hq % 2]
                    (s4, b_s4) = sm[cnt3[1] % 4]; cnt3[1] += 1
                    em.op("dve", lambda e, hq=hq, nk=nk: e.scalar_tensor_tensor(out=sb_[:, 0:nk], in0=sp_[:, 0:nk], scalar=0.125, in1=swab[:, hq, 256 - nk:256],
                                                                              op0=ALU.mult, op1=ALU.add), reads=[b_sp, b_swab], writes=[b_sb])
                    em.op("dve", lambda e, nk=nk: e.tensor_reduce(out=s4[:, 0:1], in_=sb_[:, 0:nk], axis=AX.X, op=ALU.max), reads=[b_sb], writes=[b_s4])
                    em.op("dve", lambda e, hq=hq: e.tensor_scalar(out=s4[:, 1:2], in0=s4[:, 0:1], scalar1=sinks[:, hq:hq + 1], scalar2=-1.0, op0=ALU.max, op1=ALU.mult),
                          reads=[b_s4, b_sinks], writes=[b_s4])
                    (pw, b_pw) = Psw[hq % 2]
                    em.op("act", lambda e, nk=nk: e.activation(out=pw[:, 0:nk], in_=sb_[:, 0:nk], func=AF.Exp, bias=s4[:, 1:2], scale=1.0, accum_out=s4[:, 2:3]),
                          reads=[b_sb, b_s4], writes=[b_pw, b_s4])
                    em.op("act", lambda e, hq=hq: e.activation(out=s4[:, 3:4], in_=s4[:, 1:2], func=AF.Exp, bias=sinks[:, hq:hq + 1], scale=1.0),
                          reads=[b_s4, b_sinks], writes=[b_s4])
                    em.op("dve", lambda e: e.tensor_tensor(out=s4[:, 4:5], in0=s4[:, 2:3], in1=s4[:, 3:4], op=ALU.add), reads=[b_s4], writes=[b_s4])
                    em.op("dve", lambda e: e.reciprocal(out=s4[:, 5:6], in_=s4[:, 4:5]), reads=[b_s4], writes=[b_s4])
                    tp, b_tp = gbank()
                    tpv = tp[:, 0:128].bitcast(BF16)
                    for bl in range(nk // 128):
                        em.op("pe", lambda e, bl=bl: e.transpose(out=tpv[:, bl * 128:(bl + 1) * 128], in_=pw[:, bl * 128:(bl + 1) * 128], identity=identb[:]),
                              reads=[b_pw, b_identb], writes=[b_tp])
                    (pts, b_pts) = PTs[hq % 2]
                    em.op("act", lambda e, nk=nk: e.activation(out=pts[:, 0:nk], in_=tpv[:, 0:nk], func=AF.Copy), reads=[b_tp], writes=[b_pts])
                    for bl in range(nk // 128):
                        kt = (i - 1 + bl) if i > 0 else i
                        em.op("pe", lambda e, bl=bl, kt=kt, gk=gk, nk=nk: e.matmul(tp[:, 256:320], lhsT=pts[:, bl * 128:(bl + 1) * 128], rhs=sv[:, kt, gk * 64:(gk + 1) * 64],
                                                                                   start=(bl == 0), stop=(bl == nk // 128 - 1), skip_group_check=True),
                              reads=[b_pts] + kvdeps, writes=[b_tp])
                    em.op("act", lambda e, hq=hq: e.activation(out=on[:, 512 + hq * 64:512 + (hq + 1) * 64], in_=tp[:, 256:320], func=AF.Identity, bias=cst[:, 2:3], scale=s4[:, 5:6]),
                          reads=[b_tp, b_s4], writes=[b_on])

                tp, b_tp = gbank()
                tpv = tp[:, :].bitcast(BF16)
                for c in range(KC):
                    em.op("pe", lambda e, c=c: e.transpose(out=tpv[:, c * 128:(c + 1) * 128], in_=on[:, c * 128:(c + 1) * 128], identity=identb[:]),
                          reads=[b_on, b_identb], writes=[b_tp])
                em.op("dve", lambda e: e.tensor_copy(out=oT[:].rearrange("p c t -> p (c t)"), in_=tpv[:, :]), reads=[b_tp], writes=[b_oT])
                for half in range(2):
                    mx, b_mx = MIX[half]
                    for c in range(KC):
                        em.op("pe", lambda e, c=c, half=half, mx=mx: e.matmul(mx[:, :], lhsT=oT[:, c, :], rhs=wout[:, c, half * 512:(half + 1) * 512],
                                                                             start=(c == 0), stop=(c == KC - 1)), reads=[b_oT, b_wout], writes=[b_mx])
                for half in range(2):
                    mx, b_mx = MIX[half]
                    em.op("dve", lambda e, half=half, mx=mx: e.tensor_tensor(out=tA[:, half * 512:(half + 1) * 512], in0=mx[:, :], in1=rows[:, 0, half * 512:(half + 1) * 512], op=ALU.mult),
                          reads=[b_mx, b_rows], writes=[b_tA])
                em.op("dve", lambda e: e.scalar_tensor_tensor(out=tB[:], in0=xb[:], scalar=ALPHA, in1=tA[:], op0=ALU.mult, op1=ALU.add),
                      reads=[b_xb, b_tA], writes=[b_tB])
                (s4, b_s4) = sm[cnt3[1] % 4]; cnt3[1] += 1
                for c in range(2):
                    em.op("dve", lambda e, c=c: e.bn_stats(out=st6[:, c, :], in_=tB[:, c * 512:(c + 1) * 512]), reads=[b_tB], writes=[b_st6])
                em.op("dve", lambda e: e.bn_aggr(out=s4[:, 0:2], in_=st6[:].rearrange("p a b -> p (a b)")), reads=[b_st6], writes=[b_s4])
                em.op("act", lambda e: e.activation(out=s4[:, 2:3], in_=s4[:, 1:2], func=AF.Ln, bias=cst[:, 1:2], scale=1.0), reads=[b_s4, b_cst], writes=[b_s4])
                em.op("act", lambda e: e.activation(out=s4[:, 3:4], in_=s4[:, 2:3], func=AF.Exp, scale=-0.5), reads=[b_s4], writes=[b_s4])
                em.op("dve", lambda e: e.tensor_scalar(out=tA[:], in0=tB[:], scalar1=s4[:, 0:1], scalar2=s4[:, 3:4], op0=ALU.subtract, op1=ALU.mult),
                      reads=[b_tB, b_s4], writes=[b_tA])
                (x1, b_x1) = x1o[g % 2]
                (h2, b_h2) = h2o[g % 2]
                em.op("pool", lambda e: e.tensor_tensor(out=tB[:], in0=tA[:], in1=ln1[:, 0, :], op=ALU.mult), reads=[b_tA, b_ln1], writes=[b_tB])
                em.op("pool", lambda e: e.tensor_tensor(out=x1[:], in0=tB[:], in1=ln1[:, 1, :], op=ALU.add), reads=[b_tB, b_ln1], writes=[b_x1])
                em.dma("sp", lambda e: e.dma_start(out=x1s_d[g * 128:(g + 1) * 128, :], in_=x1[:]), b_x1, reads=[b_x1])
                em.op("pool", lambda e: e.tensor_tensor(out=tA[:], in0=x1[:], in1=rows[:, 2, :], op=ALU.mult), reads=[b_x1, b_rows], writes=[b_tA])
                em.op("pool", lambda e: e.tensor_tensor(out=h2[:], in0=tA[:], in1=rows[:, 1, :], op=ALU.add), reads=[b_tA, b_rows], writes=[b_h2])
                em.dma("sp", lambda e: e.dma_start(out=h2s_d[g * 128:(g + 1) * 128, :], in_=h2[:]), b_h2, reads=[b_h2])
            em.barrier()

        with ExitStack() as pb:
            wpq, b_wpq = SB(pb, "wpq", [128, KC, 2048])
            skTp, b_skTp = SB(pb, "skTp", [128, 16, 128])
            identf, b_identf = SB(pb, "identf", [128, 128])
            ln2, b_ln2 = SB(pb, "ln2", [128, 2, D])
            g2r, b_g2r = SB(pb, "g2r", [128, D])
            GB = [SB(pb, f"GB{i}", [128, D], BF16) for i in range(NSLOT)]
            x1t = [SB(pb, f"x1t{i}", [128, D]) for i in range(2)]
            h2t = [SB(pb, f"h2t{i}", [128, D]) for i in range(2)]
            h2T, b_h2T = SB(pb, "h2T", [128, KC, 128])
            qT, b_qT = SB(pb, "qT", [128, 16, 128])
            vals, b_vals = SB(pb, "vals", [128, 16, 16])
            idxs, b_idxs = SB(pb, "idxs", [128, 16, 16], U32)
            idxf, b_idxf = SB(pb, "idxf", [128, 16, 16])
            scw = [SB(pb, f"scw{i}", [128, 128]) for i in range(2)]
            cand = [SB(pb, f"cand{i}", [128, 256]) for i in range(2)]
            cidx = [SB(pb, f"cidx{i}", [128, 256]) for i in range(2)]
            cw = [SB(pb, f"cw{i}", [128, 256]) for i in range(2)]
            tops, b_tops = SB(pb, "tops", [128, 8, 16])
            gate = [SB(pb, f"gate{i}", [128, 8, 16]) for i in range(2)]
            eidf, b_eidf = SB(pb, "eidf", [128, 128])
            eid = [SB(pb, f"eid{i}", [128, 128], I32) for i in range(2)]
            eidT = [SB(pb, f"eidT{i}", [128, 128], I32) for i in range(2)]
            apre, b_apre = SB(pb, "apre", [128, 128])
            ga, b_ga = SB(pb, "ga", [128, 128])
            gaT, b_gaT = SB(pb, "gaT", [128, 128], BF16)
            junk, _ = SB(pb, "junk", [128, D], BF16)
            junk2, _ = SB(pb, "junk2", [128, 256], BF16)
            ffnT, b_ffnT = SB(pb, "ffnT", [128, KC, 128])
            wA, b_wA = SB(pb, "wA", [128, D])
            wB, b_wB = SB(pb, "wB", [128, D])
            outt = [SB(pb, f"outt{i}", [128, D]) for i in range(2)]
            st6b, b_st6b = SB(pb, "st6b", [128, 2, 6])
            smb = [SB(pb, f"smb{i}", [128, 16]) for i in range(2)]

            load_consts([(wpq[:, 0:4, :], wpq_d[:, 0:4, :], b_wpq), (wpq[:, 4:8, :], wpq_d[:, 4:8, :], b_wpq),
                         (skTp[:], skT_d, b_skTp), (identf[:], identf_d, b_identf), (ln2[:], lnr_d[:, 2:4, :], b_ln2)])
            VT = [banks[0], banks[1]]
            bi = [0]

            def nbank():
                r = banks[2 + bi[0] % 6]; bi[0] += 1
                return r

            def load_tile_b(g):
                (a, b_a) = x1t[g % 2]
                (h, b_h) = h2t[g % 2]
                em.dma("sp", lambda e: e.dma_start(out=h[:], in_=h2s_d[g * 128:(g + 1) * 128, :]), b_h, writes=[b_h])
                em.dma("sp", lambda e: e.dma_start(out=a[:], in_=x1s_d[g * 128:(g + 1) * 128, :]), b_a, writes=[b_a])

            def score(g):
                (hh, b_hh) = h2t[g % 2]
                for half in range(2):
                    tp, b_tp = nbank()
                    for c4 in range(4):
                        c = half * 4 + c4
                        em.op("pe", lambda e, c=c, c4=c4, tp=tp: e.transpose(out=tp[:, c4 * 128:(c4 + 1) * 128], in_=hh[:, c * 128:(c + 1) * 128], identity=identf[:]),
                              reads=[b_hh, b_identf], writes=[b_tp])
                    em.op("act", lambda e, tp=tp, half=half: e.activation(out=h2T[:, half * 4:(half + 1) * 4, :].rearrange("p c t -> p (c t)"), in_=tp[:, :], func=AF.Copy),
                          reads=[b_tp], writes=[b_h2T])
                for q4 in range(4):
                    pq, b_pq = nbank()
                    for c4 in range(4):
                        c16 = q4 * 4 + c4
                        for kc in range(KC):
                            em.op("pe", lambda e, c16=c16, c4=c4, kc=kc, pq=pq: e.matmul(pq[:, c4 * 128:(c4 + 1) * 128], lhsT=wpq[:, kc, c16 * 128:(c16 + 1) * 128],
                                                                                       rhs=h2T[:, kc, :], start=(kc == 0), stop=(kc == KC - 1)),
                                  reads=[b_wpq, b_h2T], writes=[b_pq])
                    em.op("act", lambda e, pq=pq, q4=q4: e.activation(out=qT[:, q4 * 4:(q4 + 1) * 4, :].rearrange("p c t -> p (c t)"), in_=pq[:, :], func=AF.Copy),
                          reads=[b_pq], writes=[b_qT])
                scb = []
                for q4 in range(4):
                    ps_, b_ps = nbank()
                    scb.append((ps_, b_ps))
                    for c4 in range(4):
                        c16 = q4 * 4 + c4
                        em.op("pe", lambda e, c16=c16, c4=c4, ps_=ps_: e.matmul(ps_[:, c4 * 128:(c4 + 1) * 128], lhsT=qT[:, c16, :], rhs=skTp[:, c16, :], start=True, stop=True),
                              reads=[b_qT, b_skTp], writes=[b_ps])
                return scb

            def topk(g, scb):
                for c16 in range(16):
                    ps_, b_ps = scb[c16 // 4]
                    src = ps_[:, (c16 % 4) * 128:(c16 % 4 + 1) * 128]
                    (sw, b_sw) = scw[c16 % 2]
                    em.op("dve", lambda e, c16=c16, src=src: e.max(out=vals[:, c16, 0:8], in_=src), reads=[b_ps], writes=[b_vals])
                    em.op("dve", lambda e, c16=c16, src=src: e.max_index(out=idxs[:, c16, 0:8], in_max=vals[:, c16, 0:8], in_values=src), reads=[b_ps, b_vals], writes=[b_idxs])
                    em.op("dve", lambda e, c16=c16, src=src, sw=sw: e.match_replace(out=sw[:], in_to_replace=vals[:, c16, 0:8], in_values=src, imm_value=-1e30),
                          reads=[b_ps, b_vals], writes=[b_sw])
                    em.op("dve", lambda e, c16=c16, sw=sw: e.max(out=vals[:, c16, 8:16], in_=sw[:]), reads=[b_sw], writes=[b_vals])
                    em.op("dve", lambda e, c16=c16, sw=sw: e.max_index(out=idxs[:, c16, 8:16], in_max=vals[:, c16, 8:16], in_values=sw[:]), reads=[b_sw, b_vals], writes=[b_idxs])
                em.op("dve", lambda e: e.tensor_copy(out=idxf[:], in_=idxs[:]), reads=[b_idxs], writes=[b_idxf])
                i4 = idxf[:].rearrange("p (h two) k -> p h two k", two=2)
                em.op("dve", lambda e: e.tensor_scalar(out=i4[:, :, 0, :], in0=i4[:, :, 0, :], scalar1=128.0, scalar2=None, op0=ALU.mult), reads=[b_idxf], writes=[b_idxf])
                for h in range(8):
                    (cd, b_cd) = cand[h % 2]
                    (ci, b_ci) = cidx[h % 2]
                    (cw_, b_cw) = cw[h % 2]
                    em.op("dve", lambda e, h=h, cd=cd: e.tensor_tensor(out=cd[:].rearrange("p (i j) -> p i j", i=16),
                                                                     in0=vals[:, 2 * h, :].unsqueeze(2).to_broadcast([128, 16, 16]),
                                                                     in1=vals[:, 2 * h + 1, :].unsqueeze(1).to_broadcast([128, 16, 16]), op=ALU.add),
                          reads=[b_vals], writes=[b_cd])
                    em.op("dve", lambda e, h=h, ci=ci: e.tensor_tensor(out=ci[:].rearrange("p (i j) -> p i j", i=16),
                                                                     in0=idxf[:, 2 * h, :].unsqueeze(2).to_broadcast([128, 16, 16]),
                                                                     in1=idxf[:, 2 * h + 1, :].unsqueeze(1).to_broadcast([128, 16, 16]), op=ALU.add),
                          reads=[b_idxf], writes=[b_ci])
                    em.op("dve", lambda e, h=h, cd=cd: e.max(out=tops[:, h, 0:8], in_=cd[:]), reads=[b_cd], writes=[b_tops])
                    em.op("dve", lambda e, h=h, cd=cd, cw_=cw_: e.match_replace(out=cw_[:], in_to_replace=tops[:, h, 0:8], in_values=cd[:], imm_value=-1e30),
                          reads=[b_cd, b_tops], writes=[b_cw])
                    em.op("dve", lambda e, h=h, cw_=cw_: e.max(out=tops[:, h, 8:16], in_=cw_[:]), reads=[b_cw], writes=[b_tops])
                    for k in range(16):
                        last = (k == 15)
                        em.op("dve", lambda e, h=h, k=k, cd=cd, ci=ci: e.scalar_tensor_tensor(
                            out=junk2[:], in0=cd[:], scalar=tops[:, h, k:k + 1], in1=ci[:], op0=ALU.is_equal, op1=ALU.mult,
                            accum_out=eidf[:, h * 16 + k:h * 16 + k + 1]),
                            reads=[b_cd, b_ci, b_tops], writes=([b_eidf] if (last and h == 7) else []))
                (ei, b_ei) = eid[g % 2]
                (eiT, b_eiT) = eidT[g % 2]
                em.op("dve", lambda e: e.tensor_scalar(out=eidf[:], in0=eidf[:], scalar1=float(NEXP - 1), scalar2=0.0, op0=ALU.min, op1=ALU.max),
                      reads=[b_eidf], writes=[b_eidf])
                em.op("dve", lambda e: e.tensor_copy(out=ei[:], in_=eidf[:]), reads=[b_eidf], writes=[b_ei])
                (gt, b_gt) = gate[g % 2]
                em.op("dve", lambda e: e.tensor_tensor(out=gt[:], in0=tops[:], in1=tops[:, :, 0:1].to_broadcast([128, 8, 16]), op=ALU.subtract),
                      reads=[b_tops], writes=[b_gt])
                em.op("act", lambda e: e.activation(out=gt[:], in_=gt[:], func=AF.Exp), reads=[b_gt], writes=[b_gt])
                (s8, b_s8) = smb[g % 2]
                em.op("dve", lambda e: e.tensor_reduce(out=s8[:, 0:8], in_=gt[:], axis=AX.X, op=ALU.add), reads=[b_gt], writes=[b_s8])
                em.op("dve", lambda e: e.reciprocal(out=s8[:, 8:16], in_=s8[:, 0:8]), reads=[b_s8], writes=[b_s8])
                em.op("dve", lambda e: e.tensor_tensor(out=gt[:], in0=gt[:], in1=s8[:, 8:16].unsqueeze(2).to_broadcast([128, 8, 16]), op=ALU.mult),
                      reads=[b_gt, b_s8], writes=[b_gt])
                tp, b_tp = nbank()
                em.op("pe", lambda e: e.transpose(out=tp[:, 0:128], in_=eidf[:], identity=identf[:]), reads=[b_eidf, b_identf], writes=[b_tp])
                em.op("act", lambda e: e.activation(out=eiT[:], in_=tp[:, 0:128], func=AF.Copy), reads=[b_tp], writes=[b_eiT])

            slot = [0]
            NG = NB * NT
            load_tile_b(0)
            scb_cur = score(0)
            topk(0, scb_cur)
            for g in range(NG):
                b, i = divmod(g, NT)
                if i == 0:
                    em.dma("sp", lambda e: e.dma_start(out=g2r[:], in_=rows_d[b, 3, :, :]), b_g2r, writes=[b_g2r])
                if g + 1 < NG:
                    load_tile_b(g + 1)
                    scb_next = score(g + 1)
                (xa, b_xa) = x1t[g % 2]
                (hh, b_hh) = h2t[g % 2]
                (ei, b_ei) = eid[g % 2]
                (eiT, b_eiT) = eidT[g % 2]
                (gt, b_gt) = gate[g % 2]
                (s8, b_s8) = smb[g % 2]
                for j in range(128):
                    (gb, b_gb) = GB[slot[0] % NSLOT]; slot[0] += 1
                    em.dma("pool", lambda e, j=j, gb=gb: e.indirect_dma_start(out=gb[:, :], out_offset=None, in_=ubf_d,
                                                                           in_offset=bass.IndirectOffsetOnAxis(ap=ei[:, j:j + 1], axis=0)),
                           b_gb, reads=[b_ei], writes=[b_gb])
                    em.op("dve", lambda e, j=j, gb=gb: e.scalar_tensor_tensor(out=junk[:], in0=gb[:], scalar=1.0, in1=hh[:], op0=ALU.mult, op1=ALU.mult,
                                                                            accum_out=apre[:, j:j + 1]),
                          reads=[b_gb, b_hh], writes=([b_apre] if j == 127 else []))
                em.op("act", lambda e: e.activation(out=ga[:], in_=apre[:], func=AF.Gelu), reads=[b_apre], writes=[b_ga])
                em.op("dve", lambda e: e.tensor_tensor(out=ga[:], in0=ga[:], in1=gt[:].rearrange("p h k -> p (h k)"), op=ALU.mult),
                      reads=[b_ga, b_gt], writes=[b_ga])
                tp, b_tp = nbank()
                em.op("pe", lambda e: e.transpose(out=tp[:, 0:128], in_=ga[:], identity=identf[:]), reads=[b_ga, b_identf], writes=[b_tp])
                em.op("act", lambda e: e.activation(out=gaT[:], in_=tp[:, 0:128], func=AF.Copy), reads=[b_tp], writes=[b_gaT])
                for t in range(128):
                    (gb, b_gb) = GB[slot[0] % NSLOT]; slot[0] += 1
                    em.dma("pool", lambda e, t=t, gb=gb: e.indirect_dma_start(out=gb[:, :], out_offset=None, in_=vbf_d,
                                                                           in_offset=bass.IndirectOffsetOnAxis(ap=eiT[:, t:t + 1], axis=0)),
                           b_gb, reads=[b_eiT], writes=[b_gb])
                    for c in range(KC):
                        vt, b_vt = VT[c // 4]
                        col = (c % 4) * 128 + t
                        em.op("pe", lambda e, c=c, t=t, gb=gb, vt=vt, col=col: e.matmul(vt[:, col:col + 1], lhsT=gb[:, c * 128:(c + 1) * 128], rhs=gaT[:, t:t + 1],
                                                                                      start=True, stop=True, skip_group_check=True),
                              reads=[b_gb, b_gaT], writes=[b_vt])
                if g + 1 < NG:
                    topk(g + 1, scb_next)
                for half in range(2):
                    vt, b_vt = VT[half]
                    em.op("act", lambda e, half=half, vt=vt: e.activation(out=ffnT[:, half * 4:(half + 1) * 4, :].rearrange("p c t -> p (c t)"), in_=vt[:, :], func=AF.Copy),
                          reads=[b_vt], writes=[b_ffnT])
                fts = []
                for half in range(2):
                    ft, b_ft = nbank()
                    fts.append((ft, b_ft))
                    for c4 in range(4):
                        c = half * 4 + c4
                        em.op("pe", lambda e, c=c, c4=c4, ft=ft: e.transpose(out=ft[:, c4 * 128:(c4 + 1) * 128], in_=ffnT[:, c, :], identity=identf[:]),
                              reads=[b_ffnT, b_identf], writes=[b_ft])
                for half in range(2):
                    ft, b_ft = fts[half]
                    em.op("dve", lambda e, half=half, ft=ft: e.tensor_tensor(out=wB[:, half * 512:(half + 1) * 512], in0=ft[:, :], in1=g2r[:, half * 512:(half + 1) * 512], op=ALU.mult),
                          reads=[b_ft, b_g2r], writes=[b_wB])
                em.op("dve", lambda e: e.scalar_tensor_tensor(out=wA[:], in0=xa[:], scalar=ALPHA, in1=wB[:], op0=ALU.mult, op1=ALU.add),
                      reads=[b_xa, b_wB], writes=[b_wA])
                for c in range(2):
                    em.op("dve", lambda e, c=c: e.bn_stats(out=st6b[:, c, :], in_=wA[:, c * 512:(c + 1) * 512]), reads=[b_wA], writes=[b_st6b])
                em.op("dve", lambda e: e.bn_aggr(out=s8[:, 0:2], in_=st6b[:].rearrange("p a b -> p (a b)")), reads=[b_st6b], writes=[b_s8])
                em.op("act", lambda e: e.activation(out=s8[:, 2:3], in_=s8[:, 1:2], func=AF.Ln, bias=cst[:, 1:2], scale=1.0), reads=[b_s8, b_cst], writes=[b_s8])
                em.op("act", lambda e: e.activation(out=s8[:, 3:4], in_=s8[:, 2:3], func=AF.Exp, scale=-0.5), reads=[b_s8], writes=[b_s8])
                em.op("dve", lambda e: e.tensor_scalar(out=wB[:], in0=wA[:], scalar1=s8[:, 0:1], scalar2=s8[:, 3:4], op0=ALU.subtract, op1=ALU.mult),
                      reads=[b_wA, b_s8], writes=[b_wB])
                (ot, b_ot) = outt[g % 2]
                em.op("dve", lambda e: e.tensor_tensor(out=wA[:], in0=wB[:], in1=ln2[:, 0, :], op=ALU.mult), reads=[b_wB, b_ln2], writes=[b_wA])
                em.op("dve", lambda e: e.tensor_tensor(out=ot[:], in0=wA[:], in1=ln2[:, 1, :], op=ALU.add), reads=[b_wA, b_ln2], writes=[b_ot])
                em.dma("sp", lambda e: e.dma_start(out=out_d[g * 128:(g + 1) * 128, :], in_=ot[:]), b_ot, reads=[b_ot])
                if g + 1 < NG:
                    scb_cur = scb_next
            em.barrier()
        build.info = dict(ninstr=dict(em.ninstr), nsem=em.nsem)
    return nc


def _host_layout(inputs, NB, S):
    f32 = np.float32
    x = np.ascontiguousarray(inputs["x"], dtype=f32)
    B = x.shape[0]
    NT = S // 128
    c = np.asarray(inputs["c"], f32)

    def kcl(w):
        return np.ascontiguousarray(w.reshape(KC, 128, -1).transpose(1, 0, 2))

    def rep(v, n=128):
        return np.ascontiguousarray(np.broadcast_to(np.asarray(v, f32).reshape(1, -1), (n, np.asarray(v).size)))

    shared = {
        "w_ada": kcl(np.asarray(inputs["w_ada"][0], f32)),
        "b_ada_col": np.ascontiguousarray(np.asarray(inputs["b_ada"][0], f32).reshape(48, 128).T),
        "b_ada_row": np.ascontiguousarray(np.asarray(inputs["b_ada"][0], f32).reshape(1, -1)),
        "w_in": kcl(np.asarray(inputs["w_in"][0], f32)),
        "lam_in": np.ascontiguousarray(np.concatenate([rep(inputs["lambda_q1"][0]), rep(inputs["lambda_k1"][0]),
                                                       rep(inputs["lambda_q2"][0]), rep(inputs["lambda_k2"][0])], axis=1)),
        "subln_g": rep(inputs["subln_g"][0]),
        "sinks": rep(inputs["sinks"][0]),
        "w_out": kcl(np.asarray(inputs["w_out"][0], f32)),
        "ln_rows": np.ascontiguousarray(np.stack([rep(inputs["ln1_g"][0]), rep(inputs["ln1_b"][0]),
                                                  rep(inputs["ln2_g"][0]), rep(inputs["ln2_b"][0])], axis=1)),
        "w_pq": kcl(np.asarray(inputs["w_pq"][0], f32)),
        "skT": np.ascontiguousarray(np.asarray(inputs["sub_keys"][0], f32).reshape(16, 128, 128).transpose(2, 0, 1)),
        "u_tab": np.ascontiguousarray(np.asarray(inputs["u_tab"][0], f32)),
        "v_tab": np.ascontiguousarray(np.asarray(inputs["v_tab"][0], f32)),
    }
    sl = _slopes()
    kk = np.arange(128)
    shared["identb"] = np.eye(128, dtype=f32).astype(ml_dtypes.bfloat16)
    shared["identf"] = np.eye(128, dtype=f32)
    shared["maskneg"] = np.where(kk[:, None] > kk[None, :], -30000.0, 0.0).astype(f32).astype(ml_dtypes.bfloat16)
    kpos = (np.arange(NT)[None, :] * 128 + kk[:, None]).astype(np.float64)
    shared["wtab"] = np.exp(sl[8:12][None, None, :].astype(np.float64) * (kpos[:, :, None] - (S - 1))).astype(f32)
    qi = np.arange(128)[:, None]
    kj = np.arange(256)[None, :]
    dist = qi - kj + 128
    valid = (dist >= 0) & (dist < 128)
    swab = np.where(valid[:, None, :], -sl[:8][None, :, None] * dist[:, None, :].astype(f32), f32(-1e30)).astype(f32)
    shared["swab"] = np.ascontiguousarray(swab)

    in_maps = []
    for core in range(NCORES):
        xs = x[core * NB:(core + 1) * NB]
        m = dict(shared)
        m["x_tok"] = np.ascontiguousarray(xs.reshape(NB * S, D))
        m["xT"] = np.ascontiguousarray(xs.reshape(NB, S, KC, 128).transpose(0, 3, 2, 1))
        m["cT"] = np.ascontiguousarray(c[core * NB:(core + 1) * NB].reshape(NB, KC, 128).transpose(2, 1, 0))
        in_maps.append(m)
    return in_maps


_CACHE = {}


def kernel(**inputs):
    x = np.asarray(inputs["x"])
    B, S, _ = x.shape
    NB = B // NCORES
    key = (NB, S)
    if key not in _CACHE:
        _CACHE[key] = build(NB, S)
    nc = _CACHE[key]
    in_maps = _host_layout(inputs, NB, S)
    res = run_bass_kernel_spmd(nc, in_maps, core_ids=list(range(NCORES)))
    outs = [np.asarray(res.results[cidx]["out"], np.float32).reshape(NB, S, D) for cidx in range(NCORES)]
    return np.concatenate(outs, axis=0)
```

```python
import math
from contextlib import ExitStack

import numpy as np
import ml_dtypes

import concourse.bass as bass
import concourse.mybir as mybir
from concourse.bass_utils import run_bass_kernel_spmd

F32 = mybir.dt.float32
BF16 = mybir.dt.bfloat16
I32 = mybir.dt.int32
U32 = mybir.dt.uint32
AF = mybir.ActivationFunctionType
ALU = mybir.AluOpType
AX = mybir.AxisListType

NCORES = 8
D = 1024
KC = D // 128
INW = 2304
NEXP = 16384
EPS = 1e-5
ALPHA = 2.0 ** 0.25
LAMBDA_INIT = 0.8 - 0.6 * math.exp(0.0)
NSLOT = 24


class Buf:
    __slots__ = ("name", "writer", "readers", "dsem", "dcnt")

    def __init__(self, name):
        self.name = name
        self.writer = None
        self.readers = []
        self.dsem = None
        self.dcnt = 0


class Emitter:
    ROT = 30000

    def __init__(self, nc, stack):
        self.nc = nc
        self.stack = stack
        self.eng = {"pe": nc.tensor, "act": nc.scalar, "dve": nc.vector,
                    "pool": nc.gpsimd, "sp": nc.sync}
        self.sem = {}
        self.cnt = {}
        self.own = {e: set() for e in self.eng}
        self.seen = {e: {} for e in self.eng}
        self.nsem = 0
        self.tags = []
        self.ninstr = {e: 0 for e in self.eng}
        for e in self.eng:
            self._newsem(e)

    def _alloc_sem(self, name):
        self.nsem += 1
        return self.stack.enter_context(self.nc.semaphore(name))

    def _newsem(self, e):
        self.sem[e] = self._alloc_sem(f"s_{e}_{self.nsem}")
        self.own[e].add(id(self.sem[e]))
        self.cnt[e] = 0

    def _wait(self, e, ev):
        sem, val = ev
        key = id(sem)
        if e == "pe" and key in self.own["pe"]:
            return
        if self.seen[e].get(key, 0) >= val:
            return
        self.seen[e][key] = val
        self.eng[e].wait_ge(sem, val)

    def _deps(self, e, reads, writes):
        for b in reads:
            if b.writer is not None:
                self._wait(e, b.writer)
        for b in writes:
            if b.writer is not None:
                self._wait(e, b.writer)
            for r in b.readers:
                self._wait(e, r)

    def _commit(self, ev, reads, writes):
        for b in reads:
            b.readers.append(ev)
            if len(b.readers) > 48:
                last = {}
                for s, v in b.readers:
                    k = id(s)
                    if k not in last or last[k][1] < v:
                        last[k] = (s, v)
                b.readers = list(last.values())
        for b in writes:
            b.writer = ev
            b.readers = []

    def op(self, e, fn, reads=(), writes=()):
        self._deps(e, reads, writes)
        if self.cnt[e] >= self.ROT:
            self._newsem(e)
        ins = fn(self.eng[e])
        self.cnt[e] += 1
        self.ninstr[e] += 1
        ins.then_inc(self.sem[e], 1)
        ev = (self.sem[e], self.cnt[e])
        self._commit(ev, reads, writes)
        return ev

    def dma(self, e, fn, tag, reads=(), writes=()):
        self._deps(e, reads, writes)
        if tag.dsem is None:
            tag.dsem = self._alloc_sem(f"d_{tag.name}")
            self.tags.append(tag)
        ins = fn(self.eng[e])
        tag.dcnt += 16
        self.ninstr[e] += 1
        ins.then_inc(tag.dsem, 16)
        ev = (tag.dsem, tag.dcnt)
        self._commit(ev, reads, writes)
        return ev

    def barrier(self):
        evs = [(self.sem[e2], self.cnt[e2]) for e2 in self.eng if self.cnt[e2] > 0]
        evs += [(t.dsem, t.dcnt) for t in self.tags if t.dcnt > 0]
        for e in self.eng:
            for sem, val in evs:
                key = id(sem)
                if self.seen[e].get(key, 0) >= val:
                    continue
                self.seen[e][key] = val
                self.eng[e].wait_ge(sem, val)


def _slopes():
    i = np.arange(1, 13, dtype=np.float32)
    return np.exp2(-8.0 * i / 12.0).astype(np.float32)


def build(NB, S):
    NT = S // 128
    NTOK = NB * S
    nc = bass.Bass("TRN2", target_bir_lowering=False)

    def din(name, shape, dt=F32):
        return nc.dram_tensor(name, list(shape), dt, kind="ExternalInput").ap()

    x_tok = din("x_tok", [NTOK, D])
    xT_d = din("xT", [NB, 128, KC, S])
    cT_d = din("cT", [128, KC, NB])
    wada_d = din("w_ada", [128, KC, 6 * D])
    bcol_d = din("b_ada_col", [128, 48])
    brow_d = din("b_ada_row", [1, 6 * D])
    win_d = din("w_in", [128, KC, INW])
    lam_d = din("lam_in", [128, 256])
    subg_d = din("subln_g", [128, 128])
    sinks_d = din("sinks", [128, 8])
    wout_d = din("w_out", [128, KC, D])
    lnr_d = din("ln_rows", [128, 4, D])
    wpq_d = din("w_pq", [128, KC, 2048])
    skT_d = din("skT", [128, 16, 128])
    utab_d = din("u_tab", [NEXP, D])
    vtab_d = din("v_tab", [NEXP, D])
    identb_d = din("identb", [128, 128], BF16)
    identf_d = din("identf", [128, 128])
    maskneg_d = din("maskneg", [128, 128], BF16)
    wtab_d = din("wtab", [128, NT, 4])
    swab_d = din("swab", [128, 8, 256])
    out_d = nc.dram_tensor("out", [NTOK, D], F32, kind="ExternalOutput").ap()
    x1s_d = nc.dram_tensor("x1s", [NTOK, D], F32, kind="Internal").ap()
    h2s_d = nc.dram_tensor("h2s", [NTOK, D], F32, kind="Internal").ap()
    rows_d = nc.dram_tensor("rows_s", [NB, 4, 128, D], F32, kind="Internal").ap()
    ubf_d = nc.dram_tensor("u_bf", [NEXP, D], BF16, kind="Internal").ap()
    vbf_d = nc.dram_tensor("v_bf", [NEXP, D], BF16, kind="Internal").ap()

    with ExitStack() as top:
        def SB(st, name, shape, dt=F32):
            return st.enter_context(nc.sbuf_tensor("s_" + name, list(shape), dt)), Buf(name)

        banks = []
        for i in range(8):
            t = top.enter_context(nc.psum_tensor(f"pb{i}", [128, 512], F32))
            banks.append((t, Buf(f"pb{i}")))
        colmod, b_colmod = SB(top, "colmod", [128, 16, NB])
        cst, b_cst = SB(top, "cst", [128, 8])
        top.enter_context(nc.Block())
        em = Emitter(nc, top)
        ctag = Buf("ctag")

        def load_consts(items):
            ev = None
            for (s_ap, d_ap, b) in items:
                ev = em.dma("sp", lambda e, s_ap=s_ap, d_ap=d_ap: e.dma_start(out=s_ap, in_=d_ap), ctag, writes=[b])
            for (_, _, b) in items:
                b.writer = ev

        em.op("dve", lambda e: e.memset(cst[:], 0.0), writes=[b_cst])
        em.op("dve", lambda e: e.memset(cst[:, 1:2], EPS), writes=[b_cst])

        with ExitStack() as p0:
            cT, b_cT = SB(p0, "cT", [128, KC, NB])
            siluT, b_siluT = SB(p0, "siluT", [128, KC, NB])
            bcol, b_bcol = SB(p0, "bcol", [128, 48])
            lam, b_lam = SB(p0, "lam", [128, 256])
            lj, b_lj = SB(p0, "lj", [128, 64])
            ls, b_ls = SB(p0, "ls", [128, 4])
            ones, b_ones = SB(p0, "ones", [128, 128])
            sbc, b_sbc = SB(p0, "sbc", [128, NB, KC, 128])
            wst = [SB(p0, f"wst{i}", [128, KC, 512]) for i in range(2)]
            brs = [SB(p0, f"brs{i}", [1, 512]) for i in range(2)]
            rst = [SB(p0, f"rst{i}", [128, 512]) for i in range(2)]
            load_consts([(cT[:], cT_d, b_cT), (bcol[:], bcol_d, b_bcol), (lam[:], lam_d, b_lam)])
            em.op("dve", lambda e: e.memset(ones[:], 1.0), writes=[b_ones])
            em.op("act", lambda e: e.activation(out=siluT[:], in_=cT[:], func=AF.Silu), reads=[b_cT], writes=[b_siluT])
            for t in range(2):
                em.op("dve", lambda e, t=t: e.scalar_tensor_tensor(
                    out=lj[:], in0=lam[:, t * 128:t * 128 + 64], scalar=1.0, in1=lam[:, t * 128 + 64:t * 128 + 128],
                    op0=ALU.mult, op1=ALU.mult, accum_out=ls[:, t:t + 1]), reads=[b_lam], writes=[b_lj, b_ls])
            em.op("act", lambda e: e.activation(out=ls[:, 2:4], in_=ls[:, 0:2], func=AF.Exp), reads=[b_ls], writes=[b_ls])
            em.op("dve", lambda e: e.scalar_tensor_tensor(out=cst[:, 0:1], in0=ls[:, 3:4], scalar=-LAMBDA_INIT, in1=ls[:, 2:3],
                                                           op0=ALU.add, op1=ALU.subtract), reads=[b_ls], writes=[b_cst])
            for b in range(NB):
                for kc in range(KC):
                    em.op("dve", lambda e, b=b, kc=kc: e.tensor_scalar(
                        out=sbc[:, b, kc, :], in0=ones[:], scalar1=siluT[:, kc, b:b + 1], scalar2=None, op0=ALU.mult),
                        reads=[b_ones, b_siluT], writes=[b_sbc])
            rr = 0
            for piece in range(12):
                (w, b_w) = wst[piece % 2]
                em.dma("sp", lambda e, w=w, piece=piece: e.dma_start(out=w[:], in_=wada_d[:, :, piece * 512:(piece + 1) * 512]),
                       b_w, writes=[b_w])
                if piece < 4:
                    for jb4 in range(4):
                        jb = piece * 4 + jb4
                        pt, b_pt = banks[rr % 8]; rr += 1
                        for kc in range(KC):
                            em.op("pe", lambda e, pt=pt, w=w, kc=kc, jb4=jb4: e.matmul(
                                pt[:, 0:NB], lhsT=w[:, kc, jb4 * 128:(jb4 + 1) * 128], rhs=siluT[:, kc, :],
                                start=(kc == 0), stop=(kc == KC - 1)), reads=[b_w, b_siluT], writes=[b_pt])
                        em.op("dve", lambda e, pt=pt, jb=jb: e.tensor_scalar(
                            out=colmod[:, jb, :], in0=pt[:, 0:NB], scalar1=bcol[:, jb:jb + 1],
                            scalar2=(1.0 if jb >= 8 else 0.0), op0=ALU.add, op1=ALU.add),
                            reads=[b_pt, b_bcol], writes=[b_colmod])
                else:
                    prm = (piece - 4) // 2
                    half = (piece - 4) % 2
                    (br, b_br) = brs[piece % 2]
                    em.dma("sp", lambda e, br=br, piece=piece: e.dma_start(out=br[:], in_=brow_d[:, piece * 512:(piece + 1) * 512]),
                           b_br, writes=[b_br])
                    for b in range(NB):
                        pt, b_pt = banks[rr % 8]; rr += 1
                        for kc in range(KC):
                            em.op("pe", lambda e, pt=pt, w=w, kc=kc, b=b: e.matmul(
                                pt[:, :], lhsT=sbc[:, b, kc, :], rhs=w[:, kc, :], start=(kc == 0), stop=False),
                                reads=[b_w, b_sbc], writes=[b_pt])
                        em.op("pe", lambda e, pt=pt, br=br: e.matmul(pt[:, :], lhsT=ones[0:1, :], rhs=br[0:1, :], start=False, stop=True),
                              reads=[b_br, b_ones], writes=[b_pt])
                        (r, b_r) = rst[(piece * NB + b) % 2]
                        em.op("act", lambda e, r=r, pt=pt, prm=prm: e.activation(
                            out=r[:], in_=pt[:, :], func=AF.Identity, bias=(cst[:, 2:3] if prm != 2 else ones[:, 0:1]), scale=1.0),
                            reads=[b_pt, b_cst, b_ones], writes=[b_r])
                        em.dma("sp", lambda e, r=r, b=b, prm=prm, half=half: e.dma_start(
                            out=rows_d[b, prm, :, half * 512:(half + 1) * 512], in_=r[:]), b_r, reads=[b_r])
            em.barrier()

        with ExitStack() as pa:
            win, b_win = SB(pa, "win", [128, KC, INW], BF16)
            wout, b_wout = SB(pa, "wout", [128, KC, D], BF16)
            identb, b_identb = SB(pa, "identb", [128, 128], BF16)
            maskneg, b_maskneg = SB(pa, "maskneg", [128, 128], BF16)
            wtab, b_wtab = SB(pa, "wtab", [128, NT, 4])
            swab, b_swab = SB(pa, "swab", [128, 8, 256])
            ln1, b_ln1 = SB(pa, "ln1", [128, 2, D])
            subg, b_subg = SB(pa, "subg", [128, 128])
            sinks, b_sinks = SB(pa, "sinks", [128, 8])
            rows, b_rows = SB(pa, "rows", [128, 3, D])
            dkT, _ = SB(pa, "dkT", [128, 4, S], BF16)
            dV, _ = SB(pa, "dV", [128, NT, 4, 130], BF16)
            skT, _ = SB(pa, "skTa", [64, 2, S], BF16)
            sv, _ = SB(pa, "sv", [128, NT, 128], BF16)
            b_kv = [Buf(f"kv{i}") for i in range(NT)]
            xTt = [SB(pa, f"xTt{i}", [128, KC, 128]) for i in range(2)]
            xt = [SB(pa, f"xt{i}", [128, D]) for i in range(2)]
            hT, b_hT = SB(pa, "hT", [128, KC, 128], BF16)
            dqT, b_dqT = SB(pa, "dqT", [128, 4, 128], BF16)
            sqT, b_sqT = SB(pa, "sqT", [64, 8, 128], BF16)
            PT = [SB(pa, f"PT{i}", [128, 512], BF16) for i in range(3)]
            od = [SB(pa, f"od{i}", [128, 128]) for i in range(2)]
            oj, b_oj = SB(pa, "oj", [128, 128], BF16)
            on, b_on = SB(pa, "on", [128, D], BF16)
            oT, b_oT = SB(pa, "oT", [128, KC, 128], BF16)
            ssb = [SB(pa, f"ssb{i}", [128, 256]) for i in range(2)]
            Psw = [SB(pa, f"Psw{i}", [128, 256], BF16) for i in range(2)]
            PTs = [SB(pa, f"PTs{i}", [128, 256], BF16) for i in range(2)]
            sm = [SB(pa, f"sm{i}", [128, 8]) for i in range(4)]
            tA, b_tA = SB(pa, "tA", [128, D])
            tB, b_tB = SB(pa, "tB", [128, D])
            x1o = [SB(pa, f"x1o{i}", [128, D]) for i in range(2)]
            h2o = [SB(pa, f"h2o{i}", [128, D]) for i in range(2)]
            st6, b_st6 = SB(pa, "st6", [128, 2, 6])
            wstg = [SB(pa, f"wstg{i}", [128, KC, 256]) for i in range(2)]
            cf = [(wstg[i][0][:].rearrange("p k c -> p (k c)").rearrange("p (i d) -> p i d", i=2), wstg[i][1]) for i in range(2)]
            cb = [SB(pa, f"cb{i}", [128, 2, D], BF16) for i in range(2)]
            conv = []
            for (src_d, dst_d) in ((utab_d, ubf_d), (vtab_d, vbf_d)):
                sv_ = src_d.rearrange("(p i) d -> p i d", p=128)
                dv_ = dst_d.rearrange("(p i) d -> p i d", p=128)
                for i0 in range(0, NEXP // 128, 2):
                    conv.append((sv_, dv_, i0))
            conv_n = [0]

            def conv_step():
                if conv_n[0] >= len(conv):
                    return
                (sv_, dv_, i0) = conv[conv_n[0]]
                (f_, b_f) = cf[conv_n[0] % 2]
                (h_, b_h) = cb[conv_n[0] % 2]
                conv_n[0] += 1
                em.dma("sp", lambda e: e.dma_start(out=f_, in_=sv_[:, i0:i0 + 2, :]), b_f, writes=[b_f])
                em.op("pool", lambda e: e.tensor_copy(out=h_[:], in_=f_), reads=[b_f], writes=[b_h])
                em.dma("sp", lambda e: e.dma_start(out=dv_[:, i0:i0 + 2, :], in_=h_[:]), b_h, reads=[b_h])

            load_consts([(identb[:], identb_d, b_identb), (maskneg[:], maskneg_d, b_maskneg), (wtab[:], wtab_d, b_wtab),
                         (swab[:], swab_d, b_swab), (ln1[:], lnr_d[:, 0:2, :], b_ln1), (subg[:], subg_d, b_subg),
                         (sinks[:], sinks_d, b_sinks)])
            em.op("dve", lambda e: e.tensor_scalar(out=subg[:], in0=subg[:], scalar1=(1.0 - LAMBDA_INIT), scalar2=None, op0=ALU.mult),
                  reads=[b_subg], writes=[b_subg])
            for pc in range(INW // 256 + D // 256):
                (wg, b_wg) = wstg[pc % 2]
                if pc < INW // 256:
                    src = win_d[:, :, pc * 256:(pc + 1) * 256]; dst = win[:, :, pc * 256:(pc + 1) * 256]; bd = b_win
                else:
                    q = pc - INW // 256
                    src = wout_d[:, :, q * 256:(q + 1) * 256]; dst = wout[:, :, q * 256:(q + 1) * 256]; bd = b_wout
                em.dma("sp", lambda e, wg=wg, src=src: e.dma_start(out=wg[:], in_=src), b_wg, writes=[b_wg])
                em.op("dve" if pc % 2 == 0 else "pool", lambda e, wg=wg, dst=dst: e.tensor_copy(out=dst, in_=wg[:]),
                      reads=[b_wg], writes=[bd])

            ACC = [banks[0], banks[1]]
            MIX = [banks[2], banks[3]]
            gen = [banks[4], banks[5], banks[6], banks[7]]
            gi = [0]

            def gbank():
                r = gen[gi[0] % 4]; gi[0] += 1
                return r

            def load_tile(g):
                b, i = divmod(g, NT)
                (xa, b_xa) = xTt[g % 2]
                (xb, b_xb) = xt[g % 2]
                em.dma("sp", lambda e: e.dma_start(out=xa[:], in_=xT_d[b, :, :, i * 128:(i + 1) * 128]), b_xa, writes=[b_xa])
                em.dma("sp", lambda e: e.dma_start(out=xb[:], in_=x_tok[g * 128:(g + 1) * 128, :]), b_xb, writes=[b_xb])

            cnt3 = [0, 0, 0]
            load_tile(0)
            for g in range(NB * NT):
                b, i = divmod(g, NT)
                if i == 0:
                    em.dma("sp", lambda e: e.dma_start(out=rows[:], in_=rows_d[b, 0:3, :, :].rearrange("r p d -> p r d")),
                           b_rows, writes=[b_rows])
                if g + 1 < NB * NT:
                    load_tile(g + 1)
                for _ in range(-(-len(conv) // (NB * NT))):
                    conv_step()
                (xa, b_xa) = xTt[g % 2]
                (xb, b_xb) = xt[g % 2]
                bkv = b_kv[i]
                for kc in range(KC):
                    em.op("act", lambda e, kc=kc: e.activation(out=hT[:, kc, :], in_=xa[:, kc, :], func=AF.Identity,
                                                                bias=colmod[:, kc, b:b + 1], scale=colmod[:, 8 + kc, b:b + 1]),
                          reads=[b_xa, b_colmod], writes=[b_hT])
                pq, b_pq = gbank()
                for h in range(4):
                    for kc in range(KC):
                        em.op("pe", lambda e, h=h, kc=kc: e.matmul(pq[:, h * 128:(h + 1) * 128], lhsT=win[:, kc, h * 128:(h + 1) * 128],
                                                                    rhs=hT[:, kc, :], start=(kc == 0), stop=(kc == KC - 1)),
                              reads=[b_win, b_hT], writes=[b_pq])
                em.op("dve", lambda e: e.tensor_copy(out=dqT[:].rearrange("p h t -> p (h t)"), in_=pq[:, :]), reads=[b_pq], writes=[b_dqT])
                pk, b_pk = gbank()
                for h in range(4):
                    for kc in range(KC):
                        em.op("pe", lambda e, h=h, kc=kc: e.matmul(pk[:, h * 128:(h + 1) * 128], lhsT=win[:, kc, 512 + h * 128:512 + (h + 1) * 128],
                                                                    rhs=hT[:, kc, :], start=(kc == 0), stop=(kc == KC - 1)),
                              reads=[b_win, b_hT], writes=[b_pk])
                em.op("act", lambda e: e.activation(out=dkT[:, :, i * 128:(i + 1) * 128], in_=pk[:, :].rearrange("p (h t) -> p h t", h=4), func=AF.Copy),
                      reads=[b_pk], writes=[bkv])
                for half in range(2):
                    psq, b_psq = gbank()
                    for hh in range(4):
                        hq = half * 4 + hh
                        for kc in range(KC):
                            em.op("pe", lambda e, hq=hq, hh=hh, kc=kc: e.matmul(
                                psq[0:64, hh * 128:(hh + 1) * 128], lhsT=win[:, kc, 1536 + hq * 64:1536 + (hq + 1) * 64],
                                rhs=hT[:, kc, :], start=(kc == 0), stop=(kc == KC - 1)), reads=[b_win, b_hT], writes=[b_psq])
                    em.op("dve", lambda e, half=half: e.tensor_copy(out=sqT[:, half * 4:(half + 1) * 4, :].rearrange("p h t -> p (h t)"), in_=psq[0:64, :]),
                          reads=[b_psq], writes=[b_sqT])
                psk, b_psk = gbank()
                for gk in range(2):
                    for kc in range(KC):
                        em.op("pe", lambda e, gk=gk, kc=kc: e.matmul(
                            psk[0:64, gk * 128:(gk + 1) * 128], lhsT=win[:, kc, 2048 + gk * 64:2048 + (gk + 1) * 64],
                            rhs=hT[:, kc, :], start=(kc == 0), stop=(kc == KC - 1)), reads=[b_win, b_hT], writes=[b_psk])
                for kc in range(KC):
                    em.op("pe", lambda e, kc=kc: e.matmul(psk[:, 256:384], lhsT=hT[:, kc, :], rhs=win[:, kc, 2176:2304],
                                                           start=(kc == 0), stop=(kc == KC - 1), skip_group_check=True),
                          reads=[b_win, b_hT], writes=[b_psk])
                em.op("act", lambda e: e.activation(out=skT[:, :, i * 128:(i + 1) * 128], in_=psk[0:64, 0:256].rearrange("p (h t) -> p h t", h=2), func=AF.Copy),
                      reads=[b_psk], writes=[bkv])
                em.op("act", lambda e: e.activation(out=sv[:, i, :], in_=psk[:, 256:384], func=AF.Copy), reads=[b_psk], writes=[bkv])
                pv, b_pv = gbank()
                for kc in range(KC):
                    em.op("pe", lambda e, kc=kc: e.matmul(pv[:, :], lhsT=hT[:, kc, :], rhs=win[:, kc, 1024:1536],
                                                           start=(kc == 0), stop=(kc == KC - 1)), reads=[b_win, b_hT], writes=[b_pv])
                for h in range(4):
                    em.op("act" if h % 2 == 0 else "dve",
                          (lambda e, h=h: e.activation(out=dV[:, i, h, 0:128], in_=pv[:, h * 128:(h + 1) * 128], func=AF.Identity, bias=cst[:, 2:3], scale=wtab[:, i, h:h + 1]))
                          if h % 2 == 0 else
                          (lambda e, h=h: e.tensor_scalar(out=dV[:, i, h, 0:128], in0=pv[:, h * 128:(h + 1) * 128], scalar1=wtab[:, i, h:h + 1], scalar2=None, op0=ALU.mult)),
                          reads=[b_pv, b_wtab], writes=[bkv])
                em.op("dve", lambda e: e.tensor_copy(out=dV[:, i, :, 128], in_=wtab[:, i, :]), reads=[b_wtab], writes=[bkv])

                for h in range(4):
                    for m in range(2):
                        acc, b_acc = ACC[m]
                        for g0 in range(0, i + 1, 4):
                            kbs = list(range(g0, min(g0 + 4, i + 1)))
                            sp_, b_sp = gbank()
                            for s_, kb in enumerate(kbs):
                                em.op("pe", lambda e, s_=s_, kb=kb: e.matmul(
                                    sp_[:, s_ * 128:(s_ + 1) * 128], lhsT=dkT[64 * m:64 * m + 64, h, kb * 128:(kb + 1) * 128],
                                    rhs=dqT[64 * m:64 * m + 64, h, :], start=True, stop=(kb != i)),
                                    reads=[b_kv[kb], b_dqT], writes=[b_sp])
                                if kb == i:
                                    em.op("pe", lambda e, s_=s_: e.matmul(sp_[:, s_ * 128:(s_ + 1) * 128], lhsT=identb[:, :], rhs=maskneg[:, :],
                                                                          start=False, stop=True),
                                          reads=[b_identb, b_maskneg], writes=[b_sp])
                            n = len(kbs) * 128
                            (pt_, b_pt_) = PT[cnt3[0] % 3]; cnt3[0] += 1
                            em.op("act", lambda e, n=n, pt_=pt_: e.activation(out=pt_[:, 0:n], in_=sp_[:, 0:n], func=AF.Exp, scale=0.125),
                                  reads=[b_sp], writes=[b_pt_])
                            for s_, kb in enumerate(kbs):
                                em.op("pe", lambda e, s_=s_, kb=kb, pt_=pt_: e.matmul(
                                    acc[:, 0:129], lhsT=pt_[:, s_ * 128:(s_ + 1) * 128], rhs=dV[:, kb, h, 0:129],
                                    start=(kb == 0), stop=(kb == i)), reads=[b_pt_, b_kv[kb]], writes=[b_acc])
                    (s4, b_s4) = sm[cnt3[1] % 4]; cnt3[1] += 1
                    (o1, b_o1) = od[0]
                    (o2, b_o2) = od[1]
                    a0, b_a0 = ACC[0]
                    a1, b_a1 = ACC[1]
                    em.op("dve", lambda e: e.reciprocal(out=s4[:, 0:1], in_=a0[:, 128:129]), reads=[b_a0], writes=[b_s4])
                    em.op("dve", lambda e: e.reciprocal(out=s4[:, 1:2], in_=a1[:, 128:129]), reads=[b_a1], writes=[b_s4])
                    em.op("dve", lambda e: e.tensor_tensor(out=s4[:, 2:3], in0=s4[:, 1:2], in1=cst[:, 0:1], op=ALU.mult), reads=[b_s4, b_cst], writes=[b_s4])
                    em.op("act", lambda e: e.activation(out=o1[:], in_=a0[:, 0:128], func=AF.Identity, bias=cst[:, 2:3], scale=s4[:, 0:1]), reads=[b_a0, b_s4], writes=[b_o1])
                    em.op("dve", lambda e: e.scalar_tensor_tensor(out=o2[:], in0=a1[:, 0:128], scalar=s4[:, 2:3], in1=o1[:], op0=ALU.mult, op1=ALU.add),
                          reads=[b_a1, b_s4, b_o1], writes=[b_o2])
                    em.op("dve", lambda e: e.scalar_tensor_tensor(out=oj[:], in0=o2[:], scalar=1.0, in1=o2[:], op0=ALU.mult, op1=ALU.mult, accum_out=s4[:, 3:4]),
                          reads=[b_o2], writes=[b_oj, b_s4])
                    em.op("act", lambda e: e.activation(out=s4[:, 4:5], in_=s4[:, 3:4], func=AF.Ln, bias=cst[:, 1:2], scale=1.0 / 128.0), reads=[b_s4, b_cst], writes=[b_s4])
                    em.op("act", lambda e: e.activation(out=s4[:, 5:6], in_=s4[:, 4:5], func=AF.Exp, scale=-0.5), reads=[b_s4], writes=[b_s4])
                    em.op("dve", lambda e, h=h: e.scalar_tensor_tensor(out=on[:, h * 128:(h + 1) * 128], in0=o2[:], scalar=s4[:, 5:6], in1=subg[:], op0=ALU.mult, op1=ALU.mult),
                          reads=[b_o2, b_s4, b_subg], writes=[b_on])

                for hq in range(8):
                    gk = hq // 4
                    nk = 256 if i > 0 else 128
                    k0 = (i - 1) * 128 if i > 0 else 0
                    kvdeps = [b_kv[i]] + ([b_kv[i - 1]] if i > 0 else [])
                    sp_, b_sp = gbank()
                    em.op("pe", lambda e, hq=hq, gk=gk, nk=nk, k0=k0: e.matmul(sp_[:, 0:nk], lhsT=sqT[:, hq, :], rhs=skT[:, gk, k0:k0 + nk], start=True, stop=True),
                          reads=[b_sqT] + kvdeps, writes=[b_sp])
                    (sb_, b_sb) = ssb[hq % 2]
                    (s4, b_s4) = sm[cnt3[1] % 4]; cnt3[1] += 1
                    em.op("dve", lambda e, hq=hq, nk=nk: e.scalar_tensor_tensor(out=sb_[:, 0:nk], in0=sp_[:, 0:nk], scalar=0.125, in1=swab[:, hq, 256 - nk:256],
                                                                              op0=ALU.mult, op1=ALU.add), reads=[b_sp, b_swab], writes=[b_sb])
                    em.op("dve", lambda e, nk=nk: e.tensor_reduce(out=s4[:, 0:1], in_=sb_[:, 0:nk], axis=AX.X, op=ALU.max), reads=[b_sb], writes=[b_s4])
                    em.op("dve", lambda e, hq=hq: e.tensor_scalar(out=s4[:, 1:2], in0=s4[:, 0:1], scalar1=sinks[:, hq:hq + 1], scalar2=-1.0, op0=ALU.max, op1=ALU.mult),
                          reads=[b_s4, b_sinks], writes=[b_s4])
                    (pw, b_pw) = Psw[hq % 2]
                    em.op("act", lambda e, nk=nk: e.activation(out=pw[:, 0:nk], in_=sb_[:, 0:nk], func=AF.Exp, bias=s4[:, 1:2], scale=1.0, accum_out=s4[:, 2:3]),
                          reads=[b_sb, b_s4], writes=[b_pw, b_s4])
                    em.op("act", lambda e, hq=hq: e.activation(out=s4[:, 3:4], in_=s4[:, 1:2], func=AF.Exp, bias=sinks[:, hq:hq + 1], scale=1.0),
                          reads=[b_s4, b_sinks], writes=[b_s4])
                    em.op("dve", lambda e: e.tensor_tensor(out=s4[:, 4:5], in0=s4[:, 2:3], in1=s4[:, 3:4], op=ALU.add), reads=[b_s4], writes=[b_s4])
                    em.op("dve", lambda e: e.reciprocal(out=s4[:, 5:6], in_=s4[:, 4:5]), reads=[b_s4], writes=[b_s4])
                    tp, b_tp = gbank()
                    tpv = tp[:, 0:128].bitcast(BF16)
                    for bl in range(nk // 128):
                        em.op("pe", lambda e, bl=bl: e.transpose(out=tpv[:, bl * 128:(bl + 1) * 128], in_=pw[:, bl * 128:(bl + 1) * 128], identity=identb[:]),
                              reads=[b_pw, b_identb], writes=[b_tp])
                    (pts, b_pts) = PTs[hq % 2]
                    em.op("act", lambda e, nk=nk: e.activation(out=pts[:, 0:nk], in_=tpv[:, 0:nk], func=AF.Copy), reads=[b_tp], writes=[b_pts])
                    for bl in range(nk // 128):
                        kt = (i - 1 + bl) if i > 0 else i
                        em.op("pe", lambda e, bl=bl, kt=kt, gk=gk, nk=nk: e.matmul(tp[:, 256:320], lhsT=pts[:, bl * 128:(bl + 1) * 128], rhs=sv[:, kt, gk * 64:(gk + 1) * 64],
                                                                                   start=(bl == 0), stop=(bl == nk // 128 - 1), skip_group_check=True),
                              reads=[b_pts] + kvdeps, writes=[b_tp])
                    em.op("act", lambda e, hq=hq: e.activation(out=on[:, 512 + hq * 64:512 + (hq + 1) * 64], in_=tp[:, 256:320], func=AF.Identity, bias=cst[:, 2:3], scale=s4[:, 5:6]),
                          reads=[b_tp, b_s4], writes=[b_on])

                tp, b_tp = gbank()
                tpv = tp[:, :].bitcast(BF16)
                for c in range(KC):
                    em.op("pe", lambda e, c=c: e.transpose(out=tpv[:, c * 128:(c + 1) * 128], in_=on[:, c * 128:(c + 1) * 128], identity=identb[:]),
                          reads=[b_on, b_identb], writes=[b_tp])
                em.op("dve", lambda e: e.tensor_copy(out=oT[:].rearrange("p c t -> p (c t)"), in_=tpv[:, :]), reads=[b_tp], writes=[b_oT])
                for half in range(2):
                    mx, b_mx = MIX[half]
                    for c in range(KC):
                        em.op("pe", lambda e, c=c, half=half, mx=mx: e.matmul(mx[:, :], lhsT=oT[:, c, :], rhs=wout[:, c, half * 512:(half + 1) * 512],
                                                                             start=(c == 0), stop=(c == KC - 1)), reads=[b_oT, b_wout], writes=[b_mx])
                for half in range(2):
                    mx, b_mx = MIX[half]
                    em.op("dve", lambda e, half=half, mx=mx: e.tensor_tensor(out=tA[:, half * 512:(half + 1) * 512], in0=mx[:, :], in1=rows[:, 0, half * 512:(half + 1) * 512], op=ALU.mult),
                          reads=[b_mx, b_rows], writes=[b_tA])
                em.op("dve", lambda e: e.scalar_tensor_tensor(out=tB[:], in0=xb[:], scalar=ALPHA, in1=tA[:], op0=ALU.mult, op1=ALU.add),
                      reads=[b_xb, b_tA], writes=[b_tB])
                (s4, b_s4) = sm[cnt3[1] % 4]; cnt3[1] += 1
                for c in range(2):
                    em.op("dve", lambda e, c=c: e.bn_stats(out=st6[:, c, :], in_=tB[:, c * 512:(c + 1) * 512]), reads=[b_tB], writes=[b_st6])
                em.op("dve", lambda e: e.bn_aggr(out=s4[:, 0:2], in_=st6[:].rearrange("p a b -> p (a b)")), reads=[b_st6], writes=[b_s4])
                em.op("act", lambda e: e.activation(out=s4[:, 2:3], in_=s4[:, 1:2], func=AF.Ln, bias=cst[:, 1:2], scale=1.0), reads=[b_s4, b_cst], writes=[b_s4])
                em.op("act", lambda e: e.activation(out=s4[:, 3:4], in_=s4[:, 2:3], func=AF.Exp, scale=-0.5), reads=[b_s4], writes=[b_s4])
                em.op("dve", lambda e: e.tensor_scalar(out=tA[:], in0=tB[:], scalar1=s4[:, 0:1], scalar2=s4[:, 3:4], op0=ALU.subtract, op1=ALU.mult),
                      reads=[b_tB, b_s4], writes=[b_tA])
                (x1, b_x1) = x1o[g % 2]
                (h2, b_h2) = h2o[g % 2]
                em.op("pool", lambda e: e.tensor_tensor(out=tB[:], in0=tA[:], in1=ln1[:, 0, :], op=ALU.mult), reads=[b_tA, b_ln1], writes=[b_tB])
                em.op("pool", lambda e: e.tensor_tensor(out=x1[:], in0=tB[:], in1=ln1[:, 1, :], op=ALU.add), reads=[b_tB, b_ln1], writes=[b_x1])
                em.dma("sp", lambda e: e.dma_start(out=x1s_d[g * 128:(g + 1) * 128, :], in_=x1[:]), b_x1, reads=[b_x1])
                em.op("pool", lambda e: e.tensor_tensor(out=tA[:], in0=x1[:], in1=rows[:, 2, :], op=ALU.mult), reads=[b_x1, b_rows], writes=[b_tA])
                em.op("pool", lambda e: e.tensor_tensor(out=h2[:], in0=tA[:], in1=rows[:, 1, :], op=ALU.add), reads=[b_tA, b_rows], writes=[b_h2])
                em.dma("sp", lambda e: e.dma_start(out=h2s_d[g * 128:(g + 1) * 128, :], in_=h2[:]), b_h2, reads=[b_h2])
            em.barrier()

        with ExitStack() as pb:
            wpq, b_wpq = SB(pb, "wpq", [128, KC, 2048])
            skTp, b_skTp = SB(pb, "skTp", [128, 16, 128])
            identf, b_identf = SB(pb, "identf", [128, 128])
            ln2, b_ln2 = SB(pb, "ln2", [128, 2, D])
            g2r, b_g2r = SB(pb, "g2r", [128, D])
            GB = [SB(pb, f"GB{i}", [128, D], BF16) for i in range(NSLOT)]
            x1t = [SB(pb, f"x1t{i}", [128, D]) for i in range(2)]
            h2t = [SB(pb, f"h2t{i}", [128, D]) for i in range(2)]
            h2T, b_h2T = SB(pb, "h2T", [128, KC, 128])
            qT, b_qT = SB(pb, "qT", [128, 16, 128])
            vals, b_vals = SB(pb, "vals", [128, 16, 16])
            idxs, b_idxs = SB(pb, "idxs", [128, 16, 16], U32)
            idxf, b_idxf = SB(pb, "idxf", [128, 16, 16])
            scw = [SB(pb, f"scw{i}", [128, 128]) for i in range(2)]
            cand = [SB(pb, f"cand{i}", [128, 256]) for i in range(2)]
            cidx = [SB(pb, f"cidx{i}", [128, 256]) for i in range(2)]
            cw = [SB(pb, f"cw{i}", [128, 256]) for i in range(2)]
            tops, b_tops = SB(pb, "tops", [128, 8, 16])
            gate = [SB(pb, f"gate{i}", [128, 8, 16]) for i in range(2)]
            eidf, b_eidf = SB(pb, "eidf", [128, 128])
            eid = [SB(pb, f"eid{i}", [128, 128], I32) for i in range(2)]
            eidT = [SB(pb, f"eidT{i}", [128, 128], I32) for i in range(2)]
            apre, b_apre = SB(pb, "apre", [128, 128])
            ga, b_ga = SB(pb, "ga", [128, 128])
            gaT, b_gaT = SB(pb, "gaT", [128, 128], BF16)
            junks = [SB(pb, f"junk{i}", [128, D], BF16) for i in range(4)]
            junk2s = [SB(pb, f"junk2_{i}", [128, 256], BF16) for i in range(4)]
            ffnT, b_ffnT = SB(pb, "ffnT", [128, KC, 128])
            wA, b_wA = SB(pb, "wA", [128, D])
            wB, b_wB = SB(pb, "wB", [128, D])
            outt = [SB(pb, f"outt{i}", [128, D]) for i in range(2)]
            st6b, b_st6b = SB(pb, "st6b", [128, 2, 6])
            smb = [SB(pb, f"smb{i}", [128, 16]) for i in range(2)]

            load_consts([(wpq[:, 0:4, :], wpq_d[:, 0:4, :], b_wpq), (wpq[:, 4:8, :], wpq_d[:, 4:8, :], b_wpq),
                         (skTp[:], skT_d, b_skTp), (identf[:], identf_d, b_identf), (ln2[:], lnr_d[:, 2:4, :], b_ln2)])
            VT = [banks[0], banks[1]]
            bi = [0]

            def nbank():
                r = banks[2 + bi[0] % 6]; bi[0] += 1
                return r

            def load_tile_b(g):
                (a, b_a) = x1t[g % 2]
                (h, b_h) = h2t[g % 2]
                em.dma("sp", lambda e: e.dma_start(out=h[:], in_=h2s_d[g * 128:(g + 1) * 128, :]), b_h, writes=[b_h])
                em.dma("sp", lambda e: e.dma_start(out=a[:], in_=x1s_d[g * 128:(g + 1) * 128, :]), b_a, writes=[b_a])

            def score(g):
                (hh, b_hh) = h2t[g % 2]
                for half in range(2):
                    tp, b_tp = nbank()
                    for c4 in range(4):
                        c = half * 4 + c4
                        em.op("pe", lambda e, c=c, c4=c4, tp=tp: e.transpose(out=tp[:, c4 * 128:(c4 + 1) * 128], in_=hh[:, c * 128:(c + 1) * 128], identity=identf[:]),
                              reads=[b_hh, b_identf], writes=[b_tp])
                    em.op("act", lambda e, tp=tp, half=half: e.activation(out=h2T[:, half * 4:(half + 1) * 4, :].rearrange("p c t -> p (c t)"), in_=tp[:, :], func=AF.Copy),
                          reads=[b_tp], writes=[b_h2T])
                for q4 in range(4):
                    pq, b_pq = nbank()
                    for c4 in range(4):
                        c16 = q4 * 4 + c4
                        for kc in range(KC):
                            em.op("pe", lambda e, c16=c16, c4=c4, kc=kc, pq=pq: e.matmul(pq[:, c4 * 128:(c4 + 1) * 128], lhsT=wpq[:, kc, c16 * 128:(c16 + 1) * 128],
                                                                                       rhs=h2T[:, kc, :], start=(kc == 0), stop=(kc == KC - 1)),
                                  reads=[b_wpq, b_h2T], writes=[b_pq])
                    em.op("act", lambda e, pq=pq, q4=q4: e.activation(out=qT[:, q4 * 4:(q4 + 1) * 4, :].rearrange("p c t -> p (c t)"), in_=pq[:, :], func=AF.Copy),
                          reads=[b_pq], writes=[b_qT])
                scb = []
                for q4 in range(4):
                    ps_, b_ps = nbank()
                    scb.append((ps_, b_ps))
                    for c4 in range(4):
                        c16 = q4 * 4 + c4
                        em.op("pe", lambda e, c16=c16, c4=c4, ps_=ps_: e.matmul(ps_[:, c4 * 128:(c4 + 1) * 128], lhsT=qT[:, c16, :], rhs=skTp[:, c16, :], start=True, stop=True),
                              reads=[b_qT, b_skTp], writes=[b_ps])
                return scb

            def topk(g, scb):
                for c16 in range(16):
                    ps_, b_ps = scb[c16 // 4]
                    src = ps_[:, (c16 % 4) * 128:(c16 % 4 + 1) * 128]
                    (sw, b_sw) = scw[c16 % 2]
                    em.op("dve", lambda e, c16=c16, src=src: e.max(out=vals[:, c16, 0:8], in_=src), reads=[b_ps], writes=[b_vals])
                    em.op("dve", lambda e, c16=c16, src=src: e.max_index(out=idxs[:, c16, 0:8], in_max=vals[:, c16, 0:8], in_values=src), reads=[b_ps, b_vals], writes=[b_idxs])
                    em.op("dve", lambda e, c16=c16, src=src, sw=sw: e.match_replace(out=sw[:], in_to_replace=vals[:, c16, 0:8], in_values=src, imm_value=-1e30),
                          reads=[b_ps, b_vals], writes=[b_sw])
                    em.op("dve", lambda e, c16=c16, sw=sw: e.max(out=vals[:, c16, 8:16], in_=sw[:]), reads=[b_sw], writes=[b_vals])
                    em.op("dve", lambda e, c16=c16, sw=sw: e.max_index(out=idxs[:, c16, 8:16], in_max=vals[:, c16, 8:16], in_values=sw[:]), reads=[b_sw, b_vals], writes=[b_idxs])
                em.op("dve", lambda e: e.tensor_copy(out=idxf[:], in_=idxs[:]), reads=[b_idxs], writes=[b_idxf])
                i4 = idxf[:].rearrange("p (h two) k -> p h two k", two=2)
                em.op("dve", lambda e: e.tensor_scalar(out=i4[:, :, 0, :], in0=i4[:, :, 0, :], scalar1=128.0, scalar2=None, op0=ALU.mult), reads=[b_idxf], writes=[b_idxf])
                for h in range(8):
                    (cd, b_cd) = cand[h % 2]
                    (ci, b_ci) = cidx[h % 2]
                    (cw_, b_cw) = cw[h % 2]
                    em.op("dve", lambda e, h=h, cd=cd: e.tensor_tensor(out=cd[:].rearrange("p (i j) -> p i j", i=16),
                                                                     in0=vals[:, 2 * h, :].unsqueeze(2).to_broadcast([128, 16, 16]),
                                                                     in1=vals[:, 2 * h + 1, :].unsqueeze(1).to_broadcast([128, 16, 16]), op=ALU.add),
                          reads=[b_vals], writes=[b_cd])
                    em.op("dve", lambda e, h=h, ci=ci: e.tensor_tensor(out=ci[:].rearrange("p (i j) -> p i j", i=16),
                                                                     in0=idxf[:, 2 * h, :].unsqueeze(2).to_broadcast([128, 16, 16]),
                                                                     in1=idxf[:, 2 * h + 1, :].unsqueeze(1).to_broadcast([128, 16, 16]), op=ALU.add),
                          reads=[b_idxf], writes=[b_ci])
                    em.op("dve", lambda e, h=h, cd=cd: e.max(out=tops[:, h, 0:8], in_=cd[:]), reads=[b_cd], writes=[b_tops])
                    em.op("dve", lambda e, h=h, cd=cd, cw_=cw_: e.match_replace(out=cw_[:], in_to_replace=tops[:, h, 0:8], in_values=cd[:], imm_value=-1e30),
                          reads=[b_cd, b_tops], writes=[b_cw])
                    em.op("dve", lambda e, h=h, cw_=cw_: e.max(out=tops[:, h, 8:16], in_=cw_[:]), reads=[b_cw], writes=[b_tops])
                    for k in range(16):
                        last = (k == 15)
                        (j2, b_j2) = junk2s[k % 4]
                        em.op("dve", lambda e, h=h, k=k, cd=cd, ci=ci, j2=j2: e.scalar_tensor_tensor(
                            out=j2[:], in0=cd[:], scalar=tops[:, h, k:k + 1], in1=ci[:], op0=ALU.is_equal, op1=ALU.mult,
                            accum_out=eidf[:, h * 16 + k:h * 16 + k + 1]),
                            reads=[b_cd, b_ci, b_tops], writes=([b_j2, b_eidf] if (last and h == 7) else [b_j2]))
                (ei, b_ei) = eid[g % 2]
                (eiT, b_eiT) = eidT[g % 2]
                em.op("dve", lambda e: e.tensor_scalar(out=eidf[:], in0=eidf[:], scalar1=float(NEXP - 1), scalar2=0.0, op0=ALU.min, op1=ALU.max),
                      reads=[b_eidf], writes=[b_eidf])
                em.op("dve", lambda e: e.tensor_copy(out=ei[:], in_=eidf[:]), reads=[b_eidf], writes=[b_ei])
                (gt, b_gt) = gate[g % 2]
                em.op("dve", lambda e: e.tensor_tensor(out=gt[:], in0=tops[:], in1=tops[:, :, 0:1].to_broadcast([128, 8, 16]), op=ALU.subtract),
                      reads=[b_tops], writes=[b_gt])
                em.op("act", lambda e: e.activation(out=gt[:], in_=gt[:], func=AF.Exp), reads=[b_gt], writes=[b_gt])
                (s8, b_s8) = smb[g % 2]
                em.op("dve", lambda e: e.tensor_reduce(out=s8[:, 0:8], in_=gt[:], axis=AX.X, op=ALU.add), reads=[b_gt], writes=[b_s8])
                em.op("dve", lambda e: e.reciprocal(out=s8[:, 8:16], in_=s8[:, 0:8]), reads=[b_s8], writes=[b_s8])
                em.op("dve", lambda e: e.tensor_tensor(out=gt[:], in0=gt[:], in1=s8[:, 8:16].unsqueeze(2).to_broadcast([128, 8, 16]), op=ALU.mult),
                      reads=[b_gt, b_s8], writes=[b_gt])
                tp, b_tp = nbank()
                em.op("pe", lambda e: e.transpose(out=tp[:, 0:128], in_=eidf[:], identity=identf[:]), reads=[b_eidf, b_identf], writes=[b_tp])
                em.op("act", lambda e: e.activation(out=eiT[:], in_=tp[:, 0:128], func=AF.Copy), reads=[b_tp], writes=[b_eiT])

            slot = [0]
            NG = NB * NT
            load_tile_b(0)
            scb_cur = score(0)
            topk(0, scb_cur)
            for g in range(NG):
                b, i = divmod(g, NT)
                if i == 0:
                    em.dma("sp", lambda e: e.dma_start(out=g2r[:], in_=rows_d[b, 3, :, :]), b_g2r, writes=[b_g2r])
                if g + 1 < NG:
                    load_tile_b(g + 1)
                    scb_next = score(g + 1)
                (xa, b_xa) = x1t[g % 2]
                (hh, b_hh) = h2t[g % 2]
                (ei, b_ei) = eid[g % 2]
                (eiT, b_eiT) = eidT[g % 2]
                (gt, b_gt) = gate[g % 2]
                (s8, b_s8) = smb[g % 2]
                for j in range(128):
                    (gb, b_gb) = GB[slot[0] % NSLOT]; slot[0] += 1
                    em.dma("pool", lambda e, j=j, gb=gb: e.indirect_dma_start(out=gb[:, :], out_offset=None, in_=ubf_d,
                                                                           in_offset=bass.IndirectOffsetOnAxis(ap=ei[:, j:j + 1], axis=0)),
                           b_gb, reads=[b_ei], writes=[b_gb])
                    (j1, b_j1) = junks[j % 4]
                    em.op("dve", lambda e, j=j, gb=gb, j1=j1: e.scalar_tensor_tensor(out=j1[:], in0=gb[:], scalar=1.0, in1=hh[:], op0=ALU.mult, op1=ALU.mult,
                                                                            accum_out=apre[:, j:j + 1]),
                          reads=[b_gb, b_hh], writes=([b_j1, b_apre] if j == 127 else [b_j1]))
                em.op("act", lambda e: e.activation(out=ga[:], in_=apre[:], func=AF.Gelu), reads=[b_apre], writes=[b_ga])
                em.op("dve", lambda e: e.tensor_tensor(out=ga[:], in0=ga[:], in1=gt[:].rearrange("p h k -> p (h k)"), op=ALU.mult),
                      reads=[b_ga, b_gt], writes=[b_ga])
                tp, b_tp = nbank()
                em.op("pe", lambda e: e.transpose(out=tp[:, 0:128], in_=ga[:], identity=identf[:]), reads=[b_ga, b_identf], writes=[b_tp])
                em.op("act", lambda e: e.activation(out=gaT[:], in_=tp[:, 0:128], func=AF.Copy), reads=[b_tp], writes=[b_gaT])
                for t in range(128):
                    (gb, b_gb) = GB[slot[0] % NSLOT]; slot[0] += 1
                    em.dma("pool", lambda e, t=t, gb=gb: e.indirect_dma_start(out=gb[:, :], out_offset=None, in_=vbf_d,
                                                                           in_offset=bass.IndirectOffsetOnAxis(ap=eiT[:, t:t + 1], axis=0)),
                           b_gb, reads=[b_eiT], writes=[b_gb])
                    for c in range(KC):
                        vt, b_vt = VT[c // 4]
                        col = (c % 4) * 128 + t
                        em.op("pe", lambda e, c=c, t=t, gb=gb, vt=vt, col=col: e.matmul(vt[:, col:col + 1], lhsT=gb[:, c * 128:(c + 1) * 128], rhs=gaT[:, t:t + 1],
                                                                                      start=True, stop=True, skip_group_check=True),
                              reads=[b_gb, b_gaT], writes=[b_vt])
                if g + 1 < NG:
                    topk(g + 1, scb_next)
                for half in range(2):
                    vt, b_vt = VT[half]
                    em.op("act", lambda e, half=half, vt=vt: e.activation(out=ffnT[:, half * 4:(half + 1) * 4, :].rearrange("p c t -> p (c t)"), in_=vt[:, :], func=AF.Copy),
                          reads=[b_vt], writes=[b_ffnT])
                fts = []
                for half in range(2):
                    ft, b_ft = nbank()
                    fts.append((ft, b_ft))
                    for c4 in range(4):
                        c = half * 4 + c4
                        em.op("pe", lambda e, c=c, c4=c4, ft=ft: e.transpose(out=ft[:, c4 * 128:(c4 + 1) * 128], in_=ffnT[:, c, :], identity=identf[:]),
                              reads=[b_ffnT, b_identf], writes=[b_ft])
                for half in range(2):
                    ft, b_ft = fts[half]
                    em.op("dve", lambda e, half=half, ft=ft: e.tensor_tensor(out=wB[:, half * 512:(half + 1) * 512], in0=ft[:, :], in1=g2r[:, half * 512:(half + 1) * 512], op=ALU.mult),
                          reads=[b_ft, b_g2r], writes=[b_wB])
                em.op("dve", lambda e: e.scalar_tensor_tensor(out=wA[:], in0=xa[:], scalar=ALPHA, in1=wB[:], op0=ALU.mult, op1=ALU.add),
                      reads=[b_xa, b_wB], writes=[b_wA])
                for c in range(2):
                    em.op("dve", lambda e, c=c: e.bn_stats(out=st6b[:, c, :], in_=wA[:, c * 512:(c + 1) * 512]), reads=[b_wA], writes=[b_st6b])
                em.op("dve", lambda e: e.bn_aggr(out=s8[:, 0:2], in_=st6b[:].rearrange("p a b -> p (a b)")), reads=[b_st6b], writes=[b_s8])
                em.op("act", lambda e: e.activation(out=s8[:, 2:3], in_=s8[:, 1:2], func=AF.Ln, bias=cst[:, 1:2], scale=1.0), reads=[b_s8, b_cst], writes=[b_s8])
                em.op("act", lambda e: e.activation(out=s8[:, 3:4], in_=s8[:, 2:3], func=AF.Exp, scale=-0.5), reads=[b_s8], writes=[b_s8])
                em.op("dve", lambda e: e.tensor_scalar(out=wB[:], in0=wA[:], scalar1=s8[:, 0:1], scalar2=s8[:, 3:4], op0=ALU.subtract, op1=ALU.mult),
                      reads=[b_wA, b_s8], writes=[b_wB])
                (ot, b_ot) = outt[g % 2]
                em.op("dve", lambda e: e.tensor_tensor(out=wA[:], in0=wB[:], in1=ln2[:, 0, :], op=ALU.mult), reads=[b_wB, b_ln2], writes=[b_wA])
                em.op("dve", lambda e: e.tensor_tensor(out=ot[:], in0=wA[:], in1=ln2[:, 1, :], op=ALU.add), reads=[b_wA, b_ln2], writes=[b_ot])
                em.dma("sp", lambda e: e.dma_start(out=out_d[g * 128:(g + 1) * 128, :], in_=ot[:]), b_ot, reads=[b_ot])
                if g + 1 < NG:
                    scb_cur = scb_next
            em.barrier()
        build.info = dict(ninstr=dict(em.ninstr), nsem=em.nsem)
    return nc


def _host_layout(inputs, NB, S):
    f32 = np.float32
    x = np.ascontiguousarray(inputs["x"], dtype=f32)
    B = x.shape[0]
    NT = S // 128
    c = np.asarray(inputs["c"], f32)

    def kcl(w):
        return np.ascontiguousarray(w.reshape(KC, 128, -1).transpose(1, 0, 2))

    def rep(v, n=128):
        return np.ascontiguousarray(np.broadcast_to(np.asarray(v, f32).reshape(1, -1), (n, np.asarray(v).size)))

    shared = {
        "w_ada": kcl(np.asarray(inputs["w_ada"][0], f32)),
        "b_ada_col": np.ascontiguousarray(np.asarray(inputs["b_ada"][0], f32).reshape(48, 128).T),
        "b_ada_row": np.ascontiguousarray(np.asarray(inputs["b_ada"][0], f32).reshape(1, -1)),
        "w_in": kcl(np.asarray(inputs["w_in"][0], f32)),
        "lam_in": np.ascontiguousarray(np.concatenate([rep(inputs["lambda_q1"][0]), rep(inputs["lambda_k1"][0]),
                                                       rep(inputs["lambda_q2"][0]), rep(inputs["lambda_k2"][0])], axis=1)),
        "subln_g": rep(inputs["subln_g"][0]),
        "sinks": rep(inputs["sinks"][0]),
        "w_out": kcl(np.asarray(inputs["w_out"][0], f32)),
        "ln_rows": np.ascontiguousarray(np.stack([rep(inputs["ln1_g"][0]), rep(inputs["ln1_b"][0]),
                                                  rep(inputs["ln2_g"][0]), rep(inputs["ln2_b"][0])], axis=1)),
        "w_pq": kcl(np.asarray(inputs["w_pq"][0], f32)),
        "skT": np.ascontiguousarray(np.asarray(inputs["sub_keys"][0], f32).reshape(16, 128, 128).transpose(2, 0, 1)),
        "u_tab": np.ascontiguousarray(np.asarray(inputs["u_tab"][0], f32)),
        "v_tab": np.ascontiguousarray(np.asarray(inputs["v_tab"][0], f32)),
    }
    sl = _slopes()
    kk = np.arange(128)
    shared["identb"] = np.eye(128, dtype=f32).astype(ml_dtypes.bfloat16)
    shared["identf"] = np.eye(128, dtype=f32)
    shared["maskneg"] = np.where(kk[:, None] > kk[None, :], -30000.0, 0.0).astype(f32).astype(ml_dtypes.bfloat16)
    kpos = (np.arange(NT)[None, :] * 128 + kk[:, None]).astype(np.float64)
    shared["wtab"] = np.exp(sl[8:12][None, None, :].astype(np.float64) * (kpos[:, :, None] - (S - 1))).astype(f32)
    qi = np.arange(128)[:, None]
    kj = np.arange(256)[None, :]
    dist = qi - kj + 128
    valid = (dist >= 0) & (dist < 128)
    swab = np.where(valid[:, None, :], -sl[:8][None, :, None] * dist[:, None, :].astype(f32), f32(-1e30)).astype(f32)
    shared["swab"] = np.ascontiguousarray(swab)

    in_maps = []
    for core in range(NCORES):
        xs = x[core * NB:(core + 1) * NB]
        m = dict(shared)
        m["x_tok"] = np.ascontiguousarray(xs.reshape(NB * S, D))
        m["xT"] = np.ascontiguousarray(xs.reshape(NB, S, KC, 128).transpose(0, 3, 2, 1))
        m["cT"] = np.ascontiguousarray(c[core * NB:(core + 1) * NB].reshape(NB, KC, 128).transpose(2, 1, 0))
        in_maps.append(m)
    return in_maps


_CACHE = {}


def kernel(**inputs):
    x = np.asarray(inputs["x"])
    B, S, _ = x.shape
    NB = B // NCORES
    key = (NB, S)
    if key not in _CACHE:
        _CACHE[key] = build(NB, S)
    nc = _CACHE[key]
    in_maps = _host_layout(inputs, NB, S)
    res = run_bass_kernel_spmd(nc, in_maps, core_ids=list(range(NCORES)))
    outs = [np.asarray(res.results[cidx]["out"], np.float32).reshape(NB, S, D) for cidx in range(NCORES)]
    return np.concatenate(outs, axis=0)
```
